# Optimizing a Trainium2 kernel written in Bass

```python
import functools
import jax
import jax.numpy as jnp
from jax import lax
import numpy as np

D_MODEL = 1024
BATCH = 8
SEQ = 2048
DEPTH = 4

GRID_W = 64
CTX_LEN = 256
HEAD_DIM = 128
RET_HEADS = 4
MLSTM_HEADS = 4
NA_HEADS = 4
RET_WIDTH = RET_HEADS * HEAD_DIM
MLSTM_WIDTH = MLSTM_HEADS * HEAD_DIM
NA_WIDTH = NA_HEADS * HEAD_DIM
N_BRANCHES = 3
GATE_COLS = N_BRANCHES * D_MODEL
IN_COLS = 4 * RET_WIDTH + 4 * MLSTM_WIDTH + 4 * MLSTM_HEADS + 3 * NA_WIDTH + GATE_COLS
CHUNK = 128
CONV_WIDTH = 5
NA_WIN_ROWS = 8
NA_WIN_COLS = 16
ROPE_BASE = 10000.0
N_GROUPS = 4
EXPERTS_PER_GROUP = 8
N_EXPERTS = N_GROUPS * EXPERTS_PER_GROUP
TOP_K = 2
EXPERT_FF = 512
EXPERT_BLOCK = 256
EPS = 1e-6
NEG_INF = -1e30

kernel_name = 'hybrid_ret_mlstm_natten_hmoe_dit'


def rms_norm(x, g):
    x32 = x.astype(jnp.float32)
    y = x32 * lax.rsqrt(jnp.mean(x32 * x32, axis=-1, keepdims=True) + EPS)
    return (y * g.astype(jnp.float32)).astype(x.dtype)


def modulate(h, shift, scale):
    return h * (1 + scale) + shift


def head_group_norm(y, g):
    b, s, h, dh = y.shape
    y32 = y.astype(jnp.float32)
    mu = jnp.mean(y32, axis=-1, keepdims=True)
    var = jnp.mean(jnp.square(y32 - mu), axis=-1, keepdims=True)
    y32 = (y32 - mu) * lax.rsqrt(var + EPS)
    return (y32.reshape(b, s, h * dh) * g.astype(jnp.float32)).astype(y.dtype)


def split_heads(a, n_heads):
    return a.reshape(a.shape[0], a.shape[1], n_heads, HEAD_DIM)


def to_scan(a):
    return jnp.swapaxes(a, 1, 2).astype(jnp.float32)


def from_scan(a, dtype):
    return jnp.swapaxes(a, 1, 2).astype(dtype)


def split_projection(p):
    sizes = [RET_WIDTH] * 4 + [MLSTM_WIDTH] * 4 + [4 * MLSTM_HEADS] + [NA_WIDTH] * 3 + [GATE_COLS]
    return jnp.split(p, np.cumsum(sizes)[:-1].tolist(), axis=-1)


def axial_rope_tables(n_tokens):
    t = jnp.arange(n_tokens)
    rows = (t // GRID_W).astype(jnp.float32)
    cols = (t % GRID_W).astype(jnp.float32)
    n_freq = HEAD_DIM // 4
    inv_freq = ROPE_BASE ** (-jnp.arange(n_freq, dtype=jnp.float32) / n_freq)
    ang = jnp.stack([rows[:, None] * inv_freq, cols[:, None] * inv_freq], axis=1)
    return jnp.cos(ang)[:, None, :, None, :], jnp.sin(ang)[:, None, :, None, :]


def apply_axial_rope(x, cos, sin):
    b, s, h, dh = x.shape
    xr = x.astype(jnp.float32).reshape(b, s, h, 2, 2, dh // 4)
    rot = jnp.stack([-xr[..., 1, :], xr[..., 0, :]], axis=-2)
    return (xr * cos + rot * sin).reshape(b, s, h, dh).astype(x.dtype)


def retention_scan(q, k, v, log_gamma, state0, with_out):
    b, h, s, dh = q.shape
    n_chunks = s // CHUNK
    idx = jnp.arange(CHUNK, dtype=jnp.float32)
    diff = idx[:, None] - idx[None, :]
    tri = diff >= 0
    intra = jnp.where(tri, jnp.exp(jnp.where(tri, diff, 0.0)[None] * log_gamma[:, None, None]), 0.0)
    q_decay = jnp.exp((idx + 1.0)[None] * log_gamma[:, None])[:, :, None]
    k_decay = jnp.exp((CHUNK - 1.0 - idx)[None] * log_gamma[:, None])[:, :, None]
    chunk_decay = jnp.exp(CHUNK * log_gamma)[:, None, None]

    def chunks(a):
        return jnp.moveaxis(a.reshape(b, h, n_chunks, CHUNK, dh), 2, 0)

    def step(s_prev, inp):
        qb, kb, vb = inp
        s_new = s_prev * chunk_decay + jnp.einsum('bhcd,bhce->bhde', kb * k_decay, vb)
        if not with_out:
            return s_new, None
        scores = jnp.einsum('bhid,bhjd->bhij', qb, kb) * intra
        o = (jnp.einsum('bhij,bhje->bhie', scores, vb)
             + jnp.einsum('bhid,bhde->bhie', qb * q_decay, s_prev))
        return s_new, o

    s_fin, o = lax.scan(step, state0, (chunks(q), chunks(k), chunks(v)))
    if not with_out:
        return None, s_fin
    return jnp.moveaxis(o, 0, 2).reshape(b, h, s, dh), s_fin


def mlstm_scan(q, k, v, i_pre, log_f, state0, with_out):
    b, h, s, dh = q.shape
    n_chunks = s // CHUNK
    tri = jnp.tril(jnp.ones((CHUNK, CHUNK), dtype=bool))

    def chunks(a):
        return jnp.moveaxis(a.reshape(b, h, n_chunks, CHUNK, *a.shape[3:]), 2, 0)

    def step(carry, inp):
        c_prev, n_prev, m_prev = carry
        qb, kb, vb, ib, fb = inp
        cum_f = jnp.cumsum(fb, axis=-1)
        total_f = cum_f[..., -1]
        log_kw = total_f[..., None] - cum_f + ib
        m_new = jnp.maximum(total_f + m_prev, jnp.max(log_kw, axis=-1))
        kw = jnp.exp(log_kw - m_new[..., None])
        pw = jnp.exp(total_f + m_prev - m_new)
        c_new = pw[..., None, None] * c_prev + jnp.einsum('bhjd,bhje->bhde', kb * kw[..., None], vb)
        n_new = pw[..., None] * n_prev + jnp.einsum('bhj,bhjd->bhd', kw, kb)
        if not with_out:
            return (c_new, n_new, m_new), None
        log_w = jnp.where(tri, cum_f[..., :, None] - cum_f[..., None, :] + ib[..., None, :], -jnp.inf)
        log_p = cum_f + m_prev[..., None]
        m_t = jnp.maximum(log_p, jnp.max(log_w, axis=-1))
        w = jnp.exp(log_w - m_t[..., None])
        p = jnp.exp(log_p - m_t)
        qk = jnp.einsum('bhid,bhjd->bhij', qb, kb) * w
        num = jnp.einsum('bhij,bhje->bhie', qk, vb) + p[..., None] * jnp.einsum('bhid,bhde->bhie', qb, c_prev)
        den = jnp.sum(qk, axis=-1) + p * jnp.einsum('bhid,bhd->bhi', qb, n_prev)
        h_t = num / jnp.maximum(jnp.abs(den), jnp.exp(-m_t))[..., None]
        return (c_new, n_new, m_new), h_t

    state, hs = lax.scan(step, state0, (chunks(q), chunks(k), chunks(v), chunks(i_pre), chunks(log_f)))
    if not with_out:
        return None, state
    return jnp.moveaxis(hs, 0, 2).reshape(b, h, s, dh), state


def bidirectional_scan(scan_f, scan_b, lat_f, lat_b, ctx_f, ctx_b, state0, ctx_out):
    flip = lambda arrs: tuple(jnp.flip(a, axis=2) for a in arrs)
    o_ctx_f, st_f = scan_f(*ctx_f, state0=state0, with_out=ctx_out)
    o_ctx_b, st_b = scan_b(*flip(ctx_b), state0=state0, with_out=ctx_out)
    o_lat_f, _ = scan_f(*lat_f, state0=st_f, with_out=True)
    o_lat_b, _ = scan_b(*flip(lat_b), state0=st_b, with_out=True)
    y_lat = o_lat_f + jnp.flip(o_lat_b, axis=2)
    if not ctx_out:
        return y_lat, None
    return y_lat, o_ctx_f + jnp.flip(o_ctx_b, axis=2)


def short_conv(u, w, bias):
    y = lax.conv_general_dilated(
        u, w[:, None, :].astype(u.dtype), window_strides=(1,),
        padding=[(CONV_WIDTH // 2, CONV_WIDTH // 2)],
        dimension_numbers=('NWC', 'WIO', 'NWC'), feature_group_count=u.shape[-1])
    return jax.nn.silu(y + bias)


def neighbourhood_attention(q, k, v, k_ctx, v_ctx, rpb):
    b, s, h, dh = q.shape
    rows = s // GRID_W
    win_r = min(NA_WIN_ROWS, rows)
    scale = dh ** -0.5
    qg = q.reshape(b, rows, GRID_W, h, dh)
    kg = k.reshape(b, rows, GRID_W, h, dh)
    vg = v.reshape(b, rows, GRID_W, h, dh)
    r = jnp.arange(rows)
    row_start = jnp.clip(r - win_r // 2, 0, rows - win_r)
    key_rows = row_start[:, None] + jnp.arange(win_r)[None, :]
    kb = kg[:, key_rows]
    vb = vg[:, key_rows]
    col = jnp.arange(GRID_W)
    col_start = jnp.clip(col - NA_WIN_COLS // 2, 0, GRID_W - NA_WIN_COLS)
    col_ok = (col[None, :] >= col_start[:, None]) & (col[None, :] < col_start[:, None] + NA_WIN_COLS)
    row_idx = (key_rows - r[:, None] + NA_WIN_ROWS - 1)[:, None, :, None]
    col_idx = jnp.clip(col[None, :] - col[:, None] + NA_WIN_COLS - 1, 0, 2 * NA_WIN_COLS - 2)[None, :, None, :]
    bias = rpb[:, row_idx, col_idx].astype(jnp.float32)
    s_loc = jnp.einsum('brqhd,brwkhd->bhrqwk', qg, kb).astype(jnp.float32) * scale + bias
    s_loc = jnp.where(col_ok[:, None, :], s_loc, NEG_INF)
    s_ctx = jnp.einsum('brqhd,bchd->bhrqc', qg, k_ctx).astype(jnp.float32) * scale
    n_loc = win_r * GRID_W
    probs = jax.nn.softmax(jnp.concatenate([s_loc.reshape(b, h, rows, GRID_W, n_loc), s_ctx], axis=-1), axis=-1)
    probs = probs.astype(q.dtype)
    p_loc = probs[..., :n_loc].reshape(b, h, rows, GRID_W, win_r, GRID_W)
    p_ctx = probs[..., n_loc:]
    o = jnp.einsum('bhrqwk,brwkhd->brqhd', p_loc, vb) + jnp.einsum('bhrqc,bchd->brqhd', p_ctx, v_ctx)
    return o.reshape(b, s, h, dh)


def context_attention(q, k, v):
    s = jnp.einsum('bqhd,bkhd->bhqk', q, k).astype(jnp.float32) * (q.shape[-1] ** -0.5)
    p = jax.nn.softmax(s, axis=-1).astype(q.dtype)
    return jnp.einsum('bhqk,bkhd->bqhd', p, v)


def merge_branches(ys, gate_pre, w_branch, w_out):
    g = jax.nn.sigmoid(gate_pre).reshape(*gate_pre.shape[:-1], N_BRANCHES, gate_pre.shape[-1] // N_BRANCHES)
    proj = jnp.einsum('...nc,ncd->...nd', jnp.stack(ys, axis=-2), w_branch)
    return jnp.sum(g * proj, axis=-2) @ w_out


def token_mixer(h_lat, h_ctx, w_in, ret_decay, ret_norm_g, conv_w, conv_b, mlstm_gate_b,
                mlstm_norm_g, na_rpb, w_branch, w_out, ctx_out):
    b, s, _ = h_lat.shape
    n_ctx = h_ctx.shape[1]
    dt = h_lat.dtype
    lat = split_projection(h_lat @ w_in)
    ctx = split_projection(h_ctx @ w_in)
    qk_scale = HEAD_DIM ** -0.5

    cos, sin = axial_rope_tables(s)

    def retention_inputs(p, rotary):
        q, k, v = (split_heads(a, RET_HEADS) for a in p[0:3])
        if rotary:
            q, k = apply_axial_rope(q, cos, sin), apply_axial_rope(k, cos, sin)
        return to_scan(q), to_scan(k) * qk_scale, to_scan(v)

    log_gamma = jax.nn.log_sigmoid(ret_decay.astype(jnp.float32))
    ret_lat = retention_inputs(lat, True)
    ret_ctx = retention_inputs(ctx, False)
    r_lat, r_ctx = bidirectional_scan(
        functools.partial(retention_scan, log_gamma=log_gamma[0]),
        functools.partial(retention_scan, log_gamma=log_gamma[1]),
        ret_lat, ret_lat, ret_ctx, ret_ctx,
        jnp.zeros((b, RET_HEADS, HEAD_DIM, HEAD_DIM), jnp.float32), ctx_out)

    def retention_out(r, p):
        return head_group_norm(from_scan(r, dt), ret_norm_g) * jax.nn.silu(p[3])

    def mlstm_inputs(p):
        qk = short_conv(jnp.concatenate([p[4], p[5]], axis=-1), conv_w, conv_b)
        q, k = jnp.split(qk, 2, axis=-1)
        q, k, v = (to_scan(split_heads(a, MLSTM_HEADS)) for a in (q, k, p[6]))
        k = k * qk_scale
        g = p[8].astype(jnp.float32).reshape(p[8].shape[0], p[8].shape[1], 4, MLSTM_HEADS)
        g = jnp.moveaxis(g + mlstm_gate_b.astype(jnp.float32), 1, -1)
        fwd = (q, k, v, g[:, 0], jax.nn.log_sigmoid(g[:, 1]))
        bwd = (q, k, v, g[:, 2], jax.nn.log_sigmoid(g[:, 3]))
        return fwd, bwd

    m_lat_f, m_lat_b = mlstm_inputs(lat)
    m_ctx_f, m_ctx_b = mlstm_inputs(ctx)
    m0 = (jnp.zeros((b, MLSTM_HEADS, HEAD_DIM, HEAD_DIM), jnp.float32),
          jnp.zeros((b, MLSTM_HEADS, HEAD_DIM), jnp.float32),
          jnp.zeros((b, MLSTM_HEADS), jnp.float32))
    m_lat, m_ctx = bidirectional_scan(mlstm_scan, mlstm_scan, m_lat_f, m_lat_b, m_ctx_f, m_ctx_b, m0, ctx_out)

    def mlstm_out(hm, p):
        o_gate = jax.nn.sigmoid(split_heads(p[7], MLSTM_HEADS))
        return head_group_norm(from_scan(hm, dt) * o_gate, mlstm_norm_g)

    nq_l, nk_l, nv_l = (split_heads(a, NA_HEADS) for a in lat[9:12])
    nk_c, nv_c = (split_heads(a, NA_HEADS) for a in ctx[10:12])
    na_lat = neighbourhood_attention(nq_l, nk_l, nv_l, nk_c, nv_c, na_rpb).reshape(b, s, NA_WIDTH)

    y_lat = merge_branches((retention_out(r_lat, lat), mlstm_out(m_lat, lat), na_lat), lat[12], w_branch, w_out)
    if not ctx_out:
        return y_lat, None
    nq_c = split_heads(ctx[9], NA_HEADS)
    na_ctx = context_attention(nq_c, nk_c, nv_c).reshape(b, n_ctx, NA_WIDTH)
    y_ctx = merge_branches((retention_out(r_ctx, ctx), mlstm_out(m_ctx, ctx), na_ctx), ctx[12], w_branch, w_out)
    return y_lat, y_ctx


def hierarchical_moe(h, w_group, w_router, w_gate, w_up, w_down):
    n_tok, d = h.shape
    rows = jnp.arange(n_tok)
    group_logits = (h @ w_group).astype(jnp.float32)
    group = jnp.argmax(group_logits, axis=-1)
    group_w = jax.nn.softmax(group_logits, axis=-1)[rows, group]
    expert_logits = (h @ w_router).astype(jnp.float32).reshape(n_tok, N_GROUPS, EXPERTS_PER_GROUP)
    top_logit, top_idx = lax.top_k(expert_logits[rows, group], TOP_K)
    weight = group_w[:, None] * jax.nn.softmax(top_logit, axis=-1)
    expert = group[:, None] * EXPERTS_PER_GROUP + top_idx

    n_assign = n_tok * TOP_K
    e_flat = expert.reshape(n_assign)
    order = jnp.argsort(e_flat)
    e_sorted = e_flat[order]
    tok_sorted = order // TOP_K
    w_sorted = weight.reshape(n_assign)[order]
    counts = jnp.zeros((N_EXPERTS,), jnp.int32).at[e_flat].add(1)
    padded = (counts + EXPERT_BLOCK - 1) // EXPERT_BLOCK * EXPERT_BLOCK
    pad_end = jnp.cumsum(padded)
    pad_start = pad_end - padded
    start = jnp.cumsum(counts) - counts
    dest = pad_start[e_sorted] + jnp.arange(n_assign) - start[e_sorted]
    n_blocks = (n_assign + N_EXPERTS * (EXPERT_BLOCK - 1) + EXPERT_BLOCK - 1) // EXPERT_BLOCK
    xs = jnp.zeros((n_blocks * EXPERT_BLOCK, d), h.dtype).at[dest].set(h[tok_sorted])
    block_expert = jnp.minimum(
        jnp.sum(jnp.arange(n_blocks)[:, None] * EXPERT_BLOCK >= pad_end[None, :], axis=1), N_EXPERTS - 1)

    def expert_ffn(args):
        xb, e = args
        return (jax.nn.silu(xb @ w_gate[e]) * (xb @ w_up[e])) @ w_down[e]

    ys = lax.map(expert_ffn, (xs.reshape(n_blocks, EXPERT_BLOCK, d), block_expert)).reshape(-1, d)
    return jnp.zeros((n_tok, d), h.dtype).at[tok_sorted].add(ys[dest] * w_sorted[:, None].astype(h.dtype))


def setup_inputs(seed: int = 0) -> dict:
    key = jax.random.key(seed)
    ks = jax.random.split(key, 24)
    f32 = jnp.float32
    L, D = DEPTH, D_MODEL

    def normal(k, shape, std):
        return jax.random.normal(k, shape, f32) * std

    heads = jnp.arange(RET_HEADS, dtype=f32)
    ret_logit = jnp.log(2.0 ** (5.0 + heads) - 1.0)
    f_bias = jnp.linspace(3.0, 6.0, MLSTM_HEADS, dtype=f32)
    zeros_h = jnp.zeros((MLSTM_HEADS,), f32)
    gate_bias = jnp.stack([zeros_h, f_bias, zeros_h, f_bias])
    return {
        'x': normal(ks[0], (BATCH, SEQ, D), 1.0),
        'c': normal(ks[1], (BATCH, D), 1.0),
        'ctx': normal(ks[2], (BATCH, CTX_LEN, D), 1.0),
        'c_ctx': normal(ks[3], (D,), 1.0),
        'w_mod': normal(ks[4], (L, D, 6 * D), 0.5 * D ** -0.5),
        'b_mod': normal(ks[5], (L, 6 * D), 0.02),
        'norm1_g': 1.0 + normal(ks[6], (L, D), 0.02),
        'norm2_g': 1.0 + normal(ks[7], (L, D), 0.02),
        'w_in': normal(ks[8], (L, D, IN_COLS), D ** -0.5),
        'ret_decay': ret_logit + normal(ks[9], (L, 2, RET_HEADS), 0.1),
        'ret_norm_g': 1.0 + normal(ks[10], (L, RET_WIDTH), 0.02),
        'conv_w': normal(ks[11], (L, CONV_WIDTH, 2 * MLSTM_WIDTH), CONV_WIDTH ** -0.5),
        'conv_b': normal(ks[12], (L, 2 * MLSTM_WIDTH), 0.02),
        'mlstm_gate_b': gate_bias + normal(ks[13], (L, 4, MLSTM_HEADS), 0.1),
        'mlstm_norm_g': 1.0 + normal(ks[14], (L, MLSTM_WIDTH), 0.02),
        'na_rpb': normal(ks[15], (L, NA_HEADS, 2 * NA_WIN_ROWS - 1, 2 * NA_WIN_COLS - 1), 0.1),
        'w_branch': normal(ks[16], (L, N_BRANCHES, RET_WIDTH, D), RET_WIDTH ** -0.5),
        'w_out': normal(ks[17], (L, D, D), D ** -0.5),
        'w_group': normal(ks[18], (L, D, N_GROUPS), D ** -0.5),
        'w_router': normal(ks[19], (L, D, N_EXPERTS), D ** -0.5),
        'w_expert_gate': normal(ks[20], (L, N_EXPERTS, D, EXPERT_FF), D ** -0.5),
        'w_expert_up': normal(ks[21], (L, N_EXPERTS, D, EXPERT_FF), D ** -0.5),
        'w_expert_down': normal(ks[22], (L, N_EXPERTS, EXPERT_FF, D), EXPERT_FF ** -0.5),
        'final_norm_g': 1.0 + normal(ks[23], (D,), 0.02),
    }


def reference(x, c, ctx, c_ctx, w_mod, b_mod, norm1_g, norm2_g, w_in, ret_decay, ret_norm_g,
              conv_w, conv_b, mlstm_gate_b, mlstm_norm_g, na_rpb, w_branch, w_out,
              w_group, w_router, w_expert_gate, w_expert_up, w_expert_down, final_norm_g):
    b, s, d = x.shape
    n_ctx = ctx.shape[1]
    cond_lat = jax.nn.silu(c)
    cond_ctx = jax.nn.silu(c_ctx)
    xc = ctx
    for layer in range(DEPTH):
        ctx_needed = layer < DEPTH - 1
        sh1, sc1, g1, sh2, sc2, g2 = jnp.split(cond_lat @ w_mod[layer] + b_mod[layer], 6, axis=-1)
        csh1, csc1, cg1, csh2, csc2, cg2 = jnp.split(cond_ctx @ w_mod[layer] + b_mod[layer], 6, axis=-1)
        h_lat = modulate(rms_norm(x, norm1_g[layer]), sh1[:, None], sc1[:, None])
        h_ctx = modulate(rms_norm(xc, norm1_g[layer]), csh1, csc1)
        y_lat, y_ctx = token_mixer(h_lat, h_ctx, w_in[layer], ret_decay[layer], ret_norm_g[layer],
                                   conv_w[layer], conv_b[layer], mlstm_gate_b[layer], mlstm_norm_g[layer],
                                   na_rpb[layer], w_branch[layer], w_out[layer], ctx_needed)
        x = x + g1[:, None] * y_lat
        h2_lat = modulate(rms_norm(x, norm2_g[layer]), sh2[:, None], sc2[:, None]).reshape(b * s, d)
        moe_w = (w_group[layer], w_router[layer], w_expert_gate[layer], w_expert_up[layer], w_expert_down[layer])
        if ctx_needed:
            xc = xc + cg1 * y_ctx
            h2_ctx = modulate(rms_norm(xc, norm2_g[layer]), csh2, csc2).reshape(b * n_ctx, d)
            f = hierarchical_moe(jnp.concatenate([h2_lat, h2_ctx], axis=0), *moe_w)
            x = x + g2[:, None] * f[: b * s].reshape(b, s, d)
            xc = xc + cg2 * f[b * s:].reshape(b, n_ctx, d)
        else:
            f = hierarchical_moe(h2_lat, *moe_w)
            x = x + g2[:, None] * f.reshape(b, s, d)
    return rms_norm(x, final_norm_g)
```

```python
import contextlib
import numpy as np
import concourse.bass as bass
import concourse.mybir as mybir
from concourse.bass_utils import run_bass_kernel_spmd

F32 = mybir.dt.float32
BF16 = mybir.dt.bfloat16
I32 = mybir.dt.int32
AF = mybir.ActivationFunctionType
ALU = mybir.AluOpType
AX = mybir.AxisListType

P = 128
D = 1024
TL = 2048
TC = 256
T = TL + TC
NT = T // P
NTL = TL // P
DEPTH = 4
IN_COLS = 8720
NE = 32
CAP = 512
XS_ROWS = NE * CAP
EPS = 1e-6
BIG = 1e30
QK_SCALE = 128 ** -0.5
TB = [(0, 512), (512, 512), (1024, 512), (1536, 512), (2048, 256)]
ORDER_F = [16, 17] + list(range(16))
ORDER_B = [17, 16] + list(range(15, -1, -1))


class Buf:
    __slots__ = ("name", "w", "r", "excl")

    def __init__(self, name=""):
        self.name = name
        self.w = None
        self.r = {}
        self.excl = False


class Tile:
    def __init__(self, t, name=""):
        self.t = t
        self.b = Buf(name)

    def __getitem__(self, k):
        return self.t[k]


class Sched:
    EPOCH = 3500
    NDS = 24

    def __init__(self, nc):
        self.nc = nc
        self.sems = {}
        self.eng = {}
        for name in ["tensor", "vector", "scalar", "gpsimd", "sync"]:
            self.eng[name] = dict(obj=getattr(nc, name), cnt=0, ep=0, waited={}, wep={}, name=name)
            self._newsem(name, 0)
        self.dsem = [dict(sem=nc.semaphore("dq%d" % i).__enter__(), cnt=0) for i in range(self.NDS)]
        self.ndma = 0
        self.n_ins = 0
        self.trace = {n: [] for n in self.eng}

    def _newsem(self, name, ep):
        self.sems[(name, ep)] = self.nc.semaphore("s_%s_%d" % (name, ep)).__enter__()

    def _wait(self, e, tok):
        if tok[0] == "e":
            _, name, ep, cnt = tok
            if name == e["name"] and name == "tensor":
                return
            if e["wep"].get(name, -1) > ep:
                return
            key = (name, ep)
            if e["waited"].get(key, 0) >= cnt:
                return
            e["obj"].wait_ge(self.sems[key], cnt)
            self.trace[e["name"]].append(("w", key, cnt))
            e["waited"][key] = cnt
            if ep > e["wep"].get(name, -1):
                e["wep"][name] = ep
        else:
            _, i, cnt = tok
            key = ("d", i)
            if e["waited"].get(key, 0) >= cnt:
                return
            e["obj"].wait_ge(self.dsem[i]["sem"], cnt)
            self.trace[e["name"]].append(("w", key, cnt))
            e["waited"][key] = cnt

    def _deps(self, reads, writes):
        deps = []
        for b in reads:
            if b.w is not None:
                deps.append(b.w)
            if b.excl:
                deps.extend(b.r.values())
        for b in writes:
            if b.w is not None:
                deps.append(b.w)
            deps.extend(b.r.values())
        return deps

    @staticmethod
    def _bufs(xs):
        return [x.b if isinstance(x, Tile) else x for x in xs]

    def _record(self, tok, reads, writes):
        key = tok[:2] if tok[0] == "d" else ("e", tok[1])
        for b in writes:
            b.w = tok
            b.r = {}
        for b in reads:
            b.r[key] = tok

    def op(self, engname, fn, reads=(), writes=(), signal=True):
        e = self.eng[engname]
        reads = self._bufs(reads)
        writes = self._bufs(writes)
        for d in self._deps(reads, writes):
            self._wait(e, d)
        ins = fn(e["obj"])
        if signal:
            e["cnt"] += 1
            ins.then_inc(self.sems[(engname, e["ep"])], 1)
            self.trace[engname].append(("i", (engname, e["ep"]), 1))
            tok = ("e", engname, e["ep"], e["cnt"])
            if e["cnt"] >= self.EPOCH:
                e["ep"] += 1
                e["cnt"] = 0
                self._newsem(engname, e["ep"])
        else:
            tok = ("e", engname, e["ep"], e["cnt"] + 1)
        self._record(tok, reads, writes)
        self.n_ins += 1
        return ins

    def pe(self, fn, r=(), w=(), signal=True):
        return self.op("tensor", fn, r, w, signal)

    def act(self, fn, r=(), w=()):
        return self.op("scalar", fn, r, w)

    def dve(self, fn, r=(), w=()):
        return self.op("vector", fn, r, w)

    def pool(self, fn, r=(), w=()):
        return self.op("gpsimd", fn, r, w)

    def _dma_common(self, engname, issue, reads, writes):
        e = self.eng[engname]
        reads = self._bufs(reads)
        writes = self._bufs(writes)
        for d in self._deps(reads, writes):
            self._wait(e, d)
        i = self.ndma % self.NDS
        self.ndma += 1
        ds = self.dsem[i]
        if ds["cnt"] > 0:
            self._wait(e, ("d", i, ds["cnt"]))
        ins = issue(e["obj"])
        ds["cnt"] += 16
        ins.then_inc(ds["sem"], 16)
        self.trace[engname].append(("i", ("d", i), 16))
        tok = ("d", i, ds["cnt"])
        self._record(tok, reads, writes)
        self.n_ins += 1
        return ins

    def dma(self, engname, out, in_, r=(), w=()):
        return self._dma_common(engname, lambda o: o.dma_start(out=out, in_=in_), r, w)

    def idma(self, out, out_offset, in_, in_offset, bounds, r=(), w=()):
        if not hasattr(self, "_bregs"):
            self._bregs = {}
        if bounds not in self._bregs:
            self._bregs[bounds] = self.nc.gpsimd.to_reg(bounds)
        bounds = self._bregs[bounds]
        return self._dma_common(
            "gpsimd",
            lambda o: o.indirect_dma_start(out=out, out_offset=out_offset, in_=in_, in_offset=in_offset,
                                           bounds_check=bounds, oob_is_err=False), r, w)

    def simulate(self):
        vals = {}
        ptr = {n: 0 for n in self.trace}
        progress = True
        while progress:
            progress = False
            for n, tr in self.trace.items():
                while ptr[n] < len(tr):
                    kind, key, v = tr[ptr[n]]
                    if kind == "w":
                        if vals.get(key, 0) >= v:
                            ptr[n] += 1
                            progress = True
                        else:
                            break
                    else:
                        vals[key] = vals.get(key, 0) + v
                        ptr[n] += 1
                        progress = True
        stuck = {n: (ptr[n], len(tr), tr[ptr[n]] if ptr[n] < len(tr) else None) for n, tr in self.trace.items()}
        return stuck, vals

    def fence(self):
        names = list(self.eng.keys())
        toks = []
        for n in names:
            e = self.eng[n]
            if e["cnt"] > 0:
                toks.append(("e", n, e["ep"], e["cnt"]))
            elif e["ep"] > 0:
                toks.append(("e", n, e["ep"] - 1, self.EPOCH))
        dt = [("d", i, ds["cnt"]) for i, ds in enumerate(self.dsem) if ds["cnt"] > 0]
        for n in names:
            e = self.eng[n]
            for tok in toks:
                if tok[1] == n:
                    continue
                self._wait(e, tok)
            for tok in dt:
                self._wait(e, tok)


def _const_table():
    idx = np.arange(P, dtype=np.float32)
    pj = idx[:, None]
    fi = idx[None, :]
    ent = {}
    ent["ident"] = (pj == fi)
    ent["ones"] = np.ones((P, P))
    ent["trif"] = (pj <= fi)
    ent["trib"] = (pj >= fi)
    ent["tris"] = (pj < fi)
    ent["dpos"] = np.maximum(fi - pj, 0)
    ent["mpos"] = (fi >= pj)
    ent["dneg"] = np.maximum(pj - fi, 0)
    ent["mneg"] = (pj >= fi)
    ent["mij_f"] = np.where(fi <= pj, 0.0, -BIG)
    ent["mij_b"] = np.where(fi >= pj, 0.0, -BIG)
    ent["nji_f"] = np.where(pj <= fi, 0.0, BIG)
    ent["nji_b"] = np.where(pj >= fi, 0.0, BIG)
    ent["row_ip1"] = np.broadcast_to(fi + 1.0, (P, P))
    ent["row_128mi"] = np.broadcast_to(128.0 - fi, (P, P))
    ent["col_127mj"] = 127.0 - pj
    ent["col_j"] = pj + 0.0
    ent["col_128"] = np.full((P, 1), 128.0)
    ent["ecap"] = np.broadcast_to(np.arange(NE, dtype=np.float32)[None, :] * CAP, (P, NE))
    off = {}
    cols = []
    o = 0
    for k, v in ent.items():
        v = np.asarray(v, dtype=np.float32)
        off[k] = (o, v.shape[1])
        o += v.shape[1]
        cols.append(v)
    return off, np.ascontiguousarray(np.concatenate(cols, axis=1), dtype=np.float32)


COFF, CONST_NP = _const_table()
NCONST = CONST_NP.shape[1]


def _rope_tables():
    t = np.arange(TL)
    rows = (t // 64).astype(np.float32)
    colsg = (t % 64).astype(np.float32)
    inv_freq = (10000.0 ** (-np.arange(32, dtype=np.float32) / 32)).astype(np.float32)
    p = np.arange(P)
    axis = p // 64
    half = (p % 64) // 32
    f = p % 32
    pos = np.where(axis[:, None] == 0, rows[None, :], colsg[None, :]).astype(np.float32)
    ang = (pos * inv_freq[f][:, None]).astype(np.float32)
    cos = np.cos(ang)
    sin = np.sin(ang) * np.where(half == 0, -1.0, 1.0)[:, None]
    return np.ascontiguousarray(np.stack([cos, sin], axis=0), dtype=np.float32)


def _swap_perm():
    p = np.arange(P)
    axis = p // 64
    half = (p % 64) // 32
    f = p % 32
    return axis * 64 + (1 - half) * 32 + f


def _na_bias(rpb):
    L, H = rpb.shape[0], rpb.shape[1]
    qc = np.arange(64)
    kc = np.arange(64)
    cs = np.clip(qc - 8, 0, 48)
    col_ok = (kc[None, :] >= cs[:, None]) & (kc[None, :] < cs[:, None] + 16)
    cidx = np.clip(kc[None, :] - qc[:, None] + 15, 0, 30)
    out = np.full((L, P, H, 19, 64), -BIG, dtype=np.float32)

    def fill(tau, delta, valid):
        for w2 in range(2):
            if not valid[w2]:
                continue
            offr = delta + w2 + 7
            if offr < 0 or offr > 14:
                continue
            vals = rpb[:, :, offr, :][:, :, cidx]
            vals = np.where(col_ok[None, None], vals, -BIG)
            out[:, w2 * 64:(w2 + 1) * 64, :, tau, :] = np.transpose(vals, (0, 3, 1, 2))

    for delta in range(-7, 7):
        fill(delta + 7, delta, (True, True))
    fill(14, -5, (False, True))
    fill(15, -3, (True, True))
    fill(16, -1, (True, True))
    fill(17, 1, (True, True))
    fill(18, 3, (True, False))
    return out


class K:
    pass


def bc_mid(ap2d, n):
    a = [list(x) for x in ap2d.ap]
    return bass.AP(ap2d.tensor, ap2d.offset, [a[0], [0, n], a[1]])


def build(n_layers=DEPTH, debug=None, stop_after=None, wl=DEPTH):
    nc = bass.Bass("TRN2", target_bir_lowering=False)
    k = K()
    k.nc = nc
    k.debug = debug or set()
    k.stop_after = stop_after

    def din(name, shape, dt=F32):
        shape = [wl if (i == 0 and d == DEPTH and name not in ('cT',)) else d for i, d in enumerate(shape)]
        return nc.dram_tensor(name, list(shape), dt, kind="ExternalInput").ap()

    def dscr(name, shape, dt):
        kind = "ExternalOutput" if name in k.debug else "Internal"
        return nc.dram_tensor(name, list(shape), dt, kind=kind).ap()

    k.x = din("x", [TL, D])
    k.ctx = din("ctx", [TC, D])
    k.cT = din("cT", [P, 8, 2])
    k.w_mod = din("w_mod", [DEPTH, D, 6 * D])
    k.b_mod = din("b_mod", [DEPTH, 6 * D])
    k.norm1_g = din("norm1_g", [DEPTH, D])
    k.norm2_g = din("norm2_g", [DEPTH, D])
    k.w_in = din("w_in", [DEPTH, D, IN_COLS])
    k.w_swap = din("w_swap", [DEPTH, D, 1024])
    k.ret_decay = din("ret_decay", [DEPTH, 8])
    k.ret_norm_g = din("ret_norm_g", [DEPTH, 512])
    k.convT = din("convT", [DEPTH, P, 8, 5])
    k.convbT = din("convbT", [DEPTH, P, 8])
    k.gate_b = din("gate_b", [DEPTH, 16])
    k.mlstm_norm_g = din("mlstm_norm_g", [DEPTH, 512])
    k.na_bias = din("na_bias", [DEPTH, P, 4, 19, 64])
    k.w_branch = din("w_branch", [DEPTH, 3, 512, D])
    k.w_out = din("w_out", [DEPTH, D, D])
    k.w_gr = din("w_gr", [DEPTH, D, 36])
    k.w_eg = din("w_eg", [DEPTH, NE, D, 512])
    k.w_eu = din("w_eu", [DEPTH, NE, D, 512])
    k.w_ed = din("w_ed", [DEPTH, NE, 512, D])
    k.final_g = din("final_g", [1, D])
    k.consts_d = din("consts", [P, NCONST])
    k.rope_d = din("rope", [2, P, TL])
    k.out = nc.dram_tensor("out", [TL, D], F32, kind="ExternalOutput").ap()

    k.xres = dscr("xres", [T, D], F32)
    k.modd = dscr("modd", [2, 6, D], F32)
    k.YT = dscr("YT", [12, P, T], BF16)
    k.xs = dscr("xs", [XS_ROWS, D], BF16)
    k.ys = dscr("ys", [XS_ROWS + P, D], BF16)
    k.dbg_hT = dscr("dbg_hT", [P, 8, T], BF16) if "dbg_hT" in k.debug else None
    k.dbg_route = dscr("dbg_route", [P, NT, 6], F32) if "dbg_route" in k.debug else None

    S = Sched(nc)
    k.S = S
    top = contextlib.ExitStack()

    k.uid = 0

    def sb(stack, name, shape, dt):
        k.uid += 1
        nm = "sb%d_%s" % (k.uid, name)
        return Tile(stack.enter_context(nc.sbuf_tensor(nm, list(shape), dt)), nm)

    k.sb = sb
    k.banks = [Tile(top.enter_context(nc.psum_tensor("bank%d" % i, [P, 512], F32)), "bank%d" % i) for i in range(8)]
    for b_ in k.banks:
        b_.b.excl = True
    k.bank_i = 0

    def bank():
        b = k.banks[k.bank_i % 7]
        k.bank_i += 1
        return b

    k.bank = bank
    k.rbank_i = 0

    def rbank():
        b = k.banks[7]
        k.rbank_i += 1
        return b

    k.rbank = rbank

    k.C = sb(top, "consts", [P, NCONST], F32)
    k.identb = sb(top, "identb", [P, P], BF16)
    k.rope = sb(top, "rope", [P, 2, TL], BF16)
    k.condb = sb(top, "condb", [P, 8, 2], BF16)
    k.dests = sb(top, "dests", [P, NT, 2], I32)
    k.destg = sb(top, "destg", [P, NT, 2], I32)
    k.wgt = sb(top, "wgt", [P, NT, 2], F32)

    def cst(name):
        o, w = COFF[name]
        return k.C[:, o:o + w]

    k.cst = cst

    S.dma("sync", k.C[:], k.consts_d, w=[k.C])
    S.dve(lambda v: v.tensor_copy(out=k.identb[:], in_=cst("ident")), r=[k.C], w=[k.identb])
    S.dma("gpsimd", k.rope[:, 0, :], k.rope_d[0], w=[k.rope])
    S.dma("gpsimd", k.rope[:, 1, :], k.rope_d[1], w=[k.rope])
    with contextlib.ExitStack() as st:
        ct = sb(st, "ct32", [P, 8, 2], F32)
        zt = sb(st, "zt", [P, 8, D], BF16)
        S.dma("sync", ct[:], k.cT, w=[ct])
        S.act(lambda a: a.activation(out=k.condb[:], in_=ct[:], func=AF.Silu), r=[ct], w=[k.condb])
        S.dma("sync", k.xres[0:TL, :], k.x)
        S.dma("sync", k.xres[TL:T, :], k.ctx)
        S.pool(lambda g: g.memset(zt[:], 0.0), w=[zt])
        for i in range(XS_ROWS // 1024):
            S.dma("sync" if i % 2 == 0 else "scalar",
                  k.xs[i * 1024:(i + 1) * 1024, :].rearrange("(p a) d -> p a d", p=P), zt[:], r=[zt])
        S.dma("sync", k.ys[XS_ROWS:XS_ROWS + P, :], zt[:, 0, :], r=[zt])
        S.fence()

    for l in range(n_layers):
        layer(k, l, last=(l == DEPTH - 1), final=(l == n_layers - 1))

    S.fence()
    top.close()
    k.nc_S = S
    nc._mk_sched = S
    return nc


def rstd_from_ss(k, ss, tmp1, rstd, scale, eps):
    S = k.S
    S.dve(lambda v: v.tensor_scalar(out=tmp1[:], in0=ss[:], scalar1=scale, scalar2=eps, op0=ALU.mult, op1=ALU.add),
          r=[ss], w=[tmp1])
    S.act(lambda a: a.activation(out=tmp1[:], in_=tmp1[:], func=AF.Sqrt), r=[tmp1], w=[tmp1])
    S.dve(lambda v: v.reciprocal(out=rstd[:], in_=tmp1[:]), r=[tmp1], w=[rstd])


def load_row_bc(k, tile, src_row_ap, eng="sync"):
    k.S.dma(eng, tile[:], src_row_ap.partition_broadcast(P), w=[tile])


def load_w(k, dst, dst_sl, src2d, col0, n):
    k.S.dma("gpsimd", dst[:, :, dst_sl], src2d.rearrange("(c p) n -> p c n", p=P)[:, :, col0:col0 + n], w=[dst])


def proj_fm(k, ps_ap, psb, wt, wcol0, tok0, ntok, hT):
    for kc in range(8):
        k.S.pe(lambda t: t.matmul(ps_ap, lhsT=wt[:, kc, wcol0:wcol0 + P], rhs=hT[:, kc, tok0:tok0 + ntok],
                                  start=(kc == 0), stop=(kc == 7)), r=[wt, hT], w=[psb], signal=(kc == 7))


def proj_tm(k, ps_ap, psb, wt, wcol0, n, t, hT, last=True):
    for kc in range(8):
        k.S.pe(lambda te: te.matmul(ps_ap, lhsT=hT[:, kc, t * P:(t + 1) * P], rhs=wt[:, kc, wcol0:wcol0 + n],
                                    start=(kc == 0), stop=(kc == 7)), r=[wt, hT], w=[psb], signal=(kc == 7 and last))


def group_norm_out(k, st, src_ap, src_bufs, grow_ap, grow_b, mul_ap, mul_b, yb):
    S = k.S
    S.dve(lambda v: v.bn_stats(out=st["bst"][:], in_=src_ap), r=src_bufs, w=[st["bst"]])
    S.dve(lambda v: v.bn_aggr(out=st["mv"][:], in_=st["bst"][:]), r=[st["bst"]], w=[st["mv"]])
    S.dve(lambda v: v.tensor_scalar(out=st["t1"][:], in0=st["mv"][:, 1:2], scalar1=EPS, scalar2=None, op0=ALU.add),
          r=[st["mv"]], w=[st["t1"]])
    S.act(lambda a: a.activation(out=st["t1"][:], in_=st["t1"][:], func=AF.Sqrt), r=[st["t1"]], w=[st["t1"]])
    S.dve(lambda v: v.reciprocal(out=st["rs"][:], in_=st["t1"][:]), r=[st["t1"]], w=[st["rs"]])
    S.dve(lambda v: v.tensor_scalar(out=st["y"][:], in0=src_ap, scalar1=st["mv"][:, 0:1], scalar2=st["rs"][:, 0:1],
                                    op0=ALU.subtract, op1=ALU.mult), r=src_bufs + [st["mv"], st["rs"]], w=[st["y"]])
    if mul_ap is None:
        S.pool(lambda g: g.tensor_tensor(out=yb[:], in0=st["y"][:], in1=grow_ap, op=ALU.mult),
               r=[st["y"], grow_b], w=[yb])
    else:
        S.pool(lambda g: g.tensor_tensor(out=st["y"][:], in0=st["y"][:], in1=grow_ap, op=ALU.mult),
               r=[st["y"], grow_b], w=[st["y"]])
        S.pool(lambda g: g.tensor_tensor(out=yb[:], in0=st["y"][:], in1=mul_ap, op=ALU.mult),
               r=[st["y"], mul_b], w=[yb])


def bulk_norm_out(k, OB, clist, BST, MV, RSV, grow_ap, grow_b, mulT, ynb, yb, YTs, width=P):
    S = k.S
    for c in clist:
        S.dve(lambda v: v.bn_stats(out=BST[:, c, :], in_=OB[:, c, 0:width]), r=[OB], w=[BST])
    for c in clist:
        S.dve(lambda v: v.bn_aggr(out=MV[:, c, :], in_=BST[:, c, :]), r=[BST], w=[MV])
    S.dve(lambda v: v.tensor_scalar(out=RSV[:], in0=MV[:, :, 1], scalar1=EPS, scalar2=None, op0=ALU.add), r=[MV], w=[RSV])
    S.act(lambda a: a.activation(out=RSV[:], in_=RSV[:], func=AF.Sqrt), r=[RSV], w=[RSV])
    S.dve(lambda v: v.reciprocal(out=RSV[:], in_=RSV[:]), r=[RSV], w=[RSV])
    tro = TransOut(k, YTs)
    for i, c in enumerate(clist):
        y_, yb_ = ynb[i % 2], yb[i % 2]
        S.dve(lambda v: v.tensor_scalar(out=y_[:], in0=OB[:, c, 0:width], scalar1=MV[:, c, 0:1], scalar2=RSV[:, c:c + 1],
                                        op0=ALU.subtract, op1=ALU.mult), r=[OB, MV, RSV], w=[y_])
        if mulT is None:
            S.pool(lambda g: g.tensor_tensor(out=yb_[:], in0=y_[:], in1=grow_ap, op=ALU.mult), r=[y_, grow_b], w=[yb_])
        else:
            S.pool(lambda g: g.tensor_tensor(out=y_[:], in0=y_[:], in1=grow_ap, op=ALU.mult), r=[y_, grow_b], w=[y_])
            S.pool(lambda g: g.tensor_tensor(out=yb_[:], in0=y_[:], in1=mulT[:, c, :], op=ALU.mult), r=[y_, mulT], w=[yb_])
        tro.add(yb_, c)
        yield
    tro.flush()


def run_interleaved(gens):
    alive = list(gens)
    while alive:
        for g in list(alive):
            try:
                next(g)
            except StopIteration:
                alive.remove(g)


class TransOut:
    def __init__(self, k, dst):
        self.k = k
        self.dst = dst
        self.items = []
        self.bank = None

    def add(self, yb, c):
        k = self.k
        if self.bank is None:
            self.bank = k.rbank()
        i = len(self.items)
        pv = self.bank[:].bitcast(BF16)[:, 0:512].rearrange("p (a b) -> p a b", a=4)
        k.S.pe(lambda t: t.transpose(out=pv[:, i, :], in_=yb[:], identity=k.identb[:]), r=[yb, k.identb], w=[self.bank])
        self.items.append(c)
        if len(self.items) == 4 or c in (17, 15):
            self.flush()

    def flush(self):
        k = self.k
        if not self.items:
            return
        n = len(self.items)
        c0 = self.items[0]
        assert self.items == list(range(c0, c0 + n)), self.items
        pv = self.bank[:].bitcast(BF16)[:, 0:n * P]
        bnk = self.bank
        k.S.act(lambda a: a.activation(out=self.dst[:, c0 * P:(c0 + n) * P], in_=pv, func=AF.Copy), r=[bnk], w=[self.dst])
        self.items = []
        self.bank = None


def layer(k, l, last, final):
    nc, S, sb, cst, bank = k.nc, k.S, k.sb, k.cst, k.bank
    ctx_out = not last
    n_tiles_out = NT if ctx_out else NTL

    with contextlib.ExitStack() as st:
        wm = [sb(st, "wm%d" % i, [P, 8, 768], BF16) for i in range(2)]
        brow = sb(st, "brow", [2, 6 * D], F32)
        modrow = sb(st, "modrow", [2, 6 * D], F32)
        g1row = sb(st, "g1row", [2, D], F32)
        g2row = sb(st, "g2row", [2, D], F32)
        drow = sb(st, "drow", [2, 6, D], F32)
        S.dma("sync", brow[:], k.b_mod[l:l + 1, :].partition_broadcast(2), w=[brow])
        S.dma("sync", g1row[:], k.norm1_g[l:l + 1, :].partition_broadcast(2), w=[g1row])
        S.dma("sync", g2row[:], k.norm2_g[l:l + 1, :].partition_broadcast(2), w=[g2row])
        wsrc = k.w_mod[l].rearrange("(c p) n -> p c n", p=P)
        for blk in range(8):
            w = wm[blk % 2]
            S.dma("gpsimd", w[:], wsrc[:, :, blk * 768:(blk + 1) * 768], w=[w])
            for half in range(2):
                b = bank()
                c0 = blk * 768 + half * 384
                for kc in range(8):
                    S.pe(lambda t: t.matmul(b[0:2, 0:384], lhsT=k.condb[:, kc, :], rhs=w[:, kc, half * 384:(half + 1) * 384],
                                            start=(kc == 0), stop=(kc == 7)), r=[k.condb, w], w=[b], signal=(kc == 7))
                S.dve(lambda v: v.tensor_tensor(out=modrow[:, c0:c0 + 384], in0=b[0:2, 0:384], in1=brow[:, c0:c0 + 384],
                                                op=ALU.add), r=[b, brow], w=[modrow])
        S.dve(lambda v: v.scalar_tensor_tensor(out=drow[:, 0, :], in0=modrow[:, D:2 * D], scalar=1.0, in1=g1row[:],
                                               op0=ALU.add, op1=ALU.mult), r=[modrow, g1row], w=[drow])
        S.dve(lambda v: v.tensor_copy(out=drow[:, 1, :], in_=modrow[:, 0:D]), r=[modrow], w=[drow])
        S.dve(lambda v: v.tensor_copy(out=drow[:, 2, :], in_=modrow[:, 2 * D:3 * D]), r=[modrow], w=[drow])
        S.dve(lambda v: v.scalar_tensor_tensor(out=drow[:, 3, :], in0=modrow[:, 4 * D:5 * D], scalar=1.0, in1=g2row[:],
                                               op0=ALU.add, op1=ALU.mult), r=[modrow, g2row], w=[drow])
        S.dve(lambda v: v.tensor_copy(out=drow[:, 4, :], in_=modrow[:, 3 * D:4 * D]), r=[modrow], w=[drow])
        S.dve(lambda v: v.tensor_copy(out=drow[:, 5, :], in_=modrow[:, 5 * D:6 * D]), r=[modrow], w=[drow])
        S.dma("sync", k.modd, drow[:], r=[drow])
        S.fence()

    def mod_row(which, j):
        return k.modd[which:which + 1, j, :]

    with contextlib.ExitStack() as st_h:
        hT = sb(st_h, "hT", [P, 8, T], BF16)
        with contextlib.ExitStack() as st:
            rows = {}
            for which in range(2):
                for j, nm in ((0, "A"), (1, "B")):
                    rows[(which, nm)] = sb(st, "row%s%d" % (nm, which), [P, D], F32)
                    load_row_bc(k, rows[(which, nm)], mod_row(which, j))
            xt = [sb(st, "xt%d" % i, [P, D], F32) for i in range(2)]
            tmp2 = [sb(st, "ntmp%d" % i, [P, D], F32) for i in range(2)]
            hb = [sb(st, "hb%d" % i, [P, D], BF16) for i in range(2)]
            ss2 = [sb(st, "ss%d" % i, [P, 1], F32) for i in range(2)]
            t12 = [sb(st, "nt1%d" % i, [P, 1], F32) for i in range(2)]
            rstd2 = [sb(st, "rstd%d" % i, [P, 1], F32) for i in range(2)]
            for t in range(NT):
                which = 0 if t < NTL else 1
                x_, h_ = xt[t % 2], hb[t % 2]
                tmp, ss, t1, rstd = tmp2[t % 2], ss2[t % 2], t12[t % 2], rstd2[t % 2]
                S.dma("sync", x_[:], k.xres[t * P:(t + 1) * P, :], w=[x_])
                S.act(lambda a: a.activation(out=tmp[:], in_=x_[:], func=AF.Square, accum_out=ss[:]), r=[x_], w=[tmp, ss])
                rstd_from_ss(k, ss, t1, rstd, 1.0 / D, EPS)
                A, B = rows[(which, "A")], rows[(which, "B")]
                S.dve(lambda v: v.scalar_tensor_tensor(out=tmp[:], in0=x_[:], scalar=rstd[:, 0:1], in1=A[:],
                                                       op0=ALU.mult, op1=ALU.mult), r=[x_, rstd, A], w=[tmp])
                S.dve(lambda v: v.tensor_tensor(out=h_[:], in0=tmp[:], in1=B[:], op=ALU.add), r=[tmp, B], w=[h_])
                b = bank()
                pv = b[:].bitcast(BF16).rearrange("p (a b) -> p a b", a=8)
                for c in range(8):
                    S.pe(lambda te: te.transpose(out=pv[:, c, :], in_=h_[:, c * P:(c + 1) * P], identity=k.identb[:]),
                         r=[h_, k.identb], w=[b], signal=(c == 7))
                S.act(lambda a: a.activation(out=hT[:, :, t * P:(t + 1) * P], in_=pv, func=AF.Copy), r=[b], w=[hT])
            if k.dbg_hT is not None:
                S.dma("sync", k.dbg_hT, hT[:], r=[hT])
            S.fence()

        if k.stop_after == "norm1":
            st_h.close()
            return
        if "skip_ret" not in k.debug:
            retention(k, l, hT, ctx_out)
            S.fence()
        if k.stop_after == "ret":
            st_h.close()
            return
        if "skip_ml" not in k.debug:
            mlstm(k, l, hT, ctx_out)
            S.fence()
        if k.stop_after == "ml":
            st_h.close()
            return
        if "skip_na" not in k.debug:
            natten(k, l, hT, ctx_out)
            S.fence()
        if k.stop_after == "na":
            st_h.close()
            return

        with contextlib.ExitStack() as st_m:
            mergedT = sb(st_m, "mergedT", [P, 8, T], BF16)
            merge1(k, l, hT, mergedT, ctx_out)
            S.fence()
            merge2(k, l, mergedT, n_tiles_out, mod_row)
    if k.stop_after == "merge2":
        return
    if "skip_moe" not in k.debug:
        moe(k, l)
        S.fence()
    if k.stop_after == "moe":
        return
    combine(k, l, n_tiles_out, mod_row, final)
    S.fence()


def retention(k, l, hT, ctx_out):
    nc, S, sb, cst, bank = k.nc, k.S, k.sb, k.cst, k.bank
    with contextlib.ExitStack() as st:
        rd = sb(st, "rd", [P, 8], F32)
        lg = sb(st, "lg", [P, 8], F32)
        RT = sb(st, "RT", [P, 4, 3, P], F32)
        RC = sb(st, "RC", [P, 4, 4], F32)
        rt1 = sb(st, "rt1", [P, P], F32)
        rt2 = sb(st, "rt2", [P, P], F32)
        rng = sb(st, "rng", [P, 512], F32)
        load_row_bc(k, rd, k.ret_decay[l:l + 1, :])
        load_row_bc(k, rng, k.ret_norm_g[l:l + 1, :])
        S.act(lambda a: a.activation(out=lg[:], in_=rd[:], func=AF.Exp, scale=-1.0), r=[rd], w=[lg])
        S.act(lambda a: a.activation(out=lg[:], in_=lg[:], func=AF.Ln, bias=1.0), r=[lg], w=[lg])
        S.dve(lambda v: v.tensor_scalar(out=lg[:], in0=lg[:], scalar1=-1.0, scalar2=None, op0=ALU.mult), r=[lg], w=[lg])
        for h in range(4):
            lf, lb = lg[:, h:h + 1], lg[:, 4 + h:5 + h]
            S.act(lambda a: a.activation(out=rt1[:], in_=cst("dpos"), func=AF.Exp, scale=lf), r=[k.C, lg], w=[rt1])
            S.dve(lambda v: v.tensor_tensor(out=rt1[:], in0=rt1[:], in1=cst("mpos"), op=ALU.mult), r=[rt1, k.C], w=[rt1])
            S.act(lambda a: a.activation(out=rt2[:], in_=cst("dneg"), func=AF.Exp, scale=lb), r=[k.C, lg], w=[rt2])
            S.dve(lambda v: v.tensor_tensor(out=rt2[:], in0=rt2[:], in1=cst("mneg"), op=ALU.mult), r=[rt2, k.C], w=[rt2])
            S.dve(lambda v: v.tensor_tensor(out=RT[:, h, 0, :], in0=rt1[:], in1=rt2[:], op=ALU.add), r=[rt1, rt2], w=[RT])
            S.act(lambda a: a.activation(out=RT[:, h, 1, :], in_=cst("row_ip1"), func=AF.Exp, scale=lf), r=[k.C, lg], w=[RT])
            S.act(lambda a: a.activation(out=RT[:, h, 2, :], in_=cst("row_128mi"), func=AF.Exp, scale=lb), r=[k.C, lg], w=[RT])
            S.act(lambda a: a.activation(out=RC[:, h, 0:1], in_=cst("col_127mj"), func=AF.Exp, scale=lf), r=[k.C, lg], w=[RC])
            S.act(lambda a: a.activation(out=RC[:, h, 1:2], in_=cst("col_j"), func=AF.Exp, scale=lb), r=[k.C, lg], w=[RC])
            S.act(lambda a: a.activation(out=RC[:, h, 2:3], in_=cst("col_128"), func=AF.Exp, scale=lf), r=[k.C, lg], w=[RC])
            S.act(lambda a: a.activation(out=RC[:, h, 3:4], in_=cst("col_128"), func=AF.Exp, scale=lb), r=[k.C, lg], w=[RC])

        W6 = [sb(st, "W6_%d" % i, [P, 8, 6 * P], BF16) for i in range(2)]
        qT2 = [sb(st, "r_qT%d" % i, [P, T], BF16) for i in range(2)]
        kT2 = [sb(st, "r_kT%d" % i, [P, T], BF16) for i in range(2)]
        kTM2 = [sb(st, "r_kTM%d" % i, [P, NT, P], BF16) for i in range(2)]
        vTM2 = [sb(st, "r_vTM%d" % i, [P, NT, P], BF16) for i in range(2)]
        gTM2 = [sb(st, "r_gTM%d" % i, [P, NT, P], BF16) for i in range(2)]
        SbAll = sb(st, "r_SbAll", [P, NT, P], BF16)
        YTs = sb(st, "r_YTs", [P, T], BF16)
        ra = sb(st, "r_ra", [P, 512], F32)
        rb = sb(st, "r_rb", [P, 512], F32)
        S32 = sb(st, "r_S32", [P, P], F32)
        Sf = [sb(st, "r_Sf%d" % i, [P, P], BF16) for i in range(2)]
        ks = [sb(st, "r_ks%d" % i, [P, P], BF16) for i in range(2)]
        PT3 = [sb(st, "r_PT%d" % i, [P, P], BF16) for i in range(3)]
        qf3 = [sb(st, "r_qf%d" % i, [P, P], BF16) for i in range(3)]
        qb3 = [sb(st, "r_qb%d" % i, [P, P], BF16) for i in range(3)]
        yb = [sb(st, "r_yb%d" % i, [P, P], BF16) for i in range(2)]
        ynb = [sb(st, "r_yn%d" % i, [P, P], F32) for i in range(2)]
        OB = sb(st, "r_OB", [P, NT, P], F32)
        BST = sb(st, "r_BST", [P, NT, 6], F32)
        MV = sb(st, "r_MV", [P, NT, 2], F32)
        RSV = sb(st, "r_RSV", [P, NT], F32)

        def load_head(h):
            w = W6[h % 2]
            wi = k.w_in[l]
            ws = k.w_swap[l]
            load_w(k, w, slice(0, P), wi, h * P, P)
            load_w(k, w, slice(P, 2 * P), ws, h * P, P)
            load_w(k, w, slice(2 * P, 3 * P), wi, 512 + h * P, P)
            load_w(k, w, slice(3 * P, 4 * P), ws, 512 + h * P, P)
            load_w(k, w, slice(4 * P, 5 * P), wi, 1024 + h * P, P)
            load_w(k, w, slice(5 * P, 6 * P), wi, 1536 + h * P, P)

        def proj(h):
            w = W6[h % 2]
            qT, kT, kTM, vTM, gTM = qT2[h % 2], kT2[h % 2], kTM2[h % 2], vTM2[h % 2], gTM2[h % 2]
            for (dst, c0, sc) in ((qT, 0, 1.0), (kT, 2 * P, QK_SCALE)):
                for (tok0, n) in TB:
                    bA = bank()
                    proj_fm(k, bA[:, 0:n], bA, w, c0, tok0, n, hT)
                    if tok0 < TL:
                        bB = bank()
                        proj_fm(k, bB[:, 0:n], bB, w, c0 + P, tok0, n, hT)
                        S.dve(lambda v: v.scalar_tensor_tensor(out=ra[:, 0:n], in0=bA[:, 0:n], scalar=sc,
                                                               in1=k.rope[:, 0, tok0:tok0 + n], op0=ALU.mult, op1=ALU.mult),
                              r=[bA, k.rope], w=[ra])
                        S.dve(lambda v: v.scalar_tensor_tensor(out=rb[:, 0:n], in0=bB[:, 0:n], scalar=sc,
                                                               in1=k.rope[:, 1, tok0:tok0 + n], op0=ALU.mult, op1=ALU.mult),
                              r=[bB, k.rope], w=[rb])
                        S.pool(lambda g: g.tensor_tensor(out=dst[:, tok0:tok0 + n], in0=ra[:, 0:n], in1=rb[:, 0:n], op=ALU.add),
                               r=[ra, rb], w=[dst])
                    else:
                        S.act(lambda a: a.activation(out=dst[:, tok0:tok0 + n], in_=bA[:, 0:n], func=AF.Copy, scale=sc),
                              r=[bA], w=[dst])
                    yield
            for c0 in range(0, NT, 4):
                n = min(4, NT - c0)
                b = bank()
                pv = b[:].bitcast(BF16)[:, 0:512].rearrange("p (a b) -> p a b", a=4)
                for i in range(n):
                    c = c0 + i
                    S.pe(lambda t: t.transpose(out=pv[:, i, :], in_=kT[:, c * P:(c + 1) * P], identity=k.identb[:]),
                         r=[kT, k.identb], w=[b], signal=(i == n - 1))
                S.dve(lambda v: v.tensor_copy(out=kTM[:, c0:c0 + n, :], in_=pv[:, 0:n, :]), r=[b], w=[kTM])
                yield
            for c0 in range(0, NT, 2):
                bv = bank()
                pvv = bv[:].rearrange("p (a b) -> p a b", a=2)
                for i in range(2):
                    proj_tm(k, pvv[:, i, :], bv, w, 4 * P, 2 * P, c0 + i, hT, last=(i == 1))
                S.dve(lambda v: v.tensor_copy(out=vTM[:, c0:c0 + 2, :], in_=pvv[:, :, 0:P]), r=[bv], w=[vTM])
                S.act(lambda a: a.activation(out=gTM[:, c0:c0 + 2, :], in_=pvv[:, :, P:2 * P], func=AF.Silu), r=[bv], w=[gTM])
                yield

        def scan(h):
            qT, kT, kTM, vTM, gTM = qT2[h % 2], kT2[h % 2], kTM2[h % 2], vTM2[h % 2], gTM2[h % 2]
            S.dve(lambda v: v.memset(S32[:], 0.0), w=[S32])
            for si, c in enumerate(ORDER_B):
                S.act(lambda a: a.activation(out=SbAll[:, c, :], in_=S32[:], func=AF.Copy), r=[S32], w=[SbAll])
                if si == NT - 1:
                    break
                kk = ks[si % 2]
                S.act(lambda a: a.activation(out=kk[:], in_=kTM[:, c, :], func=AF.Identity, scale=RC[:, h, 1:2]), r=[kTM, RC], w=[kk])
                b = bank()
                S.pe(lambda t: t.matmul(b[:, 0:P], lhsT=kk[:], rhs=vTM[:, c, :], start=True, stop=True), r=[kk, vTM], w=[b])
                S.dve(lambda v: v.scalar_tensor_tensor(out=S32[:], in0=S32[:], scalar=RC[:, h, 3:4], in1=b[:, 0:P],
                                                       op0=ALU.mult, op1=ALU.add), r=[S32, RC, b], w=[S32])
                yield
            S.dve(lambda v: v.memset(S32[:], 0.0), w=[S32])
            S.dve(lambda v: v.memset(Sf[0][:], 0.0), w=[Sf[0]])
            GR = 3
            for g0 in range(0, NT, GR):
                steps = list(range(g0, g0 + GR))
                outs = [(si, ORDER_F[si]) for si in steps if (ORDER_F[si] < NTL or ctx_out)]
                bSs = {}
                for si, c in outs:
                    cs = slice(c * P, (c + 1) * P)
                    bSs[si] = bank()
                    S.pe(lambda t: t.matmul(bSs[si][:, 0:P], lhsT=kT[:, cs], rhs=qT[:, cs], start=True, stop=True), r=[kT, qT], w=[bSs[si]])
                for si, c in outs:
                    pt = PT3[si % GR]
                    S.dve(lambda v: v.tensor_tensor(out=pt[:], in0=bSs[si][:, 0:P], in1=RT[:, h, 0, :], op=ALU.mult), r=[bSs[si], RT], w=[pt])
                for si, c in outs:
                    cs = slice(c * P, (c + 1) * P)
                    qf_, qb_ = qf3[si % GR], qb3[si % GR]
                    S.pool(lambda g: g.tensor_tensor(out=qf_[:], in0=qT[:, cs], in1=RT[:, h, 1, :], op=ALU.mult), r=[qT, RT], w=[qf_])
                    S.pool(lambda g: g.tensor_tensor(out=qb_[:], in0=qT[:, cs], in1=RT[:, h, 2, :], op=ALU.mult), r=[qT, RT], w=[qb_])
                yield
                for si in steps:
                    c = ORDER_F[si]
                    sfc = Sf[si % 2]
                    if c < NTL or ctx_out:
                        pt, qf_, qb_ = PT3[si % GR], qf3[si % GR], qb3[si % GR]
                        bO = bank()
                        S.pe(lambda t: t.matmul(bO[:, 0:P], lhsT=pt[:], rhs=vTM[:, c, :], start=True, stop=False), r=[pt, vTM], w=[bO], signal=False)
                        S.pe(lambda t: t.matmul(bO[:, 0:P], lhsT=qf_[:], rhs=sfc[:], start=False, stop=False), r=[qf_, sfc], w=[bO], signal=False)
                        S.pe(lambda t: t.matmul(bO[:, 0:P], lhsT=qb_[:], rhs=SbAll[:, c, :], start=False, stop=True), r=[qb_, SbAll], w=[bO])
                        S.act(lambda a: a.activation(out=OB[:, c, :], in_=bO[:, 0:P], func=AF.Copy), r=[bO], w=[OB])
                    if si == NT - 1:
                        break
                    kk = ks[si % 2]
                    S.act(lambda a: a.activation(out=kk[:], in_=kTM[:, c, :], func=AF.Identity, scale=RC[:, h, 0:1]), r=[kTM, RC], w=[kk])
                    b = bank()
                    S.pe(lambda t: t.matmul(b[:, 0:P], lhsT=kk[:], rhs=vTM[:, c, :], start=True, stop=True), r=[kk, vTM], w=[b])
                    S.dve(lambda v: v.scalar_tensor_tensor(out=S32[:], in0=S32[:], scalar=RC[:, h, 2:3], in1=b[:, 0:P],
                                                           op0=ALU.mult, op1=ALU.add), r=[S32, RC, b], w=[S32])
                    sfn = Sf[(si + 1) % 2]
                    S.act(lambda a: a.activation(out=sfn[:], in_=S32[:], func=AF.Copy), r=[S32], w=[sfn])
                    yield
            clist = ([16, 17] if ctx_out else []) + list(range(NTL))
            yield from bulk_norm_out(k, OB, clist, BST, MV, RSV, rng[:, h * P:(h + 1) * P], rng, gTM, ynb, yb, YTs)
            ncols = T if ctx_out else TL
            S.dma("sync", k.YT[h, :, 0:ncols], YTs[:, 0:ncols], r=[YTs])

        load_head(0)
        load_head(1)
        for _ in proj(0):
            pass
        for h in range(4):
            if h + 2 < 4:
                load_head(h + 2)
            run_interleaved([scan(h)] + ([proj(h + 1)] if h + 1 < 4 else []))


def bc_last(ap2d, n):
    a = [list(x) for x in ap2d.ap]
    return bass.AP(ap2d.tensor, ap2d.offset, [a[0], a[1], [0, n]])


def mlstm(k, l, hT, ctx_out):
    nc, S, sb, cst, bank = k.nc, k.S, k.sb, k.cst, k.bank
    with contextlib.ExitStack() as st:
        wg = sb(st, "m_wg", [P, 8, 16], BF16)
        gb = sb(st, "m_gb", [P, 16], F32)
        G = sb(st, "m_G", [P, NT, 16], F32)
        FB = sb(st, "m_FB", [P, NT, 8], F32)
        CUM = sb(st, "m_CUM", [P, NT, 16], F32)
        A = sb(st, "m_A", [P, NT, 8], F32)
        mng = sb(st, "m_mng", [P, 512], F32)
        cw = sb(st, "m_cw", [P, 8, 5], F32)
        cb = sb(st, "m_cb", [P, 8], F32)
        load_w(k, wg, slice(0, 16), k.w_in[l], 4096, 16)
        load_row_bc(k, gb, k.gate_b[l:l + 1, :])
        load_row_bc(k, mng, k.mlstm_norm_g[l:l + 1, :])
        S.dma("sync", cw[:], k.convT[l], w=[cw])
        S.dma("sync", cb[:], k.convbT[l], w=[cb])
        b = bank()
        pv = b[:, 0:NT * 16].rearrange("p (a b) -> p a b", a=NT)
        for t in range(NT):
            proj_tm(k, pv[:, t, :], b, wg, 0, 16, t, hT, last=(t == NT - 1))
        S.dve(lambda v: v.tensor_tensor(out=G[:], in0=pv, in1=bc_mid(gb[:], NT), op=ALU.add), r=[b, gb], w=[G])
        G4 = G[:].rearrange("p t (a b) -> p t a b", a=2)
        FB4 = FB[:].rearrange("p t (a b) -> p t a b", a=2)
        A4 = A[:].rearrange("p t (a b) -> p t a b", a=2)
        S.act(lambda a: a.activation(out=FB4, in_=G4[:, :, :, 4:8], func=AF.Exp, scale=-1.0), r=[G], w=[FB])
        S.act(lambda a: a.activation(out=FB[:], in_=FB[:], func=AF.Ln, bias=1.0), r=[FB], w=[FB])
        S.dve(lambda v: v.tensor_scalar(out=FB[:], in0=FB[:], scalar1=-1.0, scalar2=None, op0=ALU.mult), r=[FB], w=[FB])
        b = bank()
        pv = b[:, 0:NT * 16].rearrange("p (a b) -> p a b", a=NT)
        for c in range(NT):
            S.pe(lambda t: t.matmul(pv[:, c, 0:4], lhsT=cst("trif"), rhs=FB[:, c, 0:4], start=True, stop=True), r=[k.C, FB], w=[b], signal=False)
            S.pe(lambda t: t.matmul(pv[:, c, 4:8], lhsT=cst("trib"), rhs=FB[:, c, 4:8], start=True, stop=True), r=[k.C, FB], w=[b], signal=False)
            S.pe(lambda t: t.matmul(pv[:, c, 8:16], lhsT=cst("ones"), rhs=FB[:, c, 0:8], start=True, stop=True), r=[k.C, FB], w=[b],
                 signal=(c == NT - 1))
        S.dve(lambda v: v.tensor_copy(out=CUM[:], in_=pv), r=[b], w=[CUM])
        CUM4 = CUM[:, :, 0:8].rearrange("p t (a b) -> p t a b", a=2)
        S.dve(lambda v: v.tensor_tensor(out=A4, in0=G4[:, :, :, 0:4], in1=CUM4, op=ALU.subtract), r=[G, CUM], w=[A])

        names = ["AMAX", "MRAW", "MPREV", "ML", "PP", "FL", "KW", "PW", "U", "T0"]
        Q = {n: sb(st, "m_" + n, [P, NT, 8], F32) for n in names}
        dg4 = [sb(st, "m_dg4_%d" % i, [P, 4, P], F32) for i in range(2)]
        tm4 = [sb(st, "m_tm4_%d" % i, [P, 4, P], F32) for i in range(2)]
        it = 0
        for c in range(NT):
            for r in range(2):
                d4, t4 = dg4[it % 2], tm4[it % 2]
                it += 1
                acols = A[:, c, r * 4:(r + 1) * 4]
                S.dve(lambda v: v.tensor_tensor(out=d4[:], in0=bc_mid(cst("ident"), 4), in1=bc_last(acols, P), op=ALU.mult),
                      r=[k.C, A], w=[d4])
                bA = bank()
                S.pe(lambda t: t.matmul(bA[:, :], lhsT=cst("ones"), rhs=d4[:].rearrange("p a b -> p (a b)"), start=True, stop=True),
                     r=[k.C, d4], w=[bA])
                bA4 = bA[:].rearrange("p (a b) -> p a b", a=4)
                S.dve(lambda v: v.tensor_reduce(out=Q["AMAX"][:, c, r * 4:(r + 1) * 4], in_=bA4, axis=AX.X, op=ALU.max), r=[bA], w=[Q["AMAX"]])
                mij = cst("mij_f" if r == 0 else "mij_b")
                S.dve(lambda v: v.tensor_tensor(out=t4[:], in0=bA4, in1=bc_mid(mij, 4), op=ALU.add), r=[bA, k.C], w=[t4])
                S.dve(lambda v: v.tensor_reduce(out=Q["MRAW"][:, c, r * 4:(r + 1) * 4], in_=t4[:], axis=AX.X, op=ALU.max), r=[t4], w=[Q["MRAW"]])
        S.dve(lambda v: v.memset(Q["MPREV"][:], 0.0), w=[Q["MPREV"]])
        for si in range(NT):
            for r in range(2):
                order = ORDER_F if r == 0 else ORDER_B
                c = order[si]
                cols = slice(r * 4, (r + 1) * 4)
                S.dve(lambda v: v.tensor_tensor(out=Q["ML"][:, c, cols], in0=Q["AMAX"][:, c, cols], in1=Q["MPREV"][:, c, cols], op=ALU.max),
                      r=[Q["AMAX"], Q["MPREV"]], w=[Q["ML"]])
                if si + 1 < NT:
                    cn = order[si + 1]
                    S.dve(lambda v: v.tensor_tensor(out=Q["MPREV"][:, cn, cols], in0=CUM[:, c, 8 + r * 4:12 + r * 4], in1=Q["ML"][:, c, cols], op=ALU.add),
                          r=[CUM, Q["ML"]], w=[Q["MPREV"]])
        T0 = Q["T0"]
        S.dve(lambda v: v.tensor_tensor(out=T0[:], in0=Q["MRAW"][:], in1=Q["MPREV"][:], op=ALU.subtract), r=[Q["MRAW"], Q["MPREV"]], w=[T0])
        S.dve(lambda v: v.tensor_scalar(out=T0[:], in0=T0[:], scalar1=0.0, scalar2=None, op0=ALU.max), r=[T0], w=[T0])
        S.act(lambda a: a.activation(out=Q["PP"][:], in_=T0[:], func=AF.Exp, scale=-1.0), r=[T0], w=[Q["PP"]])
        S.dve(lambda v: v.tensor_tensor(out=T0[:], in0=Q["MRAW"][:], in1=Q["MPREV"][:], op=ALU.max), r=[Q["MRAW"], Q["MPREV"], Q["PP"]], w=[T0])
        S.dve(lambda v: v.tensor_tensor(out=T0[:], in0=T0[:], in1=CUM[:, :, 0:8], op=ALU.add), r=[T0, CUM], w=[T0])
        S.act(lambda a: a.activation(out=Q["FL"][:], in_=T0[:], func=AF.Exp, scale=-1.0), r=[T0], w=[Q["FL"]])
        S.dve(lambda v: v.tensor_tensor(out=T0[:], in0=A[:], in1=Q["ML"][:], op=ALU.subtract), r=[A, Q["ML"], Q["FL"]], w=[T0])
        S.act(lambda a: a.activation(out=Q["KW"][:], in_=T0[:], func=AF.Exp), r=[T0], w=[Q["KW"]])
        S.dve(lambda v: v.tensor_tensor(out=T0[:], in0=Q["MPREV"][:], in1=Q["ML"][:], op=ALU.subtract), r=[Q["MPREV"], Q["ML"], Q["KW"]], w=[T0])
        S.act(lambda a: a.activation(out=Q["PW"][:], in_=T0[:], func=AF.Exp), r=[T0], w=[Q["PW"]])
        S.dve(lambda v: v.tensor_tensor(out=T0[:], in0=A[:], in1=Q["MPREV"][:], op=ALU.subtract), r=[A, Q["MPREV"], Q["PW"]], w=[T0])
        S.act(lambda a: a.activation(out=Q["U"][:], in_=T0[:], func=AF.Exp), r=[T0], w=[Q["U"]])

        W4 = [sb(st, "m_W4_%d" % i, [P, 8, 4 * P], BF16) for i in range(2)]
        u = sb(st, "m_u", [P, T], F32)
        acc = sb(st, "m_acc", [P, T], F32)
        qT2 = [sb(st, "m_qT%d" % i, [P, T], BF16) for i in range(2)]
        kT2 = [sb(st, "m_kT%d" % i, [P, T], BF16) for i in range(2)]
        kTM2 = [sb(st, "m_kTM%d" % i, [P, NT, P], BF16) for i in range(2)]
        vaug2 = [sb(st, "m_vaug%d" % i, [P, NT, P + 1], BF16) for i in range(2)]
        oTM2 = [sb(st, "m_oTM%d" % i, [P, NT, P], BF16) for i in range(2)]
        HN = [sb(st, "m_HN%d" % i, [P, NT, P + 1], F32) for i in range(2)]
        YTs = sb(st, "m_YTs", [P, T], BF16)
        yb = [sb(st, "m_yb%d" % i, [P, P], BF16) for i in range(2)]
        ynb = [sb(st, "m_yn%d" % i, [P, P], F32) for i in range(2)]
        BST = sb(st, "m_BST", [P, NT, 6], F32)
        MV = sb(st, "m_MV", [P, NT, 2], F32)
        RSV = sb(st, "m_RSV", [P, NT], F32)
        RCP = sb(st, "m_RCP", [P, 2, NT], F32)
        GROUP = 3
        NU = 2 * GROUP
        NSL = 2 * GROUP
        D_ = []
        for r in range(2):
            d = dict(
                CN32=sb(st, "m_CN32_%d" % r, [P, P + 1], F32),
                CNbf=[sb(st, "m_CNbf%d_%d" % (r, i), [P, P + 1], BF16) for i in range(2)],
                S1=[sb(st, "m_S1_%d_%d" % (r, i), [P, P + 1], F32) for i in range(NSL)],
                KV=[sb(st, "m_KV_%d_%d" % (r, i), [P, P + 1], F32) for i in range(NSL)],
                )
            D_.append(d)
        UB = dict(diagM=[sb(st, "m_udM%d" % i, [P, P], F32) for i in range(NU)],
                  W0=[sb(st, "m_uW0%d" % i, [P, P], F32) for i in range(NU)],
                  PT=[sb(st, "m_uPT%d" % i, [P, P], BF16) for i in range(NU)],
                  ks=[sb(st, "m_uks%d" % i, [P, P], BF16) for i in range(NU)])
        for vv_ in vaug2:
            S.pool(lambda g: g.memset(vv_[:], 1.0), w=[vv_])

        def load_head(h):
            w = W4[h % 2]
            wi = k.w_in[l]
            for j in range(4):
                load_w(k, w, slice(j * P, (j + 1) * P), wi, 2048 + j * 512 + h * P, P)

        def proj(h):
            w = W4[h % 2]
            qT, kT, kTM, vaug, oTM = qT2[h % 2], kT2[h % 2], kTM2[h % 2], vaug2[h % 2], oTM2[h % 2]
            for (dst, c0, blk, sc) in ((qT, 0, h, 1.0), (kT, P, 4 + h, QK_SCALE)):
                for (tok0, n) in TB:
                    bA = bank()
                    proj_fm(k, bA[:, 0:n], bA, w, c0, tok0, n, hT)
                    S.act(lambda a: a.activation(out=u[:, tok0:tok0 + n], in_=bA[:, 0:n], func=AF.Copy), r=[bA], w=[u])
                    yield
                for (s0, n) in ((0, TL), (TL, TC)):
                    S.dve(lambda v: v.tensor_scalar(out=acc[:, s0:s0 + n], in0=u[:, s0:s0 + n], scalar1=cw[:, blk, 2:3], scalar2=None,
                                                    op0=ALU.mult), r=[u, cw], w=[acc])
                    for wv in (0, 1, 3, 4):
                        sh = wv - 2
                        lo = max(0, -sh)
                        hi = n - max(0, sh)
                        S.dve(lambda v: v.scalar_tensor_tensor(out=acc[:, s0 + lo:s0 + hi], in0=u[:, s0 + lo + sh:s0 + hi + sh],
                                                               scalar=cw[:, blk, wv:wv + 1], in1=acc[:, s0 + lo:s0 + hi],
                                                               op0=ALU.mult, op1=ALU.add), r=[u, cw, acc], w=[acc])
                S.act(lambda a: a.activation(out=dst[:], in_=acc[:], func=AF.Silu, bias=cb[:, blk:blk + 1]), r=[acc, cb], w=[dst])
                if sc != 1.0:
                    S.pool(lambda g: g.tensor_scalar(out=dst[:], in0=dst[:], scalar1=sc, scalar2=1.0, op0=ALU.mult, op1=ALU.mult), r=[dst], w=[dst])
                yield
            for c0 in range(0, NT, 4):
                n = min(4, NT - c0)
                b = bank()
                pv = b[:].bitcast(BF16)[:, 0:512].rearrange("p (a b) -> p a b", a=4)
                for i in range(n):
                    c = c0 + i
                    S.pe(lambda t: t.transpose(out=pv[:, i, :], in_=kT[:, c * P:(c + 1) * P], identity=k.identb[:]),
                         r=[kT, k.identb], w=[b], signal=(i == n - 1))
                S.dve(lambda v: v.tensor_copy(out=kTM[:, c0:c0 + n, :], in_=pv[:, 0:n, :]), r=[b], w=[kTM])
                yield
            for c0 in range(0, NT, 2):
                bv = bank()
                pvv = bv[:].rearrange("p (a b) -> p a b", a=2)
                for i in range(2):
                    proj_tm(k, pvv[:, i, :], bv, w, 2 * P, 2 * P, c0 + i, hT, last=(i == 1))
                S.dve(lambda v: v.tensor_copy(out=vaug[:, c0:c0 + 2, 0:P], in_=pvv[:, :, 0:P]), r=[bv], w=[vaug])
                S.act(lambda a: a.activation(out=oTM[:, c0:c0 + 2, :], in_=pvv[:, :, P:2 * P], func=AF.Sigmoid), r=[bv], w=[oTM])
                yield

        def scan(h):
            qT, kT, kTM, vaug, oTM = qT2[h % 2], kT2[h % 2], kTM2[h % 2], vaug2[h % 2], oTM2[h % 2]
            for r in range(2):
                d = D_[r]
                S.dve(lambda v: v.memset(d["CN32"][:], 0.0), w=[d["CN32"]])
                S.dve(lambda v: v.memset(d["CNbf"][0][:], 0.0), w=[d["CNbf"][0]])

            def wbatch(units):
                info = []
                for ui, (si, r) in enumerate(units):
                    c = (ORDER_F if r == 0 else ORDER_B)[si]
                    info.append(dict(ui=ui, si=si, r=r, d=D_[r], c=c, col=r * 4 + h, cs=slice(c * P, (c + 1) * P),
                                     out=((c < NTL) or ctx_out), upd=(si < NT - 1)))
                outs = [x for x in info if x["out"]]
                for x in outs:
                    dM = UB["diagM"][x["ui"]]
                    S.dve(lambda v: v.tensor_scalar(out=dM[:], in0=cst("ident"), scalar1=Q["MRAW"][:, x["c"], x["col"]:x["col"] + 1], scalar2=None,
                                                    op0=ALU.mult), r=[k.C, Q["MRAW"]], w=[dM])
                for x in outs:
                    x["bM"] = bank()
                    dM = UB["diagM"][x["ui"]]
                    S.pe(lambda t: t.matmul(x["bM"][:, 0:P], lhsT=cst("ones"), rhs=dM[:], start=True, stop=True), r=[k.C, dM], w=[x["bM"]])
                for x in outs:
                    W0 = UB["W0"][x["ui"]]
                    nji = cst("nji_f" if x["r"] == 0 else "nji_b")
                    S.dve(lambda v: v.tensor_tensor(out=W0[:], in0=x["bM"][:, 0:P], in1=nji, op=ALU.add), r=[x["bM"], k.C], w=[W0])
                for x in outs:
                    W0 = UB["W0"][x["ui"]]
                    S.act(lambda a: a.activation(out=W0[:], in_=W0[:], func=AF.Exp, scale=-1.0, bias=A[:, x["c"], x["col"]:x["col"] + 1]),
                          r=[W0, A], w=[W0])
                for x in outs:
                    x["bS"] = bank()
                    S.pe(lambda t: t.matmul(x["bS"][:, 0:P], lhsT=kT[:, x["cs"]], rhs=qT[:, x["cs"]], start=True, stop=True), r=[kT, qT], w=[x["bS"]])
                for x in outs:
                    W0, PT = UB["W0"][x["ui"]], UB["PT"][x["ui"]]
                    S.dve(lambda v: v.scalar_tensor_tensor(out=PT[:], in0=W0[:], scalar=Q["U"][:, x["c"], x["col"]:x["col"] + 1], in1=x["bS"][:, 0:P],
                                                           op0=ALU.min, op1=ALU.mult), r=[W0, Q["U"], x["bS"]], w=[PT])
                for x in outs:
                    x["b1"] = bank()
                    PT = UB["PT"][x["ui"]]
                    S.pe(lambda t: t.matmul(x["b1"][:, 0:P + 1], lhsT=PT[:], rhs=vaug[:, x["c"], :], start=True, stop=True), r=[PT, vaug], w=[x["b1"]])
                for x in outs:
                    s1 = x["d"]["S1"][x["si"] % NSL]
                    S.act(lambda a: a.activation(out=s1[:], in_=x["b1"][:, 0:P + 1], func=AF.Copy), r=[x["b1"]], w=[s1])
                ups = [x for x in info if x["upd"]]
                for x in ups:
                    ks = UB["ks"][x["ui"]]
                    S.act(lambda a: a.activation(out=ks[:], in_=kTM[:, x["c"], :], func=AF.Identity, scale=Q["KW"][:, x["c"], x["col"]:x["col"] + 1]),
                          r=[kTM, Q["KW"]], w=[ks])
                for x in ups:
                    x["bC"] = bank()
                    ks = UB["ks"][x["ui"]]
                    S.pe(lambda t: t.matmul(x["bC"][:, 0:P + 1], lhsT=ks[:], rhs=vaug[:, x["c"], :], start=True, stop=True), r=[ks, vaug], w=[x["bC"]])
                for x in ups:
                    kv = x["d"]["KV"][x["si"] % NSL]
                    S.act(lambda a: a.activation(out=kv[:], in_=x["bC"][:, 0:P + 1], func=AF.Copy), r=[x["bC"]], w=[kv])

            def sphase(si, r):
                d = D_[r]
                c = (ORDER_F if r == 0 else ORDER_B)[si]
                col = r * 4 + h
                cs = slice(c * P, (c + 1) * P)
                need_out = (c < NTL) or ctx_out
                cnb = d["CNbf"][si % 2]
                if need_out:
                    s1 = d["S1"][si % NSL]
                    b2 = bank()
                    S.pe(lambda t: t.matmul(b2[:, 0:P + 1], lhsT=qT[:, cs], rhs=cnb[:], start=True, stop=True), r=[qT, cnb], w=[b2])
                    S.dve(lambda v: v.scalar_tensor_tensor(out=HN[r][:, c, :], in0=b2[:, 0:P + 1], scalar=Q["PP"][:, c, col:col + 1], in1=s1[:],
                                                           op0=ALU.mult, op1=ALU.add), r=[b2, Q["PP"], s1], w=[HN[r]])
                if si < NT - 1:
                    kv = d["KV"][si % NSL]
                    S.dve(lambda v: v.scalar_tensor_tensor(out=d["CN32"][:], in0=d["CN32"][:], scalar=Q["PW"][:, c, col:col + 1], in1=kv[:],
                                                           op0=ALU.mult, op1=ALU.add), r=[d["CN32"], Q["PW"], kv], w=[d["CN32"]])
                    cnn = d["CNbf"][(si + 1) % 2]
                    S.act(lambda a: a.activation(out=cnn[:], in_=d["CN32"][:], func=AF.Copy), r=[d["CN32"]], w=[cnn])

            ngroups = NT // GROUP
            for g in range(ngroups + 1):
                if g < ngroups:
                    wbatch([(si, r) for si in range(g * GROUP, (g + 1) * GROUP) for r in range(2)])
                    yield
                if g >= 1:
                    for si in range((g - 1) * GROUP, g * GROUP):
                        for r in range(2):
                            sphase(si, r)
                        yield
            clist = ([16, 17] if ctx_out else []) + list(range(NTL))
            c_lo, c_hi = (0, NT) if ctx_out else (0, NTL)
            for r in range(2):
                col = r * 4 + h
                S.act(lambda a: a.activation(out=RCP[:, r, c_lo:c_hi], in_=HN[r][:, c_lo:c_hi, P], func=AF.Abs), r=[HN[r]], w=[RCP])
                S.dve(lambda v: v.tensor_tensor(out=RCP[:, r, c_lo:c_hi], in0=RCP[:, r, c_lo:c_hi], in1=Q["FL"][:, c_lo:c_hi, col], op=ALU.max),
                      r=[RCP, Q["FL"]], w=[RCP])
                S.dve(lambda v: v.reciprocal(out=RCP[:, r, c_lo:c_hi], in_=RCP[:, r, c_lo:c_hi]), r=[RCP], w=[RCP])
            H0 = HN[0]
            for c in clist:
                S.dve(lambda v: v.tensor_scalar(out=H0[:, c, 0:P], in0=H0[:, c, 0:P], scalar1=RCP[:, 0, c:c + 1], scalar2=None, op0=ALU.mult),
                      r=[H0, RCP], w=[H0])
                S.dve(lambda v: v.scalar_tensor_tensor(out=H0[:, c, 0:P], in0=HN[1][:, c, 0:P], scalar=RCP[:, 1, c:c + 1], in1=H0[:, c, 0:P],
                                                       op0=ALU.mult, op1=ALU.add), r=[HN[1], RCP, H0], w=[H0])
                S.pool(lambda g: g.tensor_tensor(out=H0[:, c, 0:P], in0=H0[:, c, 0:P], in1=oTM[:, c, :], op=ALU.mult), r=[H0, oTM], w=[H0])
                yield
            yield from bulk_norm_out(k, H0, clist, BST, MV, RSV, mng[:, h * P:(h + 1) * P], mng, None, ynb, yb, YTs, width=P)
            ncols = T if ctx_out else TL
            S.dma("sync", k.YT[4 + h, :, 0:ncols], YTs[:, 0:ncols], r=[YTs])

        load_head(0)
        load_head(1)
        for _ in proj(0):
            pass
        for h in range(4):
            if h + 2 < 4:
                load_head(h + 2)
            run_interleaved([scan(h)] + ([proj(h + 1)] if h + 1 < 4 else []))


def natten(k, l, hT, ctx_out):
    nc, S, sb, cst, bank = k.nc, k.S, k.sb, k.cst, k.bank
    with contextlib.ExitStack() as st:
        NB = sb(st, "n_NB", [P, 4, 19, 64], F32)
        S.dma("sync", NB[:], k.na_bias[l], w=[NB])
        W3 = [sb(st, "n_W3_%d" % i, [P, 8, 3 * P], BF16) for i in range(2)]
        qT = sb(st, "n_qT", [P, T], BF16)
        kT = sb(st, "n_kT", [P, T], BF16)
        vaug = sb(st, "n_vaug", [P, NT, P + 1], BF16)
        YTs = sb(st, "n_YTs", [P, T], BF16)
        e_ = [sb(st, "n_e%d" % i, [P, 5, 64], F32) for i in range(3)]
        PT = [sb(st, "n_PT%d" % i, [P, 7, 64], BF16) for i in range(3)]
        PTc = sb(st, "n_PTc", [P, 2, TC], BF16)
        rc = [sb(st, "n_rc%d" % i, [P, 1], F32) for i in range(3)]
        ob = [sb(st, "n_ob%d" % i, [P, P], BF16) for i in range(3)]
        S.pool(lambda g: g.memset(vaug[:], 1.0), w=[vaug])

        def load_head(h):
            w = W3[h % 2]
            for j in range(3):
                load_w(k, w, slice(j * P, (j + 1) * P), k.w_in[l], 4112 + j * 512 + h * P, P)

        load_head(0)
        for h in range(4):
            w = W3[h % 2]
            if h + 1 < 4:
                load_head(h + 1)
            for (dst, c0) in ((qT, 0), (kT, P)):
                for (tok0, n) in TB:
                    bA = bank()
                    proj_fm(k, bA[:, 0:n], bA, w, c0, tok0, n, hT)
                    S.act(lambda a: a.activation(out=dst[:, tok0:tok0 + n], in_=bA[:, 0:n], func=AF.Copy), r=[bA], w=[dst])
            for c0 in range(0, NT, 4):
                n = min(4, NT - c0)
                bv = bank()
                pvv = bv[:].rearrange("p (a b) -> p a b", a=4)
                for i in range(n):
                    proj_tm(k, pvv[:, i, :], bv, w, 2 * P, P, c0 + i, hT, last=(i == n - 1))
                S.dve(lambda v: v.tensor_copy(out=vaug[:, c0:c0 + n, 0:P], in_=pvv[:, 0:n, :]), r=[bv], w=[vaug])
            bT = None
            NB_ = 3
            for r0 in range(0, 32, NB_):
                rows_ = []
                for r in range(r0, min(r0 + NB_, 32)):
                    rs = min(max(r - 4, 0), 24)
                    if rs % 2 == 0:
                        nl, t0 = 4, rs // 2
                        tau0 = rs - r + 7
                        bias_ap = NB[:, h, tau0:tau0 + 7:2, :]
                    else:
                        nl, t0 = 5, (rs - 1) // 2
                        bias_ap = NB[:, h, 14:19, :]
                    rows_.append(dict(r=r, nl=nl, bias=bias_ap, qs=slice(r * 64, (r + 1) * 64), tiles=[t0 + m for m in range(nl)] + [16, 17],
                                      ee=e_[r % NB_], pt=PT[r % NB_], rc=rc[r % NB_], ob=ob[r % NB_]))
                for x in rows_:
                    x["bS"] = bank()
                    x["pS"] = x["bS"][:, 0:448].rearrange("p (a b) -> p a b", a=7)
                    for m, tt in enumerate(x["tiles"]):
                        S.pe(lambda t: t.matmul(x["pS"][:, m, :], lhsT=kT[:, tt * P:(tt + 1) * P], rhs=qT[:, x["qs"]], start=True, stop=True),
                             r=[kT, qT], w=[x["bS"]], signal=(m == len(x["tiles"]) - 1))
                for x in rows_:
                    nl = x["nl"]
                    S.dve(lambda v: v.scalar_tensor_tensor(out=x["ee"][:, 0:nl, :], in0=x["pS"][:, 0:nl, :], scalar=QK_SCALE, in1=x["bias"],
                                                           op0=ALU.mult, op1=ALU.add), r=[x["bS"], NB], w=[x["ee"]])
                for x in rows_:
                    nl = x["nl"]
                    S.act(lambda a: a.activation(out=x["pt"][:, 0:nl, :], in_=x["ee"][:, 0:nl, :], func=AF.Exp), r=[x["ee"]], w=[x["pt"]])
                    S.act(lambda a: a.activation(out=x["pt"][:, nl:nl + 2, :], in_=x["pS"][:, nl:nl + 2, :], func=AF.Exp, scale=QK_SCALE),
                          r=[x["bS"]], w=[x["pt"]])
                for x in rows_:
                    x["bO"] = bank()
                    for m, tt in enumerate(x["tiles"]):
                        S.pe(lambda t: t.matmul(x["bO"][0:64, 0:P + 1], lhsT=x["pt"][:, m, :], rhs=vaug[:, tt, :], start=(m == 0),
                                                stop=(m == len(x["tiles"]) - 1)), r=[x["pt"], vaug], w=[x["bO"]], signal=(m == len(x["tiles"]) - 1))
                for x in rows_:
                    S.dve(lambda v: v.reciprocal(out=x["rc"][0:64, :], in_=x["bO"][0:64, P:P + 1]), r=[x["bO"]], w=[x["rc"]])
                    S.dve(lambda v: v.tensor_scalar(out=x["ob"][0:64, :], in0=x["bO"][0:64, 0:P], scalar1=x["rc"][0:64, 0:1], scalar2=None,
                                                    op0=ALU.mult), r=[x["bO"], x["rc"]], w=[x["ob"]])
                for x in rows_:
                    r = x["r"]
                    if r % 8 == 0:
                        bT = k.rbank()
                    pT = bT[:].bitcast(BF16)[:, 0:512].rearrange("p (a b) -> p a b", a=8)
                    bTl = bT
                    S.pe(lambda t: t.transpose(out=pT[:, r % 8, :], in_=x["ob"][0:64, :], identity=k.identb[0:64, 0:64]),
                         r=[x["ob"], k.identb], w=[bTl])
                    if r % 8 == 7:
                        rr0 = r - 7
                        S.act(lambda a: a.activation(out=YTs[:, rr0 * 64:rr0 * 64 + 512], in_=bTl[:].bitcast(BF16)[:, 0:512], func=AF.Copy),
                              r=[bTl], w=[YTs])
            if ctx_out:
                bS = bank()
                pS = bS[:].rearrange("p (a b) -> p a b", a=2)
                for u_ in range(2):
                    S.pe(lambda t: t.matmul(pS[:, u_, :], lhsT=kT[:, TL + u_ * P:TL + (u_ + 1) * P], rhs=qT[:, TL:T], start=True, stop=True),
                         r=[kT, qT], w=[bS], signal=(u_ == 1))
                S.act(lambda a: a.activation(out=PTc[:], in_=pS, func=AF.Exp, scale=QK_SCALE), r=[bS], w=[PTc])
                bT = k.rbank()
                pT = bT[:].bitcast(BF16)[:, 0:256].rearrange("p (a b) -> p a b", a=2)
                for qt in range(2):
                    bO = bank()
                    for u_ in range(2):
                        S.pe(lambda t: t.matmul(bO[:, 0:P + 1], lhsT=PTc[:, u_, qt * P:(qt + 1) * P], rhs=vaug[:, 16 + u_, :],
                                                start=(u_ == 0), stop=(u_ == 1)), r=[PTc, vaug], w=[bO], signal=(u_ == 1))
                    rc_, ob_ = rc[qt], ob[qt]
                    S.dve(lambda v: v.reciprocal(out=rc_[:], in_=bO[:, P:P + 1]), r=[bO], w=[rc_])
                    S.dve(lambda v: v.tensor_scalar(out=ob_[:], in0=bO[:, 0:P], scalar1=rc_[:, 0:1], scalar2=None, op0=ALU.mult),
                          r=[bO, rc_], w=[ob_])
                    S.pe(lambda t: t.transpose(out=pT[:, qt, :], in_=ob_[:], identity=k.identb[:]), r=[ob_, k.identb], w=[bT])
                S.act(lambda a: a.activation(out=YTs[:, TL:T], in_=bT[:].bitcast(BF16)[:, 0:256], func=AF.Copy), r=[bT], w=[YTs])
            ncols = T if ctx_out else TL
            S.dma("sync", k.YT[8 + h, :, 0:ncols], YTs[:, 0:ncols], r=[YTs])


def merge1(k, l, hT, mergedT, ctx_out):
    nc, S, sb, cst, bank = k.nc, k.S, k.sb, k.cst, k.bank
    blocks = TB if ctx_out else TB[:4]
    with contextlib.ExitStack() as st:
        wgt_ = [sb(st, "g_wg%d" % i, [P, 8, 3 * P], BF16) for i in range(2)]
        wbr = [sb(st, "g_wb%d" % i, [P, 12, P], BF16) for i in range(2)]
        yt = [sb(st, "g_yt%d" % i, [P, 12, 512], BF16) for i in range(2)]
        gs2 = [[sb(st, "g_gs%d_%d" % (i, j), [P, 512], F32) for i in range(3)] for j in range(2)]
        tn2 = [[sb(st, "g_tn%d_%d" % (i, j), [P, 512], F32) for i in range(3)] for j in range(2)]

        def load_fc(fc):
            wg_, wb_ = wgt_[fc % 2], wbr[fc % 2]
            for n_ in range(3):
                load_w(k, wg_, slice(n_ * P, (n_ + 1) * P), k.w_in[l], 5648 + n_ * D + fc * P, P)
            for n_ in range(3):
                S.dma("gpsimd", wb_[:, n_ * 4:(n_ + 1) * 4, :],
                      k.w_branch[l, n_].rearrange("(c p) n -> p c n", p=P)[:, :, fc * P:(fc + 1) * P], w=[wb_])

        load_fc(0)
        it = 0
        for fc in range(8):
            wg_, wb_ = wgt_[fc % 2], wbr[fc % 2]
            if fc + 1 < 8:
                load_fc(fc + 1)
            for (tok0, n) in blocks:
                y_ = yt[it % 2]
                gs, tn = gs2[it % 2], tn2[it % 2]
                it += 1
                S.dma("sync", y_[:, :, 0:n], k.YT[:, :, tok0:tok0 + n].rearrange("a p t -> p a t"), w=[y_])
                for n_ in range(3):
                    bG = bank()
                    proj_fm(k, bG[:, 0:n], bG, wg_, n_ * P, tok0, n, hT)
                    S.act(lambda a: a.activation(out=gs[n_][:, 0:n], in_=bG[:, 0:n], func=AF.Sigmoid), r=[bG], w=[gs[n_]])
                    bP = bank()
                    for kc in range(4):
                        S.pe(lambda t: t.matmul(bP[:, 0:n], lhsT=wb_[:, n_ * 4 + kc, :], rhs=y_[:, n_ * 4 + kc, 0:n],
                                                start=(kc == 0), stop=(kc == 3)), r=[wb_, y_], w=[bP], signal=(kc == 3))
                    S.dve(lambda v: v.tensor_tensor(out=tn[n_][:, 0:n], in0=bP[:, 0:n], in1=gs[n_][:, 0:n], op=ALU.mult),
                          r=[bP, gs[n_]], w=[tn[n_]])
                S.pool(lambda g: g.tensor_tensor(out=tn[0][:, 0:n], in0=tn[0][:, 0:n], in1=tn[1][:, 0:n], op=ALU.add),
                       r=[tn[0], tn[1]], w=[tn[0]])
                S.pool(lambda g: g.tensor_tensor(out=mergedT[:, fc, tok0:tok0 + n], in0=tn[0][:, 0:n], in1=tn[2][:, 0:n], op=ALU.add),
                       r=[tn[0], tn[2]], w=[mergedT])


def ap4(tile_ap, dims):
    a0 = list(tile_ap.ap[0])
    return bass.AP(tile_ap.tensor, tile_ap.offset, [a0] + [list(d) for d in dims])


def merge2(k, l, mergedT, n_tiles, mod_row):
    nc, S, sb, cst, bank = k.nc, k.S, k.sb, k.cst, k.bank
    NTn = n_tiles
    with contextlib.ExitStack() as st0:
        H2B = sb(st0, "o_H2B", [P, NTn, D], BF16)
        LALL = sb(st0, "o_LALL", [P, NTn, 36], F32)
        with contextlib.ExitStack() as st:
            wout = sb(st, "o_wout", [P, 8, D], BF16)
            S.dma("gpsimd", wout[:, :, 0:512], k.w_out[l].rearrange("(c p) n -> p c n", p=P)[:, :, 0:512], w=[wout])
            S.dma("gpsimd", wout[:, :, 512:D], k.w_out[l].rearrange("(c p) n -> p c n", p=P)[:, :, 512:D], w=[wout])
            wr = sb(st, "o_wr", [P, 8, 36], F32)
            S.dma("sync", wr[:], k.w_gr[l].rearrange("(c p) n -> p c n", p=P), w=[wr])
            rows = {}
            for which in range(2):
                for j, nm in ((2, "G1"), (3, "A2"), (4, "B2")):
                    rows[(which, nm)] = sb(st, "o_row%s%d" % (nm, which), [P, D], F32)
                    load_row_bc(k, rows[(which, nm)], mod_row(which, j))
            xt = [sb(st, "o_xt%d" % i, [P, D], F32) for i in range(2)]
            tmp_2 = [sb(st, "o_tmp0", [P, D], F32)] * 2
            xn = [sb(st, "o_xn%d" % i, [P, D], F32) for i in range(2)]
            h2_2 = [sb(st, "o_h2%d" % i, [P, D], F32) for i in range(2)]
            h2T_2 = [sb(st, "o_h2T0", [P, 8, P], F32)] * 2
            ss_2 = [sb(st, "o_ss%d" % i, [P, 1], F32) for i in range(2)]
            t1_2 = [sb(st, "o_t1%d" % i, [P, 1], F32) for i in range(2)]
            rstd_2 = [sb(st, "o_rstd%d" % i, [P, 1], F32) for i in range(2)]
            def part_a(t):
                which = 0 if t < NTL else 1
                x_, xn_ = xt[t % 2], xn[t % 2]
                tmp = tmp_2[t % 2]
                G1 = rows[(which, "G1")]
                S.dma("sync", x_[:], k.xres[t * P:(t + 1) * P, :], w=[x_])
                for half in range(2):
                    bY = bank()
                    for fc in range(8):
                        S.pe(lambda te: te.matmul(bY[:, :], lhsT=mergedT[:, fc, t * P:(t + 1) * P], rhs=wout[:, fc, half * 512:(half + 1) * 512],
                                                  start=(fc == 0), stop=(fc == 7)), r=[mergedT, wout], w=[bY], signal=(fc == 7))
                    S.dve(lambda v: v.tensor_tensor(out=tmp[:, half * 512:(half + 1) * 512], in0=bY[:, :], in1=G1[:, half * 512:(half + 1) * 512],
                                                    op=ALU.mult), r=[bY, G1], w=[tmp])
                S.dve(lambda v: v.tensor_tensor(out=xn_[:], in0=x_[:], in1=tmp[:], op=ALU.add), r=[x_, tmp], w=[xn_])
                S.dma("sync", k.xres[t * P:(t + 1) * P, :], xn_[:], r=[xn_])

            def part_b(t):
                which = 0 if t < NTL else 1
                xn_ = xn[t % 2]
                tmp, h2, h2T, ss, t1, rstd = tmp_2[t % 2], h2_2[t % 2], h2T_2[t % 2], ss_2[t % 2], t1_2[t % 2], rstd_2[t % 2]
                A2, B2 = rows[(which, "A2")], rows[(which, "B2")]
                S.act(lambda a: a.activation(out=h2[:], in_=xn_[:], func=AF.Square, accum_out=ss[:]), r=[xn_], w=[h2, ss])
                rstd_from_ss(k, ss, t1, rstd, 1.0 / D, EPS)
                S.dve(lambda v: v.scalar_tensor_tensor(out=tmp[:], in0=xn_[:], scalar=rstd[:, 0:1], in1=A2[:], op0=ALU.mult, op1=ALU.mult),
                      r=[xn_, rstd, A2], w=[tmp])
                S.dve(lambda v: v.tensor_tensor(out=h2[:], in0=tmp[:], in1=B2[:], op=ALU.add), r=[tmp, B2], w=[h2])
                S.act(lambda a: a.activation(out=H2B[:, t, :], in_=h2[:], func=AF.Copy), r=[h2], w=[H2B])
                for half in range(2):
                    bT = bank()
                    pT = bT[:].rearrange("p (a b) -> p a b", a=4)
                    for i in range(4):
                        c = half * 4 + i
                        S.pe(lambda te: te.transpose(out=pT[:, i, :], in_=h2[:, c * P:(c + 1) * P], identity=cst("ident")),
                             r=[h2, k.C], w=[bT], signal=(i == 3))
                    if half == 0:
                        S.act(lambda a: a.activation(out=h2T[:, 0:4, :], in_=pT, func=AF.Copy), r=[bT], w=[h2T])
                    else:
                        S.dve(lambda v: v.tensor_copy(out=h2T[:, 4:8, :], in_=pT), r=[bT], w=[h2T])
                bL = bank()
                for kc in range(8):
                    S.pe(lambda te: te.matmul(bL[:, 0:36], lhsT=h2T[:, kc, :], rhs=wr[:, kc, :], start=(kc == 0), stop=(kc == 7)),
                         r=[h2T, wr], w=[bL], signal=(kc == 7))
                S.dve(lambda v: v.tensor_copy(out=LALL[:, t, :], in_=bL[:, 0:36]), r=[bL], w=[LALL])

            part_a(0)
            for t in range(n_tiles):
                if t + 1 < n_tiles:
                    part_a(t + 1)
                part_b(t)
            S.fence()
        with contextlib.ExitStack() as st:
            route_all(k, st, LALL, NTn)
            scb = [Buf("scat0"), Buf("scat1")]
            for t in range(n_tiles):
                for kk in range(2):
                    S.idma(out=k.xs[:, :], out_offset=bass.IndirectOffsetOnAxis(ap=k.dests[:, t, kk:kk + 1], axis=0),
                           in_=H2B[:, t, :], in_offset=None, bounds=XS_ROWS - 1, r=[H2B, k.dests], w=[scb[kk]])
            if k.dbg_route is not None:
                dbg = sb(st, "o_dbg", [P, NT, 6], F32)
                S.dve(lambda v: v.tensor_copy(out=dbg[:, :, 0:2], in_=k.dests[:]), r=[k.dests], w=[dbg])
                S.dve(lambda v: v.tensor_copy(out=dbg[:, :, 2:4], in_=k.destg[:]), r=[k.destg], w=[dbg])
                S.dve(lambda v: v.tensor_copy(out=dbg[:, :, 4:6], in_=k.wgt[:]), r=[k.wgt], w=[dbg])
                S.dma("sync", k.dbg_route, dbg[:], r=[dbg])
            S.fence()


def route_all(k, st, LALL, NTn):
    S, sb, cst, bank = k.S, k.sb, k.cst, k.bank
    f3 = lambda nm, w: sb(st, "q_" + nm, [P, NTn, w], F32)
    f2 = lambda nm: sb(st, "q_" + nm, [P, NTn], F32)
    gmax, gsum, gw, t1, t2, dd, e2, rden, w1 = (f2(n) for n in ("gmax", "gsum", "gw", "t1", "t2", "dd", "e2", "rden", "w1"))
    gd, gm, pen = f3("gd", 4), f3("gm", 4), f3("pen", 4)
    mk, sel1, sel2, sel12, pos, CS, BASE, okm, t32 = (f3(n, NE) for n in ("mk", "sel1", "sel2", "sel12", "pos", "CS", "BASE", "okm", "t32"))
    dv, ok, ds, dg = (sb(st, "q_" + n, [P, 2, NTn], F32) for n in ("dv", "ok", "ds", "dg"))
    GL = LALL[:, :, 0:4]
    S.dve(lambda v: v.tensor_reduce(out=gmax[:], in_=GL, axis=AX.X, op=ALU.max), r=[LALL], w=[gmax])
    S.dve(lambda v: v.tensor_tensor(out=gd[:], in0=GL, in1=bc_last(gmax[:], 4), op=ALU.subtract), r=[LALL, gmax], w=[gd])
    S.dve(lambda v: v.tensor_tensor(out=gm[:], in0=GL, in1=bc_last(gmax[:], 4), op=ALU.is_ge), r=[LALL, gmax], w=[gm])
    S.act(lambda a: a.activation(out=gd[:], in_=gd[:], func=AF.Exp), r=[gd], w=[gd])
    S.dve(lambda v: v.tensor_reduce(out=gsum[:], in_=gd[:], axis=AX.X, op=ALU.add), r=[gd], w=[gsum])
    S.dve(lambda v: v.reciprocal(out=gw[:], in_=gsum[:]), r=[gsum], w=[gw])
    S.dve(lambda v: v.tensor_scalar(out=pen[:], in0=gm[:], scalar1=BIG, scalar2=-BIG, op0=ALU.mult, op1=ALU.add), r=[gm], w=[pen])
    el4 = ap4(LALL[:, :, 4:36], [[36, NTn], [8, 4], [1, 8]])
    pen4 = ap4(pen[:], [[4, NTn], [1, 4], [0, 8]])
    mk4 = ap4(mk[:], [[NE, NTn], [8, 4], [1, 8]])
    S.dve(lambda v: v.tensor_tensor(out=mk4, in0=el4, in1=pen4, op=ALU.add), r=[LALL, pen], w=[mk])
    S.dve(lambda v: v.tensor_reduce(out=t1[:], in_=mk[:], axis=AX.X, op=ALU.max), r=[mk], w=[t1])
    S.dve(lambda v: v.tensor_tensor(out=sel1[:], in0=mk[:], in1=bc_last(t1[:], NE), op=ALU.is_ge), r=[mk, t1], w=[sel1])
    S.dve(lambda v: v.scalar_tensor_tensor(out=mk[:], in0=sel1[:], scalar=-BIG, in1=mk[:], op0=ALU.mult, op1=ALU.add), r=[sel1, mk], w=[mk])
    S.dve(lambda v: v.tensor_reduce(out=t2[:], in_=mk[:], axis=AX.X, op=ALU.max), r=[mk], w=[t2])
    S.dve(lambda v: v.tensor_tensor(out=sel2[:], in0=mk[:], in1=bc_last(t2[:], NE), op=ALU.is_ge), r=[mk, t2], w=[sel2])
    S.dve(lambda v: v.tensor_tensor(out=sel12[:], in0=sel1[:], in1=sel2[:], op=ALU.add), r=[sel1, sel2], w=[sel12])
    S.dve(lambda v: v.tensor_tensor(out=dd[:], in0=t2[:], in1=t1[:], op=ALU.subtract), r=[t1, t2], w=[dd])
    S.act(lambda a: a.activation(out=e2[:], in_=dd[:], func=AF.Exp), r=[dd], w=[e2])
    S.dve(lambda v: v.tensor_scalar(out=rden[:], in0=e2[:], scalar1=1.0, scalar2=None, op0=ALU.add), r=[e2], w=[rden])
    S.dve(lambda v: v.reciprocal(out=rden[:], in_=rden[:]), r=[rden], w=[rden])
    S.dve(lambda v: v.tensor_tensor(out=w1[:], in0=gw[:], in1=rden[:], op=ALU.mult), r=[gw, rden], w=[w1])
    NB9 = 9
    for g0 in range(0, NTn, NB9):
        n = min(NB9, NTn - g0)
        bP = bank()
        bC = bank()
        for i in range(n):
            t = g0 + i
            S.pe(lambda te: te.matmul(bP[:, i * NE:(i + 1) * NE], lhsT=cst("tris"), rhs=sel12[:, t, :], start=True, stop=True),
                 r=[k.C, sel12], w=[bP], signal=(i == n - 1))
        for i in range(n):
            t = g0 + i
            S.pe(lambda te: te.matmul(bC[:, i * NE:(i + 1) * NE], lhsT=cst("ones"), rhs=sel12[:, t, :], start=True, stop=True),
                 r=[k.C, sel12], w=[bC], signal=(i == n - 1))
        S.dve(lambda v: v.tensor_copy(out=pos[:, g0:g0 + n, :], in_=bP[:, 0:n * NE].rearrange("p (a b) -> p a b", a=n)), r=[bP], w=[pos])
        S.dve(lambda v: v.tensor_copy(out=CS[:, g0:g0 + n, :], in_=bC[:, 0:n * NE].rearrange("p (a b) -> p a b", a=n)), r=[bC], w=[CS])
    S.dve(lambda v: v.memset(BASE[:, 0, :], 0.0), w=[BASE])
    for t in range(1, NTn):
        S.dve(lambda v: v.tensor_tensor(out=BASE[:, t, :], in0=BASE[:, t - 1, :], in1=CS[:, t - 1, :], op=ALU.add), r=[BASE, CS], w=[BASE])
    S.dve(lambda v: v.tensor_tensor(out=pos[:], in0=pos[:], in1=BASE[:], op=ALU.add), r=[pos, BASE], w=[pos])
    S.dve(lambda v: v.tensor_scalar(out=okm[:], in0=pos[:], scalar1=float(CAP), scalar2=None, op0=ALU.is_lt), r=[pos], w=[okm])
    S.dve(lambda v: v.tensor_tensor(out=pos[:], in0=pos[:], in1=bc_mid(cst("ecap"), NTn), op=ALU.add), r=[pos, k.C], w=[pos])
    for kk, sel in enumerate((sel1, sel2)):
        S.dve(lambda v: v.tensor_tensor(out=t32[:], in0=pos[:], in1=sel[:], op=ALU.mult), r=[pos, sel], w=[t32])
        S.dve(lambda v: v.tensor_reduce(out=dv[:, kk, :], in_=t32[:], axis=AX.X, op=ALU.add), r=[t32], w=[dv])
        S.dve(lambda v: v.tensor_tensor(out=t32[:], in0=okm[:], in1=sel[:], op=ALU.mult), r=[okm, sel], w=[t32])
        S.dve(lambda v: v.tensor_reduce(out=ok[:, kk, :], in_=t32[:], axis=AX.X, op=ALU.add), r=[t32], w=[ok])
    S.dve(lambda v: v.tensor_scalar(out=ds[:], in0=ok[:], scalar1=-1.0e6, scalar2=1.0e6, op0=ALU.mult, op1=ALU.add), r=[ok], w=[ds])
    S.dve(lambda v: v.tensor_tensor(out=ds[:], in0=ds[:], in1=dv[:], op=ALU.add), r=[ds, dv], w=[ds])
    S.dve(lambda v: v.tensor_scalar(out=dg[:], in0=ds[:], scalar1=float(XS_ROWS), scalar2=None, op0=ALU.min), r=[ds], w=[dg])
    for kk in range(2):
        S.dve(lambda v: v.tensor_copy(out=k.dests[:, 0:NTn, kk], in_=ds[:, kk, :]), r=[ds], w=[k.dests])
        S.dve(lambda v: v.tensor_copy(out=k.destg[:, 0:NTn, kk], in_=dg[:, kk, :]), r=[dg], w=[k.destg])
    S.dve(lambda v: v.tensor_tensor(out=k.wgt[:, 0:NTn, 0], in0=w1[:], in1=ok[:, 0, :], op=ALU.mult), r=[w1, ok], w=[k.wgt])
    S.dve(lambda v: v.tensor_tensor(out=w1[:], in0=w1[:], in1=e2[:], op=ALU.mult), r=[w1, e2], w=[w1])
    S.dve(lambda v: v.tensor_tensor(out=k.wgt[:, 0:NTn, 1], in0=w1[:], in1=ok[:, 1, :], op=ALU.mult), r=[w1, ok], w=[k.wgt])


def route_tile(k, t, L, base, rt):
    S, cst, bank = k.S, k.cst, k.bank
    sc = rt["sc"]
    gl = L[:, 0:4]
    S.dve(lambda v: v.reduce_max(out=sc[:, 0:1], in_=gl, axis=AX.X), r=[L], w=[sc])
    S.dve(lambda v: v.tensor_scalar(out=sc[:, 1:2], in0=sc[:, 0:1], scalar1=-1.0, scalar2=None, op0=ALU.mult), r=[sc], w=[sc])
    S.act(lambda a: a.activation(out=rt["ge"][:], in_=gl, func=AF.Exp, bias=sc[:, 1:2], accum_out=sc[:, 2:3]), r=[L, sc], w=[rt["ge"], sc])
    S.dve(lambda v: v.reciprocal(out=sc[:, 3:4], in_=sc[:, 2:3]), r=[sc], w=[sc])
    S.dve(lambda v: v.tensor_scalar(out=rt["gm"][:], in0=gl, scalar1=sc[:, 0:1], scalar2=None, op0=ALU.is_ge), r=[L, sc], w=[rt["gm"]])
    S.dve(lambda v: v.tensor_scalar(out=rt["pen"][:], in0=rt["gm"][:], scalar1=BIG, scalar2=-BIG, op0=ALU.mult, op1=ALU.add),
          r=[rt["gm"]], w=[rt["pen"]])
    for g in range(4):
        S.dve(lambda v: v.tensor_scalar(out=rt["mk"][:, g * 8:(g + 1) * 8], in0=L[:, 4 + g * 8:12 + g * 8], scalar1=rt["pen"][:, g:g + 1],
                                        scalar2=None, op0=ALU.add), r=[L, rt["pen"]], w=[rt["mk"]])
    S.dve(lambda v: v.max(out=rt["top"][:], in_=rt["mk"][:]), r=[rt["mk"]], w=[rt["top"]])
    S.dve(lambda v: v.tensor_scalar(out=rt["sel1"][:], in0=rt["mk"][:], scalar1=rt["top"][:, 0:1], scalar2=None, op0=ALU.is_ge),
          r=[rt["mk"], rt["top"]], w=[rt["sel1"]])
    S.dve(lambda v: v.tensor_scalar(out=rt["sel12"][:], in0=rt["mk"][:], scalar1=rt["top"][:, 1:2], scalar2=None, op0=ALU.is_ge),
          r=[rt["mk"], rt["top"]], w=[rt["sel12"]])
    S.dve(lambda v: v.tensor_tensor(out=rt["sel2"][:], in0=rt["sel12"][:], in1=rt["sel1"][:], op=ALU.subtract),
          r=[rt["sel12"], rt["sel1"]], w=[rt["sel2"]])
    S.dve(lambda v: v.tensor_tensor(out=sc[:, 4:5], in0=rt["top"][:, 1:2], in1=rt["top"][:, 0:1], op=ALU.subtract), r=[rt["top"]], w=[sc])
    S.act(lambda a: a.activation(out=sc[:, 5:6], in_=sc[:, 4:5], func=AF.Exp), r=[sc], w=[sc])
    S.dve(lambda v: v.tensor_scalar(out=sc[:, 6:7], in0=sc[:, 5:6], scalar1=1.0, scalar2=None, op0=ALU.add), r=[sc], w=[sc])
    S.dve(lambda v: v.reciprocal(out=sc[:, 7:8], in_=sc[:, 6:7]), r=[sc], w=[sc])
    bP = bank()
    S.pe(lambda te: te.matmul(bP[:, 0:NE], lhsT=cst("tris"), rhs=rt["sel12"][:], start=True, stop=True), r=[k.C, rt["sel12"]], w=[bP])
    S.dve(lambda v: v.tensor_tensor(out=rt["pos"][:], in0=bP[:, 0:NE], in1=base[:], op=ALU.add), r=[bP, base], w=[rt["pos"]])
    bC = bank()
    S.pe(lambda te: te.matmul(bC[:, 0:NE], lhsT=cst("ones"), rhs=rt["sel12"][:], start=True, stop=True), r=[k.C, rt["sel12"]], w=[bC])
    S.dve(lambda v: v.tensor_tensor(out=base[:], in0=base[:], in1=bC[:, 0:NE], op=ALU.add), r=[base, bC], w=[base])
    S.dve(lambda v: v.tensor_scalar(out=rt["okm"][:], in0=rt["pos"][:], scalar1=float(CAP), scalar2=None, op0=ALU.is_lt), r=[rt["pos"]], w=[rt["okm"]])
    S.dve(lambda v: v.tensor_tensor(out=rt["pos"][:], in0=rt["pos"][:], in1=cst("ecap"), op=ALU.add), r=[rt["pos"], k.C], w=[rt["pos"]])
    for kk, sel in enumerate((rt["sel1"], rt["sel2"])):
        S.dve(lambda v: v.tensor_tensor(out=rt["t32"][:], in0=rt["pos"][:], in1=sel[:], op=ALU.mult), r=[rt["pos"], sel], w=[rt["t32"]])
        S.dve(lambda v: v.reduce_sum(out=rt["dv"][:, kk:kk + 1], in_=rt["t32"][:], axis=AX.X), r=[rt["t32"]], w=[rt["dv"]])
        S.dve(lambda v: v.tensor_tensor(out=rt["t32"][:], in0=rt["okm"][:], in1=sel[:], op=ALU.mult), r=[rt["okm"], sel], w=[rt["t32"]])
        S.dve(lambda v: v.reduce_sum(out=rt["ok"][:, kk:kk + 1], in_=rt["t32"][:], axis=AX.X), r=[rt["t32"]], w=[rt["ok"]])
    S.dve(lambda v: v.tensor_scalar(out=rt["ds"][:], in0=rt["ok"][:], scalar1=-1.0e6, scalar2=1.0e6, op0=ALU.mult, op1=ALU.add), r=[rt["ok"]], w=[rt["ds"]])
    S.dve(lambda v: v.tensor_tensor(out=rt["ds"][:], in0=rt["ds"][:], in1=rt["dv"][:], op=ALU.add), r=[rt["ds"], rt["dv"]], w=[rt["ds"]])
    S.dve(lambda v: v.tensor_scalar(out=rt["dg"][:], in0=rt["ds"][:], scalar1=float(XS_ROWS), scalar2=None, op0=ALU.min), r=[rt["ds"]], w=[rt["dg"]])
    S.dve(lambda v: v.tensor_copy(out=k.dests[:, t, :], in_=rt["ds"][:]), r=[rt["ds"]], w=[k.dests])
    S.dve(lambda v: v.tensor_copy(out=k.destg[:, t, :], in_=rt["dg"][:]), r=[rt["dg"]], w=[k.destg])
    S.dve(lambda v: v.tensor_tensor(out=sc[:, 8:9], in0=sc[:, 3:4], in1=sc[:, 7:8], op=ALU.mult), r=[sc], w=[sc])
    S.dve(lambda v: v.tensor_tensor(out=k.wgt[:, t, 0:1], in0=sc[:, 8:9], in1=rt["ok"][:, 0:1], op=ALU.mult), r=[sc, rt["ok"]], w=[k.wgt])
    S.dve(lambda v: v.tensor_tensor(out=sc[:, 9:10], in0=sc[:, 8:9], in1=sc[:, 5:6], op=ALU.mult), r=[sc], w=[sc])
    S.dve(lambda v: v.tensor_tensor(out=k.wgt[:, t, 1:2], in0=sc[:, 9:10], in1=rt["ok"][:, 1:2], op=ALU.mult), r=[sc, rt["ok"]], w=[k.wgt])


def moe(k, l):
    nc, S, sb, cst, bank = k.nc, k.S, k.sb, k.cst, k.bank
    with contextlib.ExitStack() as st:
        wg = [sb(st, "e_wg%d" % i, [P, 8, 512], BF16) for i in range(2)]
        wu = [sb(st, "e_wu%d" % i, [P, 8, 512], BF16) for i in range(2)]
        wd = [sb(st, "e_wd%d" % i, [P, 4, D], BF16) for i in range(2)]
        xrow = [sb(st, "e_xrow%d" % i, [P, D], BF16) for i in range(2)]
        xsT2 = [sb(st, "e_xsT%d" % i, [P, 8, CAP], BF16) for i in range(2)]
        AT = sb(st, "e_AT", [P, 4, CAP], BF16)
        sg = [sb(st, "e_sg%d" % i, [P, CAP], BF16) for i in range(2)]
        ysb = [sb(st, "e_ysb%d" % i, [P, D], BF16) for i in range(2)]

        def load_e(e):
            S.dma("gpsimd", wg[e % 2][:], k.w_eg[l, e].rearrange("(c p) n -> p c n", p=P), w=[wg[e % 2]])
            S.dma("gpsimd", wu[e % 2][:], k.w_eu[l, e].rearrange("(c p) n -> p c n", p=P), w=[wu[e % 2]])
            S.dma("gpsimd", wd[e % 2][:], k.w_ed[l, e].rearrange("(c p) n -> p c n", p=P), w=[wd[e % 2]])

        load_e(0)
        itc = [0]

        def xs_transposes(e):
            xsT = xsT2[e % 2]
            for rt_ in range(CAP // P):
                xr = xrow[itc[0] % 2]
                itc[0] += 1
                r0 = e * CAP + rt_ * P
                S.dma("sync", xr[:], k.xs[r0:r0 + P, :], w=[xr])
                b = bank()
                pv = b[:].bitcast(BF16).rearrange("p (a b) -> p a b", a=8)
                for c in range(8):
                    S.pe(lambda te: te.transpose(out=pv[:, c, :], in_=xr[:, c * P:(c + 1) * P], identity=k.identb[:]),
                         r=[xr, k.identb], w=[b], signal=(c == 7))
                if rt_ % 2 == 0:
                    S.act(lambda a: a.activation(out=xsT[:, :, rt_ * P:(rt_ + 1) * P], in_=pv, func=AF.Copy), r=[b], w=[xsT])
                else:
                    S.dve(lambda v: v.tensor_copy(out=xsT[:, :, rt_ * P:(rt_ + 1) * P], in_=pv), r=[b], w=[xsT])

        xs_transposes(0)
        for e in range(NE):
            if e + 1 < NE:
                load_e(e + 1)
            wg_, wu_, wd_ = wg[e % 2], wu[e % 2], wd[e % 2]
            xsT = xsT2[e % 2]
            for fc in range(4):
                bG = bank()
                bU = bank()
                for kc in range(8):
                    S.pe(lambda te: te.matmul(bG[:, 0:CAP], lhsT=wg_[:, kc, fc * P:(fc + 1) * P], rhs=xsT[:, kc, :], start=(kc == 0), stop=(kc == 7)),
                         r=[wg_, xsT], w=[bG], signal=(kc == 7))
                for kc in range(8):
                    S.pe(lambda te: te.matmul(bU[:, 0:CAP], lhsT=wu_[:, kc, fc * P:(fc + 1) * P], rhs=xsT[:, kc, :], start=(kc == 0), stop=(kc == 7)),
                         r=[wu_, xsT], w=[bU], signal=(kc == 7))
                sg_ = sg[fc % 2]
                S.act(lambda a: a.activation(out=sg_[:], in_=bG[:, 0:CAP], func=AF.Silu), r=[bG], w=[sg_])
                S.dve(lambda v: v.tensor_tensor(out=AT[:, fc, :], in0=bU[:, 0:CAP], in1=sg_[:], op=ALU.mult), r=[bU, sg_], w=[AT])
            if e + 1 < NE:
                xs_transposes(e + 1)
            for rt_ in range(CAP // P):
                yb_ = ysb[rt_ % 2]
                for half in range(2):
                    bY = bank()
                    for fc in range(4):
                        S.pe(lambda te: te.matmul(bY[:, :], lhsT=AT[:, fc, rt_ * P:(rt_ + 1) * P], rhs=wd_[:, fc, half * 512:(half + 1) * 512],
                                                  start=(fc == 0), stop=(fc == 3)), r=[AT, wd_], w=[bY], signal=(fc == 3))
                    if half == 0:
                        S.act(lambda a: a.activation(out=yb_[:, 0:512], in_=bY[:, :], func=AF.Copy), r=[bY], w=[yb_])
                    else:
                        S.dve(lambda v: v.tensor_copy(out=yb_[:, 512:D], in_=bY[:, :]), r=[bY], w=[yb_])
                r0 = e * CAP + rt_ * P
                S.dma("sync", k.ys[r0:r0 + P, :], yb_[:], r=[yb_])


def combine(k, l, n_tiles, mod_row, final):
    nc, S, sb, cst, bank = k.nc, k.S, k.sb, k.cst, k.bank
    with contextlib.ExitStack() as st:
        G2 = [sb(st, "c_G2_%d" % i, [P, D], F32) for i in range(2)]
        for which in range(2):
            load_row_bc(k, G2[which], mod_row(which, 5))
        fg = None
        if final:
            fg = sb(st, "c_fg", [P, D], F32)
            load_row_bc(k, fg, k.final_g[0:1, :])
        y1 = [sb(st, "c_y1_%d" % i, [P, D], BF16) for i in range(2)]
        y2 = [sb(st, "c_y2_%d" % i, [P, D], BF16) for i in range(2)]
        xt = [sb(st, "c_xt%d" % i, [P, D], F32) for i in range(2)]
        f2 = [sb(st, "c_f%d" % i, [P, D], F32) for i in range(2)]
        xn = [sb(st, "c_xn%d" % i, [P, D], F32) for i in range(2)]
        ss = sb(st, "c_ss", [P, 1], F32)
        t1 = sb(st, "c_t1", [P, 1], F32)
        rstd = sb(st, "c_rstd", [P, 1], F32)
        for t in range(n_tiles):
            which = 0 if t < NTL else 1
            a, b2, x_, xn_ = y1[t % 2], y2[t % 2], xt[t % 2], xn[t % 2]
            f = f2[t % 2]
            S.idma(out=a[:, :], out_offset=None, in_=k.ys[:, :], in_offset=bass.IndirectOffsetOnAxis(ap=k.destg[:, t, 0:1], axis=0),
                   bounds=XS_ROWS + P - 1, r=[k.destg], w=[a])
            S.idma(out=b2[:, :], out_offset=None, in_=k.ys[:, :], in_offset=bass.IndirectOffsetOnAxis(ap=k.destg[:, t, 1:2], axis=0),
                   bounds=XS_ROWS + P - 1, r=[k.destg], w=[b2])
            S.dma("sync", x_[:], k.xres[t * P:(t + 1) * P, :], w=[x_])
            S.dve(lambda v: v.tensor_scalar(out=f[:], in0=a[:], scalar1=k.wgt[:, t, 0:1], scalar2=None, op0=ALU.mult), r=[a, k.wgt], w=[f])
            S.dve(lambda v: v.scalar_tensor_tensor(out=f[:], in0=b2[:], scalar=k.wgt[:, t, 1:2], in1=f[:], op0=ALU.mult, op1=ALU.add),
                  r=[b2, k.wgt, f], w=[f])
            S.dve(lambda v: v.tensor_tensor(out=f[:], in0=f[:], in1=G2[which][:], op=ALU.mult), r=[f, G2[which]], w=[f])
            S.dve(lambda v: v.tensor_tensor(out=xn_[:], in0=x_[:], in1=f[:], op=ALU.add), r=[x_, f], w=[xn_])
            if not final:
                S.dma("sync", k.xres[t * P:(t + 1) * P, :], xn_[:], r=[xn_])
            else:
                if "xres" in k.debug:
                    S.dma("sync", k.xres[t * P:(t + 1) * P, :], xn_[:], r=[xn_])
                if t >= NTL:
                    continue
                S.act(lambda a_: a_.activation(out=f[:], in_=xn_[:], func=AF.Square, accum_out=ss[:]), r=[xn_, f], w=[f, ss])
                rstd_from_ss(k, ss, t1, rstd, 1.0 / D, EPS)
                S.dve(lambda v: v.scalar_tensor_tensor(out=xn_[:], in0=xn_[:], scalar=rstd[:, 0:1], in1=fg[:], op0=ALU.mult, op1=ALU.mult),
                      r=[xn_, rstd, fg], w=[xn_])
                S.dma("sync", k.out[t * P:(t + 1) * P, :], xn_[:], r=[xn_])


def host_inputs(inputs, b, shared=None):
    f = lambda a: np.ascontiguousarray(np.asarray(a, dtype=np.float32))
    if shared is None:
        shared = {}
        perm = _swap_perm()
        w_in = f(inputs["w_in"])
        cols = []
        for base in (0, 512):
            for h in range(4):
                cols.append(base + h * P + perm)
        cols = np.concatenate(cols)
        shared["w_in"] = w_in
        shared["w_swap"] = np.ascontiguousarray(w_in[:, :, cols])
        shared["w_mod"] = f(inputs["w_mod"])
        shared["b_mod"] = f(inputs["b_mod"])
        shared["norm1_g"] = f(inputs["norm1_g"])
        shared["norm2_g"] = f(inputs["norm2_g"])
        shared["ret_decay"] = f(inputs["ret_decay"]).reshape(DEPTH, 8)
        shared["ret_norm_g"] = f(inputs["ret_norm_g"])
        cw = f(inputs["conv_w"])
        shared["convT"] = np.ascontiguousarray(cw.reshape(DEPTH, 5, 8, P).transpose(0, 3, 2, 1))
        shared["convbT"] = np.ascontiguousarray(f(inputs["conv_b"]).reshape(DEPTH, 8, P).transpose(0, 2, 1))
        shared["gate_b"] = f(inputs["mlstm_gate_b"]).reshape(DEPTH, 16)
        shared["mlstm_norm_g"] = f(inputs["mlstm_norm_g"])
        shared["na_bias"] = _na_bias(f(inputs["na_rpb"]))
        shared["w_branch"] = f(inputs["w_branch"])
        shared["w_out"] = f(inputs["w_out"])
        shared["w_gr"] = np.ascontiguousarray(np.concatenate([f(inputs["w_group"]), f(inputs["w_router"])], axis=-1))
        shared["w_eg"] = f(inputs["w_expert_gate"])
        shared["w_eu"] = f(inputs["w_expert_up"])
        shared["w_ed"] = f(inputs["w_expert_down"])
        shared["final_g"] = f(inputs["final_norm_g"]).reshape(1, D)
        shared["consts"] = CONST_NP
        shared["rope"] = _rope_tables()
        shared["c_ctx"] = f(inputs["c_ctx"])
    m = dict(shared)
    c_ctx = m.pop("c_ctx")
    cT = np.stack([f(inputs["c"])[b].reshape(8, P).T, c_ctx.reshape(8, P).T], axis=-1)
    m["cT"] = np.ascontiguousarray(cT)
    m["x"] = f(inputs["x"])[b]
    m["ctx"] = f(inputs["ctx"])[b]
    return m, shared


_NC_CACHE = {}


def kernel(**inputs):
    if "nc" not in _NC_CACHE:
        _NC_CACHE["nc"] = build()
    nc = _NC_CACHE["nc"]
    in_maps = []
    shared = None
    for b in range(8):
        m, shared = host_inputs(inputs, b, shared)
        in_maps.append(m)
    res = run_bass_kernel_spmd(nc, in_maps, core_ids=list(range(8)))
    return np.stack([np.asarray(r["out"], dtype=np.float32) for r in res.results], axis=0)
```

```python
import contextlib
import numpy as np
import concourse.bass as bass
import concourse.mybir as mybir
from concourse.bass_utils import run_bass_kernel_spmd

F32 = mybir.dt.float32
BF16 = mybir.dt.bfloat16
I32 = mybir.dt.int32
AF = mybir.ActivationFunctionType
ALU = mybir.AluOpType
AX = mybir.AxisListType

P = 128
D = 1024
TL = 2048
TC = 256
T = TL + TC
NT = T // P
NTL = TL // P
DEPTH = 4
IN_COLS = 8720
NE = 32
CAP = 512
XS_ROWS = NE * CAP
EPS = 1e-6
BIG = 1e30
QK_SCALE = 128 ** -0.5
TB = [(0, 512), (512, 512), (1024, 512), (1536, 512), (2048, 256)]
ORDER_F = [16, 17] + list(range(16))
ORDER_B = [17, 16] + list(range(15, -1, -1))


class Buf:
    __slots__ = ("name", "w", "r", "excl")

    def __init__(self, name=""):
        self.name = name
        self.w = None
        self.r = {}
        self.excl = False


class Tile:
    def __init__(self, t, name=""):
        self.t = t
        self.b = Buf(name)

    def __getitem__(self, k):
        return self.t[k]


class Sched:
    EPOCH = 3500
    NDS = 24

    def __init__(self, nc):
        self.nc = nc
        self.sems = {}
        self.eng = {}
        for name in ["tensor", "vector", "scalar", "gpsimd", "sync"]:
            self.eng[name] = dict(obj=getattr(nc, name), cnt=0, ep=0, waited={}, wep={}, name=name)
            self._newsem(name, 0)
        self.dsem = [dict(sem=nc.semaphore("dq%d" % i).__enter__(), cnt=0) for i in range(self.NDS)]
        self.ndma = 0
        self.n_ins = 0
        self.trace = {n: [] for n in self.eng}

    def _newsem(self, name, ep):
        self.sems[(name, ep)] = self.nc.semaphore("s_%s_%d" % (name, ep)).__enter__()

    def _wait(self, e, tok):
        if tok[0] == "e":
            _, name, ep, cnt = tok
            if name == e["name"] and name == "tensor":
                return
            if e["wep"].get(name, -1) > ep:
                return
            key = (name, ep)
            if e["waited"].get(key, 0) >= cnt:
                return
            e["obj"].wait_ge(self.sems[key], cnt)
            self.trace[e["name"]].append(("w", key, cnt))
            e["waited"][key] = cnt
            if ep > e["wep"].get(name, -1):
                e["wep"][name] = ep
        else:
            _, i, cnt = tok
            key = ("d", i)
            if e["waited"].get(key, 0) >= cnt:
                return
            e["obj"].wait_ge(self.dsem[i]["sem"], cnt)
            self.trace[e["name"]].append(("w", key, cnt))
            e["waited"][key] = cnt

    def _deps(self, reads, writes):
        deps = []
        for b in reads:
            if b.w is not None:
                deps.append(b.w)
            if b.excl:
                deps.extend(b.r.values())
        for b in writes:
            if b.w is not None:
                deps.append(b.w)
            deps.extend(b.r.values())
        return deps

    @staticmethod
    def _bufs(xs):
        return [x.b if isinstance(x, Tile) else x for x in xs]

    def _record(self, tok, reads, writes):
        key = tok[:2] if tok[0] == "d" else ("e", tok[1])
        for b in writes:
            b.w = tok
            b.r = {}
        for b in reads:
            b.r[key] = tok

    def op(self, engname, fn, reads=(), writes=(), signal=True):
        e = self.eng[engname]
        reads = self._bufs(reads)
        writes = self._bufs(writes)
        for d in self._deps(reads, writes):
            self._wait(e, d)
        ins = fn(e["obj"])
        if signal:
            e["cnt"] += 1
            ins.then_inc(self.sems[(engname, e["ep"])], 1)
            self.trace[engname].append(("i", (engname, e["ep"]), 1))
            tok = ("e", engname, e["ep"], e["cnt"])
            if e["cnt"] >= self.EPOCH:
                e["ep"] += 1
                e["cnt"] = 0
                self._newsem(engname, e["ep"])
        else:
            tok = ("e", engname, e["ep"], e["cnt"] + 1)
        self._record(tok, reads, writes)
        self.n_ins += 1
        return ins

    def pe(self, fn, r=(), w=(), signal=True):
        return self.op("tensor", fn, r, w, signal)

    def act(self, fn, r=(), w=()):
        return self.op("scalar", fn, r, w)

    def dve(self, fn, r=(), w=()):
        return self.op("vector", fn, r, w)

    def pool(self, fn, r=(), w=()):
        return self.op("gpsimd", fn, r, w)

    def _dma_common(self, engname, issue, reads, writes):
        e = self.eng[engname]
        reads = self._bufs(reads)
        writes = self._bufs(writes)
        for d in self._deps(reads, writes):
            self._wait(e, d)
        i = self.ndma % self.NDS
        self.ndma += 1
        ds = self.dsem[i]
        if ds["cnt"] > 0:
            self._wait(e, ("d", i, ds["cnt"]))
        ins = issue(e["obj"])
        ds["cnt"] += 16
        ins.then_inc(ds["sem"], 16)
        self.trace[engname].append(("i", ("d", i), 16))
        tok = ("d", i, ds["cnt"])
        self._record(tok, reads, writes)
        self.n_ins += 1
        return ins

    def dma(self, engname, out, in_, r=(), w=()):
        return self._dma_common(engname, lambda o: o.dma_start(out=out, in_=in_), r, w)

    def idma(self, out, out_offset, in_, in_offset, bounds, r=(), w=()):
        if not hasattr(self, "_bregs"):
            self._bregs = {}
        if bounds not in self._bregs:
            self._bregs[bounds] = self.nc.gpsimd.to_reg(bounds)
        bounds = self._bregs[bounds]
        return self._dma_common(
            "gpsimd",
            lambda o: o.indirect_dma_start(out=out, out_offset=out_offset, in_=in_, in_offset=in_offset,
                                           bounds_check=bounds, oob_is_err=False), r, w)

    def simulate(self):
        vals = {}
        ptr = {n: 0 for n in self.trace}
        progress = True
        while progress:
            progress = False
            for n, tr in self.trace.items():
                while ptr[n] < len(tr):
                    kind, key, v = tr[ptr[n]]
                    if kind == "w":
                        if vals.get(key, 0) >= v:
                            ptr[n] += 1
                            progress = True
                        else:
                            break
                    else:
                        vals[key] = vals.get(key, 0) + v
                        ptr[n] += 1
                        progress = True
        stuck = {n: (ptr[n], len(tr), tr[ptr[n]] if ptr[n] < len(tr) else None) for n, tr in self.trace.items()}
        return stuck, vals

    def fence(self):
        names = list(self.eng.keys())
        toks = []
        for n in names:
            e = self.eng[n]
            if e["cnt"] > 0:
                toks.append(("e", n, e["ep"], e["cnt"]))
            elif e["ep"] > 0:
                toks.append(("e", n, e["ep"] - 1, self.EPOCH))
        dt = [("d", i, ds["cnt"]) for i, ds in enumerate(self.dsem) if ds["cnt"] > 0]
        for n in names:
            e = self.eng[n]
            for tok in toks:
                if tok[1] == n:
                    continue
                self._wait(e, tok)
            for tok in dt:
                self._wait(e, tok)


def _const_table():
    idx = np.arange(P, dtype=np.float32)
    pj = idx[:, None]
    fi = idx[None, :]
    ent = {}
    ent["ident"] = (pj == fi)
    ent["ones"] = np.ones((P, P))
    ent["trif"] = (pj <= fi)
    ent["trib"] = (pj >= fi)
    ent["tris"] = (pj < fi)
    ent["dpos"] = np.maximum(fi - pj, 0)
    ent["mpos"] = (fi >= pj)
    ent["dneg"] = np.maximum(pj - fi, 0)
    ent["mneg"] = (pj >= fi)
    ent["mij_f"] = np.where(fi <= pj, 0.0, -BIG)
    ent["mij_b"] = np.where(fi >= pj, 0.0, -BIG)
    ent["nji_f"] = np.where(pj <= fi, 0.0, BIG)
    ent["nji_b"] = np.where(pj >= fi, 0.0, BIG)
    ent["row_ip1"] = np.broadcast_to(fi + 1.0, (P, P))
    ent["row_128mi"] = np.broadcast_to(128.0 - fi, (P, P))
    ent["col_127mj"] = 127.0 - pj
    ent["col_j"] = pj + 0.0
    ent["col_128"] = np.full((P, 1), 128.0)
    ent["ecap"] = np.broadcast_to(np.arange(NE, dtype=np.float32)[None, :] * CAP, (P, NE))
    off = {}
    cols = []
    o = 0
    for k, v in ent.items():
        v = np.asarray(v, dtype=np.float32)
        off[k] = (o, v.shape[1])
        o += v.shape[1]
        cols.append(v)
    return off, np.ascontiguousarray(np.concatenate(cols, axis=1), dtype=np.float32)


COFF, CONST_NP = _const_table()
NCONST = CONST_NP.shape[1]


def _rope_tables():
    t = np.arange(TL)
    rows = (t // 64).astype(np.float32)
    colsg = (t % 64).astype(np.float32)
    inv_freq = (10000.0 ** (-np.arange(32, dtype=np.float32) / 32)).astype(np.float32)
    p = np.arange(P)
    axis = p // 64
    half = (p % 64) // 32
    f = p % 32
    pos = np.where(axis[:, None] == 0, rows[None, :], colsg[None, :]).astype(np.float32)
    ang = (pos * inv_freq[f][:, None]).astype(np.float32)
    cos = np.cos(ang)
    sin = np.sin(ang) * np.where(half == 0, -1.0, 1.0)[:, None]
    return np.ascontiguousarray(np.stack([cos, sin], axis=0), dtype=np.float32)


def _swap_perm():
    p = np.arange(P)
    axis = p // 64
    half = (p % 64) // 32
    f = p % 32
    return axis * 64 + (1 - half) * 32 + f


def _na_bias(rpb):
    L, H = rpb.shape[0], rpb.shape[1]
    qc = np.arange(64)
    kc = np.arange(64)
    cs = np.clip(qc - 8, 0, 48)
    col_ok = (kc[None, :] >= cs[:, None]) & (kc[None, :] < cs[:, None] + 16)
    cidx = np.clip(kc[None, :] - qc[:, None] + 15, 0, 30)
    out = np.full((L, P, H, 19, 64), -BIG, dtype=np.float32)

    def fill(tau, delta, valid):
        for w2 in range(2):
            if not valid[w2]:
                continue
            offr = delta + w2 + 7
            if offr < 0 or offr > 14:
                continue
            vals = rpb[:, :, offr, :][:, :, cidx]
            vals = np.where(col_ok[None, None], vals, -BIG)
            out[:, w2 * 64:(w2 + 1) * 64, :, tau, :] = np.transpose(vals, (0, 3, 1, 2))

    for delta in range(-7, 7):
        fill(delta + 7, delta, (True, True))
    fill(14, -5, (False, True))
    fill(15, -3, (True, True))
    fill(16, -1, (True, True))
    fill(17, 1, (True, True))
    fill(18, 3, (True, False))
    return out


class K:
    pass


def bc_mid(ap2d, n):
    a = [list(x) for x in ap2d.ap]
    return bass.AP(ap2d.tensor, ap2d.offset, [a[0], [0, n], a[1]])


def build(n_layers=DEPTH, debug=None, stop_after=None, wl=DEPTH):
    nc = bass.Bass("TRN2", target_bir_lowering=False)
    k = K()
    k.nc = nc
    k.debug = debug or set()
    k.stop_after = stop_after

    def din(name, shape, dt=F32):
        shape = [wl if (i == 0 and d == DEPTH and name not in ('cT',)) else d for i, d in enumerate(shape)]
        return nc.dram_tensor(name, list(shape), dt, kind="ExternalInput").ap()

    def dscr(name, shape, dt):
        kind = "ExternalOutput" if name in k.debug else "Internal"
        return nc.dram_tensor(name, list(shape), dt, kind=kind).ap()

    k.x = din("x", [TL, D])
    k.ctx = din("ctx", [TC, D])
    k.cT = din("cT", [P, 8, 2])
    k.w_mod = din("w_mod", [DEPTH, D, 6 * D])
    k.b_mod = din("b_mod", [DEPTH, 6 * D])
    k.norm1_g = din("norm1_g", [DEPTH, D])
    k.norm2_g = din("norm2_g", [DEPTH, D])
    k.w_in = din("w_in", [DEPTH, D, IN_COLS])
    k.w_swap = din("w_swap", [DEPTH, D, 1024])
    k.ret_decay = din("ret_decay", [DEPTH, 8])
    k.ret_norm_g = din("ret_norm_g", [DEPTH, 512])
    k.convT = din("convT", [DEPTH, P, 8, 5])
    k.convbT = din("convbT", [DEPTH, P, 8])
    k.gate_b = din("gate_b", [DEPTH, 16])
    k.mlstm_norm_g = din("mlstm_norm_g", [DEPTH, 512])
    k.na_bias = din("na_bias", [DEPTH, P, 4, 19, 64])
    k.w_branch = din("w_branch", [DEPTH, 3, 512, D])
    k.w_out = din("w_out", [DEPTH, D, D])
    k.w_gr = din("w_gr", [DEPTH, D, 36])
    k.w_eg = din("w_eg", [DEPTH, NE, D, 512])
    k.w_eu = din("w_eu", [DEPTH, NE, D, 512])
    k.w_ed = din("w_ed", [DEPTH, NE, 512, D])
    k.final_g = din("final_g", [1, D])
    k.consts_d = din("consts", [P, NCONST])
    k.rope_d = din("rope", [2, P, TL])
    k.out = nc.dram_tensor("out", [TL, D], F32, kind="ExternalOutput").ap()

    k.xres = dscr("xres", [T, D], F32)
    k.modd = dscr("modd", [2, 6, D], F32)
    k.YT = dscr("YT", [12, P, T], BF16)
    k.xs = dscr("xs", [XS_ROWS, D], BF16)
    k.ys = dscr("ys", [XS_ROWS + P, D], BF16)
    k.dbg_hT = dscr("dbg_hT", [P, 8, T], BF16) if "dbg_hT" in k.debug else None
    k.dbg_route = dscr("dbg_route", [P, NT, 6], F32) if "dbg_route" in k.debug else None

    S = Sched(nc)
    k.S = S
    top = contextlib.ExitStack()

    k.uid = 0

    def sb(stack, name, shape, dt):
        k.uid += 1
        nm = "sb%d_%s" % (k.uid, name)
        return Tile(stack.enter_context(nc.sbuf_tensor(nm, list(shape), dt)), nm)

    k.sb = sb
    k.banks = [Tile(top.enter_context(nc.psum_tensor("bank%d" % i, [P, 512], F32)), "bank%d" % i) for i in range(8)]
    for b_ in k.banks:
        b_.b.excl = True
    k.bank_i = 0

    def bank():
        b = k.banks[k.bank_i % 7]
        k.bank_i += 1
        return b

    k.bank = bank
    k.rbank_i = 0

    def rbank():
        b = k.banks[7]
        k.rbank_i += 1
        return b

    k.rbank = rbank

    k.C = sb(top, "consts", [P, NCONST], F32)
    k.identb = sb(top, "identb", [P, P], BF16)
    k.rope = sb(top, "rope", [P, 2, TL], BF16)
    k.condb = sb(top, "condb", [P, 8, 2], BF16)
    k.dests = sb(top, "dests", [P, NT, 2], I32)
    k.destg = sb(top, "destg", [P, NT, 2], I32)
    k.wgt = sb(top, "wgt", [P, NT, 2], F32)

    def cst(name):
        o, w = COFF[name]
        return k.C[:, o:o + w]

    k.cst = cst

    S.dma("sync", k.C[:], k.consts_d, w=[k.C])
    S.dve(lambda v: v.tensor_copy(out=k.identb[:], in_=cst("ident")), r=[k.C], w=[k.identb])
    S.dma("gpsimd", k.rope[:, 0, :], k.rope_d[0], w=[k.rope])
    S.dma("gpsimd", k.rope[:, 1, :], k.rope_d[1], w=[k.rope])
    with contextlib.ExitStack() as st:
        ct = sb(st, "ct32", [P, 8, 2], F32)
        zt = sb(st, "zt", [P, 8, D], BF16)
        S.dma("sync", ct[:], k.cT, w=[ct])
        S.act(lambda a: a.activation(out=k.condb[:], in_=ct[:], func=AF.Silu), r=[ct], w=[k.condb])
        S.dma("sync", k.xres[0:TL, :], k.x)
        S.dma("sync", k.xres[TL:T, :], k.ctx)
        S.pool(lambda g: g.memset(zt[:], 0.0), w=[zt])
        for i in range(XS_ROWS // 1024):
            S.dma("sync" if i % 2 == 0 else "scalar",
                  k.xs[i * 1024:(i + 1) * 1024, :].rearrange("(p a) d -> p a d", p=P), zt[:], r=[zt])
        S.dma("sync", k.ys[XS_ROWS:XS_ROWS + P, :], zt[:, 0, :], r=[zt])
        S.fence()

    for l in range(n_layers):
        layer(k, l, last=(l == DEPTH - 1), final=(l == n_layers - 1))

    S.fence()
    top.close()
    k.nc_S = S
    nc._mk_sched = S
    return nc


def rstd_from_ss(k, ss, tmp1, rstd, scale, eps):
    S = k.S
    S.dve(lambda v: v.tensor_scalar(out=tmp1[:], in0=ss[:], scalar1=scale, scalar2=eps, op0=ALU.mult, op1=ALU.add),
          r=[ss], w=[tmp1])
    S.act(lambda a: a.activation(out=tmp1[:], in_=tmp1[:], func=AF.Sqrt), r=[tmp1], w=[tmp1])
    S.dve(lambda v: v.reciprocal(out=rstd[:], in_=tmp1[:]), r=[tmp1], w=[rstd])


def load_row_bc(k, tile, src_row_ap, eng="sync"):
    k.S.dma(eng, tile[:], src_row_ap.partition_broadcast(P), w=[tile])


def load_w(k, dst, dst_sl, src2d, col0, n):
    k.S.dma("gpsimd", dst[:, :, dst_sl], src2d.rearrange("(c p) n -> p c n", p=P)[:, :, col0:col0 + n], w=[dst])


def proj_fm(k, ps_ap, psb, wt, wcol0, tok0, ntok, hT):
    for kc in range(8):
        k.S.pe(lambda t: t.matmul(ps_ap, lhsT=wt[:, kc, wcol0:wcol0 + P], rhs=hT[:, kc, tok0:tok0 + ntok],
                                  start=(kc == 0), stop=(kc == 7)), r=[wt, hT], w=[psb], signal=(kc == 7))


def proj_tm(k, ps_ap, psb, wt, wcol0, n, t, hT, last=True):
    for kc in range(8):
        k.S.pe(lambda te: te.matmul(ps_ap, lhsT=hT[:, kc, t * P:(t + 1) * P], rhs=wt[:, kc, wcol0:wcol0 + n],
                                    start=(kc == 0), stop=(kc == 7)), r=[wt, hT], w=[psb], signal=(kc == 7 and last))


def group_norm_out(k, st, src_ap, src_bufs, grow_ap, grow_b, mul_ap, mul_b, yb):
    S = k.S
    S.dve(lambda v: v.bn_stats(out=st["bst"][:], in_=src_ap), r=src_bufs, w=[st["bst"]])
    S.dve(lambda v: v.bn_aggr(out=st["mv"][:], in_=st["bst"][:]), r=[st["bst"]], w=[st["mv"]])
    S.dve(lambda v: v.tensor_scalar(out=st["t1"][:], in0=st["mv"][:, 1:2], scalar1=EPS, scalar2=None, op0=ALU.add),
          r=[st["mv"]], w=[st["t1"]])
    S.act(lambda a: a.activation(out=st["t1"][:], in_=st["t1"][:], func=AF.Sqrt), r=[st["t1"]], w=[st["t1"]])
    S.dve(lambda v: v.reciprocal(out=st["rs"][:], in_=st["t1"][:]), r=[st["t1"]], w=[st["rs"]])
    S.dve(lambda v: v.tensor_scalar(out=st["y"][:], in0=src_ap, scalar1=st["mv"][:, 0:1], scalar2=st["rs"][:, 0:1],
                                    op0=ALU.subtract, op1=ALU.mult), r=src_bufs + [st["mv"], st["rs"]], w=[st["y"]])
    if mul_ap is None:
        S.pool(lambda g: g.tensor_tensor(out=yb[:], in0=st["y"][:], in1=grow_ap, op=ALU.mult),
               r=[st["y"], grow_b], w=[yb])
    else:
        S.pool(lambda g: g.tensor_tensor(out=st["y"][:], in0=st["y"][:], in1=grow_ap, op=ALU.mult),
               r=[st["y"], grow_b], w=[st["y"]])
        S.pool(lambda g: g.tensor_tensor(out=yb[:], in0=st["y"][:], in1=mul_ap, op=ALU.mult),
               r=[st["y"], mul_b], w=[yb])


def bulk_norm_out(k, OB, clist, BST, MV, RSV, grow_ap, grow_b, mulT, ynb, yb, YTs, width=P):
    S = k.S
    for c in clist:
        S.dve(lambda v: v.bn_stats(out=BST[:, c, :], in_=OB[:, c, 0:width]), r=[OB], w=[BST])
    for c in clist:
        S.dve(lambda v: v.bn_aggr(out=MV[:, c, :], in_=BST[:, c, :]), r=[BST], w=[MV])
    S.dve(lambda v: v.tensor_scalar(out=RSV[:], in0=MV[:, :, 1], scalar1=EPS, scalar2=None, op0=ALU.add), r=[MV], w=[RSV])
    S.act(lambda a: a.activation(out=RSV[:], in_=RSV[:], func=AF.Sqrt), r=[RSV], w=[RSV])
    S.dve(lambda v: v.reciprocal(out=RSV[:], in_=RSV[:]), r=[RSV], w=[RSV])
    tro = TransOut(k, YTs)
    for i, c in enumerate(clist):
        y_, yb_ = ynb[i % 2], yb[i % 2]
        S.dve(lambda v: v.tensor_scalar(out=y_[:], in0=OB[:, c, 0:width], scalar1=MV[:, c, 0:1], scalar2=RSV[:, c:c + 1],
                                        op0=ALU.subtract, op1=ALU.mult), r=[OB, MV, RSV], w=[y_])
        if mulT is None:
            S.pool(lambda g: g.tensor_tensor(out=yb_[:], in0=y_[:], in1=grow_ap, op=ALU.mult), r=[y_, grow_b], w=[yb_])
        else:
            S.pool(lambda g: g.tensor_tensor(out=y_[:], in0=y_[:], in1=grow_ap, op=ALU.mult), r=[y_, grow_b], w=[y_])
            S.pool(lambda g: g.tensor_tensor(out=yb_[:], in0=y_[:], in1=mulT[:, c, :], op=ALU.mult), r=[y_, mulT], w=[yb_])
        tro.add(yb_, c)
        yield
    tro.flush()


def run_interleaved(gens):
    alive = list(gens)
    while alive:
        for g in list(alive):
            try:
                next(g)
            except StopIteration:
                alive.remove(g)


class TransOut:
    def __init__(self, k, dst):
        self.k = k
        self.dst = dst
        self.items = []
        self.bank = None

    def add(self, yb, c):
        k = self.k
        if self.bank is None:
            self.bank = k.rbank()
        i = len(self.items)
        pv = self.bank[:].bitcast(BF16)[:, 0:512].rearrange("p (a b) -> p a b", a=4)
        k.S.pe(lambda t: t.transpose(out=pv[:, i, :], in_=yb[:], identity=k.identb[:]), r=[yb, k.identb], w=[self.bank])
        self.items.append(c)
        if len(self.items) == 4 or c in (17, 15):
            self.flush()

    def flush(self):
        k = self.k
        if not self.items:
            return
        n = len(self.items)
        c0 = self.items[0]
        assert self.items == list(range(c0, c0 + n)), self.items
        pv = self.bank[:].bitcast(BF16)[:, 0:n * P]
        bnk = self.bank
        k.S.act(lambda a: a.activation(out=self.dst[:, c0 * P:(c0 + n) * P], in_=pv, func=AF.Copy), r=[bnk], w=[self.dst])
        self.items = []
        self.bank = None


def layer(k, l, last, final):
    nc, S, sb, cst, bank = k.nc, k.S, k.sb, k.cst, k.bank
    ctx_out = not last
    n_tiles_out = NT if ctx_out else NTL

    with contextlib.ExitStack() as st:
        wm = [sb(st, "wm%d" % i, [P, 8, 768], BF16) for i in range(2)]
        brow = sb(st, "brow", [2, 6 * D], F32)
        modrow = sb(st, "modrow", [2, 6 * D], F32)
        g1row = sb(st, "g1row", [2, D], F32)
        g2row = sb(st, "g2row", [2, D], F32)
        drow = sb(st, "drow", [2, 6, D], F32)
        S.dma("sync", brow[:], k.b_mod[l:l + 1, :].partition_broadcast(2), w=[brow])
        S.dma("sync", g1row[:], k.norm1_g[l:l + 1, :].partition_broadcast(2), w=[g1row])
        S.dma("sync", g2row[:], k.norm2_g[l:l + 1, :].partition_broadcast(2), w=[g2row])
        wsrc = k.w_mod[l].rearrange("(c p) n -> p c n", p=P)
        for blk in range(8):
            w = wm[blk % 2]
            S.dma("gpsimd", w[:], wsrc[:, :, blk * 768:(blk + 1) * 768], w=[w])
            for half in range(2):
                b = bank()
                c0 = blk * 768 + half * 384
                for kc in range(8):
                    S.pe(lambda t: t.matmul(b[0:2, 0:384], lhsT=k.condb[:, kc, :], rhs=w[:, kc, half * 384:(half + 1) * 384],
                                            start=(kc == 0), stop=(kc == 7)), r=[k.condb, w], w=[b], signal=(kc == 7))
                S.dve(lambda v: v.tensor_tensor(out=modrow[:, c0:c0 + 384], in0=b[0:2, 0:384], in1=brow[:, c0:c0 + 384],
                                                op=ALU.add), r=[b, brow], w=[modrow])
        S.dve(lambda v: v.scalar_tensor_tensor(out=drow[:, 0, :], in0=modrow[:, D:2 * D], scalar=1.0, in1=g1row[:],
                                               op0=ALU.add, op1=ALU.mult), r=[modrow, g1row], w=[drow])
        S.dve(lambda v: v.tensor_copy(out=drow[:, 1, :], in_=modrow[:, 0:D]), r=[modrow], w=[drow])
        S.dve(lambda v: v.tensor_copy(out=drow[:, 2, :], in_=modrow[:, 2 * D:3 * D]), r=[modrow], w=[drow])
        S.dve(lambda v: v.scalar_tensor_tensor(out=drow[:, 3, :], in0=modrow[:, 4 * D:5 * D], scalar=1.0, in1=g2row[:],
                                               op0=ALU.add, op1=ALU.mult), r=[modrow, g2row], w=[drow])
        S.dve(lambda v: v.tensor_copy(out=drow[:, 4, :], in_=modrow[:, 3 * D:4 * D]), r=[modrow], w=[drow])
        S.dve(lambda v: v.tensor_copy(out=drow[:, 5, :], in_=modrow[:, 5 * D:6 * D]), r=[modrow], w=[drow])
        S.dma("sync", k.modd, drow[:], r=[drow])
        S.fence()

    def mod_row(which, j):
        return k.modd[which:which + 1, j, :]

    with contextlib.ExitStack() as st_h:
        hT = sb(st_h, "hT", [P, 8, T], BF16)
        with contextlib.ExitStack() as st:
            rows = {}
            for which in range(2):
                for j, nm in ((0, "A"), (1, "B")):
                    rows[(which, nm)] = sb(st, "row%s%d" % (nm, which), [P, D], F32)
                    load_row_bc(k, rows[(which, nm)], mod_row(which, j))
            xt = [sb(st, "xt%d" % i, [P, D], F32) for i in range(2)]
            tmp2 = [sb(st, "ntmp%d" % i, [P, D], F32) for i in range(2)]
            hb = [sb(st, "hb%d" % i, [P, D], BF16) for i in range(2)]
            ss2 = [sb(st, "ss%d" % i, [P, 1], F32) for i in range(2)]
            t12 = [sb(st, "nt1%d" % i, [P, 1], F32) for i in range(2)]
            rstd2 = [sb(st, "rstd%d" % i, [P, 1], F32) for i in range(2)]
            def n1_a(t):
                x_ = xt[t % 2]
                tmp, ss, t1, rstd = tmp2[t % 2], ss2[t % 2], t12[t % 2], rstd2[t % 2]
                S.dma("sync", x_[:], k.xres[t * P:(t + 1) * P, :], w=[x_])
                S.act(lambda a: a.activation(out=tmp[:], in_=x_[:], func=AF.Square, accum_out=ss[:]), r=[x_], w=[tmp, ss])
                rstd_from_ss(k, ss, t1, rstd, 1.0 / D, EPS)

            def n1_b(t):
                which = 0 if t < NTL else 1
                x_, h_ = xt[t % 2], hb[t % 2]
                tmp, rstd = tmp2[t % 2], rstd2[t % 2]
                A, B = rows[(which, "A")], rows[(which, "B")]
                S.dve(lambda v: v.scalar_tensor_tensor(out=tmp[:], in0=x_[:], scalar=rstd[:, 0:1], in1=A[:],
                                                       op0=ALU.mult, op1=ALU.mult), r=[x_, rstd, A], w=[tmp])
                S.dve(lambda v: v.tensor_tensor(out=h_[:], in0=tmp[:], in1=B[:], op=ALU.add), r=[tmp, B], w=[h_])
                b = bank()
                pv = b[:].bitcast(BF16).rearrange("p (a b) -> p a b", a=8)
                for c in range(8):
                    S.pe(lambda te: te.transpose(out=pv[:, c, :], in_=h_[:, c * P:(c + 1) * P], identity=k.identb[:]),
                         r=[h_, k.identb], w=[b], signal=(c == 7))
                S.act(lambda a: a.activation(out=hT[:, :, t * P:(t + 1) * P], in_=pv, func=AF.Copy), r=[b], w=[hT])

            n1_a(0)
            for t in range(NT):
                if t + 1 < NT:
                    n1_a(t + 1)
                n1_b(t)
            if k.dbg_hT is not None:
                S.dma("sync", k.dbg_hT, hT[:], r=[hT])
            S.fence()

        if k.stop_after == "norm1":
            st_h.close()
            return
        if "skip_ret" not in k.debug:
            retention(k, l, hT, ctx_out)
            S.fence()
        if k.stop_after == "ret":
            st_h.close()
            return
        if "skip_ml" not in k.debug:
            mlstm(k, l, hT, ctx_out)
            S.fence()
        if k.stop_after == "ml":
            st_h.close()
            return
        if "skip_na" not in k.debug:
            natten(k, l, hT, ctx_out)
            S.fence()
        if k.stop_after == "na":
            st_h.close()
            return

        with contextlib.ExitStack() as st_m:
            mergedT = sb(st_m, "mergedT", [P, 8, T], BF16)
            merge1(k, l, hT, mergedT, ctx_out)
            S.fence()
            merge2(k, l, mergedT, n_tiles_out, mod_row)
    if k.stop_after == "merge2":
        return
    if "skip_moe" not in k.debug:
        moe(k, l)
        S.fence()
    if k.stop_after == "moe":
        return
    combine(k, l, n_tiles_out, mod_row, final)
    S.fence()


def retention(k, l, hT, ctx_out):
    nc, S, sb, cst, bank = k.nc, k.S, k.sb, k.cst, k.bank
    with contextlib.ExitStack() as st:
        rd = sb(st, "rd", [P, 8], F32)
        lg = sb(st, "lg", [P, 8], F32)
        RT = sb(st, "RT", [P, 4, 3, P], F32)
        RC = sb(st, "RC", [P, 4, 4], F32)
        rt1 = sb(st, "rt1", [P, P], F32)
        rt2 = sb(st, "rt2", [P, P], F32)
        rng = sb(st, "rng", [P, 512], F32)
        load_row_bc(k, rd, k.ret_decay[l:l + 1, :])
        load_row_bc(k, rng, k.ret_norm_g[l:l + 1, :])
        S.act(lambda a: a.activation(out=lg[:], in_=rd[:], func=AF.Exp, scale=-1.0), r=[rd], w=[lg])
        S.act(lambda a: a.activation(out=lg[:], in_=lg[:], func=AF.Ln, bias=1.0), r=[lg], w=[lg])
        S.dve(lambda v: v.tensor_scalar(out=lg[:], in0=lg[:], scalar1=-1.0, scalar2=None, op0=ALU.mult), r=[lg], w=[lg])
        for h in range(4):
            lf, lb = lg[:, h:h + 1], lg[:, 4 + h:5 + h]
            S.act(lambda a: a.activation(out=rt1[:], in_=cst("dpos"), func=AF.Exp, scale=lf), r=[k.C, lg], w=[rt1])
            S.dve(lambda v: v.tensor_tensor(out=rt1[:], in0=rt1[:], in1=cst("mpos"), op=ALU.mult), r=[rt1, k.C], w=[rt1])
            S.act(lambda a: a.activation(out=rt2[:], in_=cst("dneg"), func=AF.Exp, scale=lb), r=[k.C, lg], w=[rt2])
            S.dve(lambda v: v.tensor_tensor(out=rt2[:], in0=rt2[:], in1=cst("mneg"), op=ALU.mult), r=[rt2, k.C], w=[rt2])
            S.dve(lambda v: v.tensor_tensor(out=RT[:, h, 0, :], in0=rt1[:], in1=rt2[:], op=ALU.add), r=[rt1, rt2], w=[RT])
            S.act(lambda a: a.activation(out=RT[:, h, 1, :], in_=cst("row_ip1"), func=AF.Exp, scale=lf), r=[k.C, lg], w=[RT])
            S.act(lambda a: a.activation(out=RT[:, h, 2, :], in_=cst("row_128mi"), func=AF.Exp, scale=lb), r=[k.C, lg], w=[RT])
            S.act(lambda a: a.activation(out=RC[:, h, 0:1], in_=cst("col_127mj"), func=AF.Exp, scale=lf), r=[k.C, lg], w=[RC])
            S.act(lambda a: a.activation(out=RC[:, h, 1:2], in_=cst("col_j"), func=AF.Exp, scale=lb), r=[k.C, lg], w=[RC])
            S.act(lambda a: a.activation(out=RC[:, h, 2:3], in_=cst("col_128"), func=AF.Exp, scale=lf), r=[k.C, lg], w=[RC])
            S.act(lambda a: a.activation(out=RC[:, h, 3:4], in_=cst("col_128"), func=AF.Exp, scale=lb), r=[k.C, lg], w=[RC])

        W6 = [sb(st, "W6_%d" % i, [P, 8, 6 * P], BF16) for i in range(2)]
        qT2 = [sb(st, "r_qT%d" % i, [P, T], BF16) for i in range(2)]
        kT2 = [sb(st, "r_kT%d" % i, [P, T], BF16) for i in range(2)]
        kTM2 = [sb(st, "r_kTM%d" % i, [P, NT, P], BF16) for i in range(2)]
        vTM2 = [sb(st, "r_vTM%d" % i, [P, NT, P], BF16) for i in range(2)]
        gTM2 = [sb(st, "r_gTM%d" % i, [P, NT, P], BF16) for i in range(2)]
        SbAll = sb(st, "r_SbAll", [P, NT, P], BF16)
        YTs = sb(st, "r_YTs", [P, T], BF16)
        ra = sb(st, "r_ra", [P, 512], F32)
        rb = sb(st, "r_rb", [P, 512], F32)
        S32 = sb(st, "r_S32", [P, P], F32)
        Sf = [sb(st, "r_Sf%d" % i, [P, P], BF16) for i in range(2)]
        ks = [sb(st, "r_ks%d" % i, [P, P], BF16) for i in range(2)]
        PT3 = [sb(st, "r_PT%d" % i, [P, P], BF16) for i in range(3)]
        qf3 = [sb(st, "r_qf%d" % i, [P, P], BF16) for i in range(3)]
        qb3 = [sb(st, "r_qb%d" % i, [P, P], BF16) for i in range(3)]
        yb = [sb(st, "r_yb%d" % i, [P, P], BF16) for i in range(2)]
        ynb = [sb(st, "r_yn%d" % i, [P, P], F32) for i in range(2)]
        OB = sb(st, "r_OB", [P, NT, P], F32)
        BST = sb(st, "r_BST", [P, NT, 6], F32)
        MV = sb(st, "r_MV", [P, NT, 2], F32)
        RSV = sb(st, "r_RSV", [P, NT], F32)

        def load_head(h):
            w = W6[h % 2]
            wi = k.w_in[l]
            ws = k.w_swap[l]
            load_w(k, w, slice(0, P), wi, h * P, P)
            load_w(k, w, slice(P, 2 * P), ws, h * P, P)
            load_w(k, w, slice(2 * P, 3 * P), wi, 512 + h * P, P)
            load_w(k, w, slice(3 * P, 4 * P), ws, 512 + h * P, P)
            load_w(k, w, slice(4 * P, 5 * P), wi, 1024 + h * P, P)
            load_w(k, w, slice(5 * P, 6 * P), wi, 1536 + h * P, P)

        def proj(h):
            w = W6[h % 2]
            qT, kT, kTM, vTM, gTM = qT2[h % 2], kT2[h % 2], kTM2[h % 2], vTM2[h % 2], gTM2[h % 2]
            for (dst, c0, sc) in ((qT, 0, 1.0), (kT, 2 * P, QK_SCALE)):
                for (tok0, n) in TB:
                    bA = bank()
                    proj_fm(k, bA[:, 0:n], bA, w, c0, tok0, n, hT)
                    if tok0 < TL:
                        bB = bank()
                        proj_fm(k, bB[:, 0:n], bB, w, c0 + P, tok0, n, hT)
                        S.dve(lambda v: v.scalar_tensor_tensor(out=ra[:, 0:n], in0=bA[:, 0:n], scalar=sc,
                                                               in1=k.rope[:, 0, tok0:tok0 + n], op0=ALU.mult, op1=ALU.mult),
                              r=[bA, k.rope], w=[ra])
                        S.dve(lambda v: v.scalar_tensor_tensor(out=rb[:, 0:n], in0=bB[:, 0:n], scalar=sc,
                                                               in1=k.rope[:, 1, tok0:tok0 + n], op0=ALU.mult, op1=ALU.mult),
                              r=[bB, k.rope], w=[rb])
                        S.pool(lambda g: g.tensor_tensor(out=dst[:, tok0:tok0 + n], in0=ra[:, 0:n], in1=rb[:, 0:n], op=ALU.add),
                               r=[ra, rb], w=[dst])
                    else:
                        S.act(lambda a: a.activation(out=dst[:, tok0:tok0 + n], in_=bA[:, 0:n], func=AF.Copy, scale=sc),
                              r=[bA], w=[dst])
                    yield
            for c0 in range(0, NT, 4):
                n = min(4, NT - c0)
                b = bank()
                pv = b[:].bitcast(BF16)[:, 0:512].rearrange("p (a b) -> p a b", a=4)
                for i in range(n):
                    c = c0 + i
                    S.pe(lambda t: t.transpose(out=pv[:, i, :], in_=kT[:, c * P:(c + 1) * P], identity=k.identb[:]),
                         r=[kT, k.identb], w=[b], signal=(i == n - 1))
                S.dve(lambda v: v.tensor_copy(out=kTM[:, c0:c0 + n, :], in_=pv[:, 0:n, :]), r=[b], w=[kTM])
                yield
            for c0 in range(0, NT, 2):
                bv = bank()
                pvv = bv[:].rearrange("p (a b) -> p a b", a=2)
                for i in range(2):
                    proj_tm(k, pvv[:, i, :], bv, w, 4 * P, 2 * P, c0 + i, hT, last=(i == 1))
                S.dve(lambda v: v.tensor_copy(out=vTM[:, c0:c0 + 2, :], in_=pvv[:, :, 0:P]), r=[bv], w=[vTM])
                S.act(lambda a: a.activation(out=gTM[:, c0:c0 + 2, :], in_=pvv[:, :, P:2 * P], func=AF.Silu), r=[bv], w=[gTM])
                yield

        def scan(h):
            qT, kT, kTM, vTM, gTM = qT2[h % 2], kT2[h % 2], kTM2[h % 2], vTM2[h % 2], gTM2[h % 2]
            S.dve(lambda v: v.memset(S32[:], 0.0), w=[S32])
            for si, c in enumerate(ORDER_B):
                S.act(lambda a: a.activation(out=SbAll[:, c, :], in_=S32[:], func=AF.Copy), r=[S32], w=[SbAll])
                if si == NT - 1:
                    break
                kk = ks[si % 2]
                S.act(lambda a: a.activation(out=kk[:], in_=kTM[:, c, :], func=AF.Identity, scale=RC[:, h, 1:2]), r=[kTM, RC], w=[kk])
                b = bank()
                S.pe(lambda t: t.matmul(b[:, 0:P], lhsT=kk[:], rhs=vTM[:, c, :], start=True, stop=True), r=[kk, vTM], w=[b])
                S.dve(lambda v: v.scalar_tensor_tensor(out=S32[:], in0=S32[:], scalar=RC[:, h, 3:4], in1=b[:, 0:P],
                                                       op0=ALU.mult, op1=ALU.add), r=[S32, RC, b], w=[S32])
                yield
            S.dve(lambda v: v.memset(S32[:], 0.0), w=[S32])
            S.dve(lambda v: v.memset(Sf[0][:], 0.0), w=[Sf[0]])
            GR = 3
            for g0 in range(0, NT, GR):
                steps = list(range(g0, g0 + GR))
                outs = [(si, ORDER_F[si]) for si in steps if (ORDER_F[si] < NTL or ctx_out)]
                bSs = {}
                for si, c in outs:
                    cs = slice(c * P, (c + 1) * P)
                    bSs[si] = bank()
                    S.pe(lambda t: t.matmul(bSs[si][:, 0:P], lhsT=kT[:, cs], rhs=qT[:, cs], start=True, stop=True), r=[kT, qT], w=[bSs[si]])
                for si, c in outs:
                    pt = PT3[si % GR]
                    S.dve(lambda v: v.tensor_tensor(out=pt[:], in0=bSs[si][:, 0:P], in1=RT[:, h, 0, :], op=ALU.mult), r=[bSs[si], RT], w=[pt])
                for si, c in outs:
                    cs = slice(c * P, (c + 1) * P)
                    qf_, qb_ = qf3[si % GR], qb3[si % GR]
                    S.pool(lambda g: g.tensor_tensor(out=qf_[:], in0=qT[:, cs], in1=RT[:, h, 1, :], op=ALU.mult), r=[qT, RT], w=[qf_])
                    S.pool(lambda g: g.tensor_tensor(out=qb_[:], in0=qT[:, cs], in1=RT[:, h, 2, :], op=ALU.mult), r=[qT, RT], w=[qb_])
                yield
                for si in steps:
                    c = ORDER_F[si]
                    sfc = Sf[si % 2]
                    if c < NTL or ctx_out:
                        pt, qf_, qb_ = PT3[si % GR], qf3[si % GR], qb3[si % GR]
                        bO = bank()
                        S.pe(lambda t: t.matmul(bO[:, 0:P], lhsT=pt[:], rhs=vTM[:, c, :], start=True, stop=False), r=[pt, vTM], w=[bO], signal=False)
                        S.pe(lambda t: t.matmul(bO[:, 0:P], lhsT=qf_[:], rhs=sfc[:], start=False, stop=False), r=[qf_, sfc], w=[bO], signal=False)
                        S.pe(lambda t: t.matmul(bO[:, 0:P], lhsT=qb_[:], rhs=SbAll[:, c, :], start=False, stop=True), r=[qb_, SbAll], w=[bO])
                        S.act(lambda a: a.activation(out=OB[:, c, :], in_=bO[:, 0:P], func=AF.Copy), r=[bO], w=[OB])
                    if si == NT - 1:
                        break
                    kk = ks[si % 2]
                    S.act(lambda a: a.activation(out=kk[:], in_=kTM[:, c, :], func=AF.Identity, scale=RC[:, h, 0:1]), r=[kTM, RC], w=[kk])
                    b = bank()
                    S.pe(lambda t: t.matmul(b[:, 0:P], lhsT=kk[:], rhs=vTM[:, c, :], start=True, stop=True), r=[kk, vTM], w=[b])
                    S.dve(lambda v: v.scalar_tensor_tensor(out=S32[:], in0=S32[:], scalar=RC[:, h, 2:3], in1=b[:, 0:P],
                                                           op0=ALU.mult, op1=ALU.add), r=[S32, RC, b], w=[S32])
                    sfn = Sf[(si + 1) % 2]
                    S.act(lambda a: a.activation(out=sfn[:], in_=S32[:], func=AF.Copy), r=[S32], w=[sfn])
                    yield
            clist = ([16, 17] if ctx_out else []) + list(range(NTL))
            yield from bulk_norm_out(k, OB, clist, BST, MV, RSV, rng[:, h * P:(h + 1) * P], rng, gTM, ynb, yb, YTs)
            ncols = T if ctx_out else TL
            S.dma("sync", k.YT[h, :, 0:ncols], YTs[:, 0:ncols], r=[YTs])

        load_head(0)
        load_head(1)
        for _ in proj(0):
            pass
        for h in range(4):
            if h + 2 < 4:
                load_head(h + 2)
            run_interleaved([scan(h)] + ([proj(h + 1)] if h + 1 < 4 else []))


def bc_last(ap2d, n):
    a = [list(x) for x in ap2d.ap]
    return bass.AP(ap2d.tensor, ap2d.offset, [a[0], a[1], [0, n]])


def mlstm(k, l, hT, ctx_out):
    nc, S, sb, cst, bank = k.nc, k.S, k.sb, k.cst, k.bank
    with contextlib.ExitStack() as st:
        wg = sb(st, "m_wg", [P, 8, 16], BF16)
        gb = sb(st, "m_gb", [P, 16], F32)
        G = sb(st, "m_G", [P, NT, 16], F32)
        FB = sb(st, "m_FB", [P, NT, 8], F32)
        CUM = sb(st, "m_CUM", [P, NT, 16], F32)
        A = sb(st, "m_A", [P, NT, 8], F32)
        mng = sb(st, "m_mng", [P, 512], F32)
        cw = sb(st, "m_cw", [P, 8, 5], F32)
        cb = sb(st, "m_cb", [P, 8], F32)
        load_w(k, wg, slice(0, 16), k.w_in[l], 4096, 16)
        load_row_bc(k, gb, k.gate_b[l:l + 1, :])
        load_row_bc(k, mng, k.mlstm_norm_g[l:l + 1, :])
        S.dma("sync", cw[:], k.convT[l], w=[cw])
        S.dma("sync", cb[:], k.convbT[l], w=[cb])
        b = bank()
        pv = b[:, 0:NT * 16].rearrange("p (a b) -> p a b", a=NT)
        for t in range(NT):
            proj_tm(k, pv[:, t, :], b, wg, 0, 16, t, hT, last=(t == NT - 1))
        S.dve(lambda v: v.tensor_tensor(out=G[:], in0=pv, in1=bc_mid(gb[:], NT), op=ALU.add), r=[b, gb], w=[G])
        G4 = G[:].rearrange("p t (a b) -> p t a b", a=2)
        FB4 = FB[:].rearrange("p t (a b) -> p t a b", a=2)
        A4 = A[:].rearrange("p t (a b) -> p t a b", a=2)
        S.act(lambda a: a.activation(out=FB4, in_=G4[:, :, :, 4:8], func=AF.Exp, scale=-1.0), r=[G], w=[FB])
        S.act(lambda a: a.activation(out=FB[:], in_=FB[:], func=AF.Ln, bias=1.0), r=[FB], w=[FB])
        S.dve(lambda v: v.tensor_scalar(out=FB[:], in0=FB[:], scalar1=-1.0, scalar2=None, op0=ALU.mult), r=[FB], w=[FB])
        b = bank()
        pv = b[:, 0:NT * 16].rearrange("p (a b) -> p a b", a=NT)
        for c in range(NT):
            S.pe(lambda t: t.matmul(pv[:, c, 0:4], lhsT=cst("trif"), rhs=FB[:, c, 0:4], start=True, stop=True), r=[k.C, FB], w=[b], signal=False)
            S.pe(lambda t: t.matmul(pv[:, c, 4:8], lhsT=cst("trib"), rhs=FB[:, c, 4:8], start=True, stop=True), r=[k.C, FB], w=[b], signal=False)
            S.pe(lambda t: t.matmul(pv[:, c, 8:16], lhsT=cst("ones"), rhs=FB[:, c, 0:8], start=True, stop=True), r=[k.C, FB], w=[b],
                 signal=(c == NT - 1))
        S.dve(lambda v: v.tensor_copy(out=CUM[:], in_=pv), r=[b], w=[CUM])
        CUM4 = CUM[:, :, 0:8].rearrange("p t (a b) -> p t a b", a=2)
        S.dve(lambda v: v.tensor_tensor(out=A4, in0=G4[:, :, :, 0:4], in1=CUM4, op=ALU.subtract), r=[G, CUM], w=[A])

        names = ["AMAX", "MRAW", "MPREV", "ML", "PP", "FL", "KW", "PW", "U", "T0"]
        Q = {n: sb(st, "m_" + n, [P, NT, 8], F32) for n in names}
        dg4 = [sb(st, "m_dg4_%d" % i, [P, 4, P], F32) for i in range(2)]
        tm4 = [sb(st, "m_tm4_%d" % i, [P, 4, P], F32) for i in range(2)]
        it = 0
        for c in range(NT):
            for r in range(2):
                d4, t4 = dg4[it % 2], tm4[it % 2]
                it += 1
                acols = A[:, c, r * 4:(r + 1) * 4]
                S.dve(lambda v: v.tensor_tensor(out=d4[:], in0=bc_mid(cst("ident"), 4), in1=bc_last(acols, P), op=ALU.mult),
                      r=[k.C, A], w=[d4])
                bA = bank()
                S.pe(lambda t: t.matmul(bA[:, :], lhsT=cst("ones"), rhs=d4[:].rearrange("p a b -> p (a b)"), start=True, stop=True),
                     r=[k.C, d4], w=[bA])
                bA4 = bA[:].rearrange("p (a b) -> p a b", a=4)
                S.dve(lambda v: v.tensor_reduce(out=Q["AMAX"][:, c, r * 4:(r + 1) * 4], in_=bA4, axis=AX.X, op=ALU.max), r=[bA], w=[Q["AMAX"]])
                mij = cst("mij_f" if r == 0 else "mij_b")
                S.dve(lambda v: v.tensor_tensor(out=t4[:], in0=bA4, in1=bc_mid(mij, 4), op=ALU.add), r=[bA, k.C], w=[t4])
                S.dve(lambda v: v.tensor_reduce(out=Q["MRAW"][:, c, r * 4:(r + 1) * 4], in_=t4[:], axis=AX.X, op=ALU.max), r=[t4], w=[Q["MRAW"]])
        S.dve(lambda v: v.memset(Q["MPREV"][:], 0.0), w=[Q["MPREV"]])
        for si in range(NT):
            for r in range(2):
                order = ORDER_F if r == 0 else ORDER_B
                c = order[si]
                cols = slice(r * 4, (r + 1) * 4)
                S.dve(lambda v: v.tensor_tensor(out=Q["ML"][:, c, cols], in0=Q["AMAX"][:, c, cols], in1=Q["MPREV"][:, c, cols], op=ALU.max),
                      r=[Q["AMAX"], Q["MPREV"]], w=[Q["ML"]])
                if si + 1 < NT:
                    cn = order[si + 1]
                    S.dve(lambda v: v.tensor_tensor(out=Q["MPREV"][:, cn, cols], in0=CUM[:, c, 8 + r * 4:12 + r * 4], in1=Q["ML"][:, c, cols], op=ALU.add),
                          r=[CUM, Q["ML"]], w=[Q["MPREV"]])
        T0 = Q["T0"]
        S.dve(lambda v: v.tensor_tensor(out=T0[:], in0=Q["MRAW"][:], in1=Q["MPREV"][:], op=ALU.subtract), r=[Q["MRAW"], Q["MPREV"]], w=[T0])
        S.dve(lambda v: v.tensor_scalar(out=T0[:], in0=T0[:], scalar1=0.0, scalar2=None, op0=ALU.max), r=[T0], w=[T0])
        S.act(lambda a: a.activation(out=Q["PP"][:], in_=T0[:], func=AF.Exp, scale=-1.0), r=[T0], w=[Q["PP"]])
        S.dve(lambda v: v.tensor_tensor(out=T0[:], in0=Q["MRAW"][:], in1=Q["MPREV"][:], op=ALU.max), r=[Q["MRAW"], Q["MPREV"], Q["PP"]], w=[T0])
        S.dve(lambda v: v.tensor_tensor(out=T0[:], in0=T0[:], in1=CUM[:, :, 0:8], op=ALU.add), r=[T0, CUM], w=[T0])
        S.act(lambda a: a.activation(out=Q["FL"][:], in_=T0[:], func=AF.Exp, scale=-1.0), r=[T0], w=[Q["FL"]])
        S.dve(lambda v: v.tensor_tensor(out=T0[:], in0=A[:], in1=Q["ML"][:], op=ALU.subtract), r=[A, Q["ML"], Q["FL"]], w=[T0])
        S.act(lambda a: a.activation(out=Q["KW"][:], in_=T0[:], func=AF.Exp), r=[T0], w=[Q["KW"]])
        S.dve(lambda v: v.tensor_tensor(out=T0[:], in0=Q["MPREV"][:], in1=Q["ML"][:], op=ALU.subtract), r=[Q["MPREV"], Q["ML"], Q["KW"]], w=[T0])
        S.act(lambda a: a.activation(out=Q["PW"][:], in_=T0[:], func=AF.Exp), r=[T0], w=[Q["PW"]])
        S.dve(lambda v: v.tensor_tensor(out=T0[:], in0=A[:], in1=Q["MPREV"][:], op=ALU.subtract), r=[A, Q["MPREV"], Q["PW"]], w=[T0])
        S.act(lambda a: a.activation(out=Q["U"][:], in_=T0[:], func=AF.Exp), r=[T0], w=[Q["U"]])

        W4 = [sb(st, "m_W4_%d" % i, [P, 8, 4 * P], BF16) for i in range(2)]
        u = sb(st, "m_u", [P, T], F32)
        acc = sb(st, "m_acc", [P, T], F32)
        qT2 = [sb(st, "m_qT%d" % i, [P, T], BF16) for i in range(2)]
        kT2 = [sb(st, "m_kT%d" % i, [P, T], BF16) for i in range(2)]
        kTM2 = [sb(st, "m_kTM%d" % i, [P, NT, P], BF16) for i in range(2)]
        vaug2 = [sb(st, "m_vaug%d" % i, [P, NT, P + 1], BF16) for i in range(2)]
        oTM2 = [sb(st, "m_oTM%d" % i, [P, NT, P], BF16) for i in range(2)]
        HN = [sb(st, "m_HN%d" % i, [P, NT, P + 1], F32) for i in range(2)]
        YTs = sb(st, "m_YTs", [P, T], BF16)
        yb = [sb(st, "m_yb%d" % i, [P, P], BF16) for i in range(2)]
        ynb = [sb(st, "m_yn%d" % i, [P, P], F32) for i in range(2)]
        BST = sb(st, "m_BST", [P, NT, 6], F32)
        MV = sb(st, "m_MV", [P, NT, 2], F32)
        RSV = sb(st, "m_RSV", [P, NT], F32)
        RCP = sb(st, "m_RCP", [P, 2, NT], F32)
        GROUP = 3
        NU = 2 * GROUP
        NSL = 2 * GROUP
        D_ = []
        for r in range(2):
            d = dict(
                CN32=sb(st, "m_CN32_%d" % r, [P, P + 1], F32),
                CNbf=[sb(st, "m_CNbf%d_%d" % (r, i), [P, P + 1], BF16) for i in range(2)],
                S1=[sb(st, "m_S1_%d_%d" % (r, i), [P, P + 1], F32) for i in range(NSL)],
                KV=[sb(st, "m_KV_%d_%d" % (r, i), [P, P + 1], F32) for i in range(NSL)],
                )
            D_.append(d)
        UB = dict(diagM=[sb(st, "m_udM%d" % i, [P, P], F32) for i in range(NU)],
                  W0=[sb(st, "m_uW0%d" % i, [P, P], F32) for i in range(NU)],
                  PT=[sb(st, "m_uPT%d" % i, [P, P], BF16) for i in range(NU)],
                  ks=[sb(st, "m_uks%d" % i, [P, P], BF16) for i in range(NU)])
        for vv_ in vaug2:
            S.pool(lambda g: g.memset(vv_[:], 1.0), w=[vv_])

        def load_head(h):
            w = W4[h % 2]
            wi = k.w_in[l]
            for j in range(4):
                load_w(k, w, slice(j * P, (j + 1) * P), wi, 2048 + j * 512 + h * P, P)

        def proj(h):
            w = W4[h % 2]
            qT, kT, kTM, vaug, oTM = qT2[h % 2], kT2[h % 2], kTM2[h % 2], vaug2[h % 2], oTM2[h % 2]
            for (dst, c0, blk, sc) in ((qT, 0, h, 1.0), (kT, P, 4 + h, QK_SCALE)):
                for (tok0, n) in TB:
                    bA = bank()
                    proj_fm(k, bA[:, 0:n], bA, w, c0, tok0, n, hT)
                    S.act(lambda a: a.activation(out=u[:, tok0:tok0 + n], in_=bA[:, 0:n], func=AF.Copy), r=[bA], w=[u])
                    yield
                for (s0, n) in ((0, TL), (TL, TC)):
                    S.dve(lambda v: v.tensor_scalar(out=acc[:, s0:s0 + n], in0=u[:, s0:s0 + n], scalar1=cw[:, blk, 2:3], scalar2=None,
                                                    op0=ALU.mult), r=[u, cw], w=[acc])
                    for wv in (0, 1, 3, 4):
                        sh = wv - 2
                        lo = max(0, -sh)
                        hi = n - max(0, sh)
                        S.dve(lambda v: v.scalar_tensor_tensor(out=acc[:, s0 + lo:s0 + hi], in0=u[:, s0 + lo + sh:s0 + hi + sh],
                                                               scalar=cw[:, blk, wv:wv + 1], in1=acc[:, s0 + lo:s0 + hi],
                                                               op0=ALU.mult, op1=ALU.add), r=[u, cw, acc], w=[acc])
                S.act(lambda a: a.activation(out=dst[:], in_=acc[:], func=AF.Silu, bias=cb[:, blk:blk + 1]), r=[acc, cb], w=[dst])
                if sc != 1.0:
                    S.pool(lambda g: g.tensor_scalar(out=dst[:], in0=dst[:], scalar1=sc, scalar2=1.0, op0=ALU.mult, op1=ALU.mult), r=[dst], w=[dst])
                yield
            for c0 in range(0, NT, 4):
                n = min(4, NT - c0)
                b = bank()
                pv = b[:].bitcast(BF16)[:, 0:512].rearrange("p (a b) -> p a b", a=4)
                for i in range(n):
                    c = c0 + i
                    S.pe(lambda t: t.transpose(out=pv[:, i, :], in_=kT[:, c * P:(c + 1) * P], identity=k.identb[:]),
                         r=[kT, k.identb], w=[b], signal=(i == n - 1))
                S.dve(lambda v: v.tensor_copy(out=kTM[:, c0:c0 + n, :], in_=pv[:, 0:n, :]), r=[b], w=[kTM])
                yield
            for c0 in range(0, NT, 2):
                bv = bank()
                pvv = bv[:].rearrange("p (a b) -> p a b", a=2)
                for i in range(2):
                    proj_tm(k, pvv[:, i, :], bv, w, 2 * P, 2 * P, c0 + i, hT, last=(i == 1))
                S.dve(lambda v: v.tensor_copy(out=vaug[:, c0:c0 + 2, 0:P], in_=pvv[:, :, 0:P]), r=[bv], w=[vaug])
                S.act(lambda a: a.activation(out=oTM[:, c0:c0 + 2, :], in_=pvv[:, :, P:2 * P], func=AF.Sigmoid), r=[bv], w=[oTM])
                yield

        def scan(h):
            qT, kT, kTM, vaug, oTM = qT2[h % 2], kT2[h % 2], kTM2[h % 2], vaug2[h % 2], oTM2[h % 2]
            for r in range(2):
                d = D_[r]
                S.dve(lambda v: v.memset(d["CN32"][:], 0.0), w=[d["CN32"]])
                S.dve(lambda v: v.memset(d["CNbf"][0][:], 0.0), w=[d["CNbf"][0]])

            def wbatch(units):
                info = []
                for ui, (si, r) in enumerate(units):
                    c = (ORDER_F if r == 0 else ORDER_B)[si]
                    info.append(dict(ui=ui, si=si, r=r, d=D_[r], c=c, col=r * 4 + h, cs=slice(c * P, (c + 1) * P),
                                     out=((c < NTL) or ctx_out), upd=(si < NT - 1)))
                outs = [x for x in info if x["out"]]
                for x in outs:
                    dM = UB["diagM"][x["ui"]]
                    S.dve(lambda v: v.tensor_scalar(out=dM[:], in0=cst("ident"), scalar1=Q["MRAW"][:, x["c"], x["col"]:x["col"] + 1], scalar2=None,
                                                    op0=ALU.mult), r=[k.C, Q["MRAW"]], w=[dM])
                for x in outs:
                    x["bM"] = bank()
                    dM = UB["diagM"][x["ui"]]
                    S.pe(lambda t: t.matmul(x["bM"][:, 0:P], lhsT=cst("ones"), rhs=dM[:], start=True, stop=True), r=[k.C, dM], w=[x["bM"]])
                for x in outs:
                    W0 = UB["W0"][x["ui"]]
                    nji = cst("nji_f" if x["r"] == 0 else "nji_b")
                    S.dve(lambda v: v.tensor_tensor(out=W0[:], in0=x["bM"][:, 0:P], in1=nji, op=ALU.add), r=[x["bM"], k.C], w=[W0])
                for x in outs:
                    W0 = UB["W0"][x["ui"]]
                    S.act(lambda a: a.activation(out=W0[:], in_=W0[:], func=AF.Exp, scale=-1.0, bias=A[:, x["c"], x["col"]:x["col"] + 1]),
                          r=[W0, A], w=[W0])
                for x in outs:
                    x["bS"] = bank()
                    S.pe(lambda t: t.matmul(x["bS"][:, 0:P], lhsT=kT[:, x["cs"]], rhs=qT[:, x["cs"]], start=True, stop=True), r=[kT, qT], w=[x["bS"]])
                for x in outs:
                    W0, PT = UB["W0"][x["ui"]], UB["PT"][x["ui"]]
                    S.dve(lambda v: v.scalar_tensor_tensor(out=PT[:], in0=W0[:], scalar=Q["U"][:, x["c"], x["col"]:x["col"] + 1], in1=x["bS"][:, 0:P],
                                                           op0=ALU.min, op1=ALU.mult), r=[W0, Q["U"], x["bS"]], w=[PT])
                for x in outs:
                    x["b1"] = bank()
                    PT = UB["PT"][x["ui"]]
                    S.pe(lambda t: t.matmul(x["b1"][:, 0:P + 1], lhsT=PT[:], rhs=vaug[:, x["c"], :], start=True, stop=True), r=[PT, vaug], w=[x["b1"]])
                for x in outs:
                    s1 = x["d"]["S1"][x["si"] % NSL]
                    S.act(lambda a: a.activation(out=s1[:], in_=x["b1"][:, 0:P + 1], func=AF.Copy), r=[x["b1"]], w=[s1])
                ups = [x for x in info if x["upd"]]
                for x in ups:
                    ks = UB["ks"][x["ui"]]
                    S.act(lambda a: a.activation(out=ks[:], in_=kTM[:, x["c"], :], func=AF.Identity, scale=Q["KW"][:, x["c"], x["col"]:x["col"] + 1]),
                          r=[kTM, Q["KW"]], w=[ks])
                for x in ups:
                    x["bC"] = bank()
                    ks = UB["ks"][x["ui"]]
                    S.pe(lambda t: t.matmul(x["bC"][:, 0:P + 1], lhsT=ks[:], rhs=vaug[:, x["c"], :], start=True, stop=True), r=[ks, vaug], w=[x["bC"]])
                for x in ups:
                    kv = x["d"]["KV"][x["si"] % NSL]
                    S.act(lambda a: a.activation(out=kv[:], in_=x["bC"][:, 0:P + 1], func=AF.Copy), r=[x["bC"]], w=[kv])

            def sphase(si, r):
                d = D_[r]
                c = (ORDER_F if r == 0 else ORDER_B)[si]
                col = r * 4 + h
                cs = slice(c * P, (c + 1) * P)
                need_out = (c < NTL) or ctx_out
                cnb = d["CNbf"][si % 2]
                if need_out:
                    s1 = d["S1"][si % NSL]
                    b2 = bank()
                    S.pe(lambda t: t.matmul(b2[:, 0:P + 1], lhsT=qT[:, cs], rhs=cnb[:], start=True, stop=True), r=[qT, cnb], w=[b2])
                    S.dve(lambda v: v.scalar_tensor_tensor(out=HN[r][:, c, :], in0=b2[:, 0:P + 1], scalar=Q["PP"][:, c, col:col + 1], in1=s1[:],
                                                           op0=ALU.mult, op1=ALU.add), r=[b2, Q["PP"], s1], w=[HN[r]])
                if si < NT - 1:
                    kv = d["KV"][si % NSL]
                    S.dve(lambda v: v.scalar_tensor_tensor(out=d["CN32"][:], in0=d["CN32"][:], scalar=Q["PW"][:, c, col:col + 1], in1=kv[:],
                                                           op0=ALU.mult, op1=ALU.add), r=[d["CN32"], Q["PW"], kv], w=[d["CN32"]])
                    cnn = d["CNbf"][(si + 1) % 2]
                    S.act(lambda a: a.activation(out=cnn[:], in_=d["CN32"][:], func=AF.Copy), r=[d["CN32"]], w=[cnn])

            ngroups = NT // GROUP
            for g in range(ngroups + 1):
                if g < ngroups:
                    wbatch([(si, r) for si in range(g * GROUP, (g + 1) * GROUP) for r in range(2)])
                    yield
                if g >= 1:
                    for si in range((g - 1) * GROUP, g * GROUP):
                        for r in range(2):
                            sphase(si, r)
                        yield
            clist = ([16, 17] if ctx_out else []) + list(range(NTL))
            c_lo, c_hi = (0, NT) if ctx_out else (0, NTL)
            for r in range(2):
                col = r * 4 + h
                S.act(lambda a: a.activation(out=RCP[:, r, c_lo:c_hi], in_=HN[r][:, c_lo:c_hi, P], func=AF.Abs), r=[HN[r]], w=[RCP])
                S.dve(lambda v: v.tensor_tensor(out=RCP[:, r, c_lo:c_hi], in0=RCP[:, r, c_lo:c_hi], in1=Q["FL"][:, c_lo:c_hi, col], op=ALU.max),
                      r=[RCP, Q["FL"]], w=[RCP])
                S.dve(lambda v: v.reciprocal(out=RCP[:, r, c_lo:c_hi], in_=RCP[:, r, c_lo:c_hi]), r=[RCP], w=[RCP])
            H0 = HN[0]
            for c in clist:
                S.dve(lambda v: v.tensor_scalar(out=H0[:, c, 0:P], in0=H0[:, c, 0:P], scalar1=RCP[:, 0, c:c + 1], scalar2=None, op0=ALU.mult),
                      r=[H0, RCP], w=[H0])
                S.dve(lambda v: v.scalar_tensor_tensor(out=H0[:, c, 0:P], in0=HN[1][:, c, 0:P], scalar=RCP[:, 1, c:c + 1], in1=H0[:, c, 0:P],
                                                       op0=ALU.mult, op1=ALU.add), r=[HN[1], RCP, H0], w=[H0])
                S.pool(lambda g: g.tensor_tensor(out=H0[:, c, 0:P], in0=H0[:, c, 0:P], in1=oTM[:, c, :], op=ALU.mult), r=[H0, oTM], w=[H0])
                yield
            yield from bulk_norm_out(k, H0, clist, BST, MV, RSV, mng[:, h * P:(h + 1) * P], mng, None, ynb, yb, YTs, width=P)
            ncols = T if ctx_out else TL
            S.dma("sync", k.YT[4 + h, :, 0:ncols], YTs[:, 0:ncols], r=[YTs])

        load_head(0)
        load_head(1)
        for _ in proj(0):
            pass
        for h in range(4):
            if h + 2 < 4:
                load_head(h + 2)
            run_interleaved([scan(h)] + ([proj(h + 1)] if h + 1 < 4 else []))


def natten(k, l, hT, ctx_out):
    nc, S, sb, cst, bank = k.nc, k.S, k.sb, k.cst, k.bank
    with contextlib.ExitStack() as st:
        NB = sb(st, "n_NB", [P, 4, 19, 64], F32)
        S.dma("sync", NB[:], k.na_bias[l], w=[NB])
        W3 = [sb(st, "n_W3_%d" % i, [P, 8, 3 * P], BF16) for i in range(2)]
        qT = sb(st, "n_qT", [P, T], BF16)
        kT = sb(st, "n_kT", [P, T], BF16)
        vaug = sb(st, "n_vaug", [P, NT, P + 1], BF16)
        YTs = sb(st, "n_YTs", [P, T], BF16)
        e_ = [sb(st, "n_e%d" % i, [P, 5, 64], F32) for i in range(3)]
        PT = [sb(st, "n_PT%d" % i, [P, 7, 64], BF16) for i in range(3)]
        PTc = sb(st, "n_PTc", [P, 2, TC], BF16)
        rc = [sb(st, "n_rc%d" % i, [P, 1], F32) for i in range(3)]
        ob = [sb(st, "n_ob%d" % i, [P, P], BF16) for i in range(3)]
        S.pool(lambda g: g.memset(vaug[:], 1.0), w=[vaug])

        def load_head(h):
            w = W3[h % 2]
            for j in range(3):
                load_w(k, w, slice(j * P, (j + 1) * P), k.w_in[l], 4112 + j * 512 + h * P, P)

        load_head(0)
        for h in range(4):
            w = W3[h % 2]
            if h + 1 < 4:
                load_head(h + 1)
            for (dst, c0) in ((qT, 0), (kT, P)):
                for (tok0, n) in TB:
                    bA = bank()
                    proj_fm(k, bA[:, 0:n], bA, w, c0, tok0, n, hT)
                    S.act(lambda a: a.activation(out=dst[:, tok0:tok0 + n], in_=bA[:, 0:n], func=AF.Copy), r=[bA], w=[dst])
            for c0 in range(0, NT, 4):
                n = min(4, NT - c0)
                bv = bank()
                pvv = bv[:].rearrange("p (a b) -> p a b", a=4)
                for i in range(n):
                    proj_tm(k, pvv[:, i, :], bv, w, 2 * P, P, c0 + i, hT, last=(i == n - 1))
                S.dve(lambda v: v.tensor_copy(out=vaug[:, c0:c0 + n, 0:P], in_=pvv[:, 0:n, :]), r=[bv], w=[vaug])
            bT = None
            NB_ = 3
            for r0 in range(0, 32, NB_):
                rows_ = []
                for r in range(r0, min(r0 + NB_, 32)):
                    rs = min(max(r - 4, 0), 24)
                    if rs % 2 == 0:
                        nl, t0 = 4, rs // 2
                        tau0 = rs - r + 7
                        bias_ap = NB[:, h, tau0:tau0 + 7:2, :]
                    else:
                        nl, t0 = 5, (rs - 1) // 2
                        bias_ap = NB[:, h, 14:19, :]
                    rows_.append(dict(r=r, nl=nl, bias=bias_ap, qs=slice(r * 64, (r + 1) * 64), tiles=[t0 + m for m in range(nl)] + [16, 17],
                                      ee=e_[r % NB_], pt=PT[r % NB_], rc=rc[r % NB_], ob=ob[r % NB_]))
                for x in rows_:
                    x["bS"] = bank()
                    x["pS"] = x["bS"][:, 0:448].rearrange("p (a b) -> p a b", a=7)
                    for m, tt in enumerate(x["tiles"]):
                        S.pe(lambda t: t.matmul(x["pS"][:, m, :], lhsT=kT[:, tt * P:(tt + 1) * P], rhs=qT[:, x["qs"]], start=True, stop=True),
                             r=[kT, qT], w=[x["bS"]], signal=(m == len(x["tiles"]) - 1))
                for x in rows_:
                    nl = x["nl"]
                    S.dve(lambda v: v.scalar_tensor_tensor(out=x["ee"][:, 0:nl, :], in0=x["pS"][:, 0:nl, :], scalar=QK_SCALE, in1=x["bias"],
                                                           op0=ALU.mult, op1=ALU.add), r=[x["bS"], NB], w=[x["ee"]])
                for x in rows_:
                    nl = x["nl"]
                    S.act(lambda a: a.activation(out=x["pt"][:, 0:nl, :], in_=x["ee"][:, 0:nl, :], func=AF.Exp), r=[x["ee"]], w=[x["pt"]])
                    S.act(lambda a: a.activation(out=x["pt"][:, nl:nl + 2, :], in_=x["pS"][:, nl:nl + 2, :], func=AF.Exp, scale=QK_SCALE),
                          r=[x["bS"]], w=[x["pt"]])
                for x in rows_:
                    x["bO"] = bank()
                    for m, tt in enumerate(x["tiles"]):
                        S.pe(lambda t: t.matmul(x["bO"][0:64, 0:P + 1], lhsT=x["pt"][:, m, :], rhs=vaug[:, tt, :], start=(m == 0),
                                                stop=(m == len(x["tiles"]) - 1)), r=[x["pt"], vaug], w=[x["bO"]], signal=(m == len(x["tiles"]) - 1))
                for x in rows_:
                    S.dve(lambda v: v.reciprocal(out=x["rc"][0:64, :], in_=x["bO"][0:64, P:P + 1]), r=[x["bO"]], w=[x["rc"]])
                    S.dve(lambda v: v.tensor_scalar(out=x["ob"][0:64, :], in0=x["bO"][0:64, 0:P], scalar1=x["rc"][0:64, 0:1], scalar2=None,
                                                    op0=ALU.mult), r=[x["bO"], x["rc"]], w=[x["ob"]])
                for x in rows_:
                    r = x["r"]
                    if r % 8 == 0:
                        bT = k.rbank()
                    pT = bT[:].bitcast(BF16)[:, 0:512].rearrange("p (a b) -> p a b", a=8)
                    bTl = bT
                    S.pe(lambda t: t.transpose(out=pT[:, r % 8, :], in_=x["ob"][0:64, :], identity=k.identb[0:64, 0:64]),
                         r=[x["ob"], k.identb], w=[bTl])
                    if r % 8 == 7:
                        rr0 = r - 7
                        S.act(lambda a: a.activation(out=YTs[:, rr0 * 64:rr0 * 64 + 512], in_=bTl[:].bitcast(BF16)[:, 0:512], func=AF.Copy),
                              r=[bTl], w=[YTs])
            if ctx_out:
                bS = bank()
                pS = bS[:].rearrange("p (a b) -> p a b", a=2)
                for u_ in range(2):
                    S.pe(lambda t: t.matmul(pS[:, u_, :], lhsT=kT[:, TL + u_ * P:TL + (u_ + 1) * P], rhs=qT[:, TL:T], start=True, stop=True),
                         r=[kT, qT], w=[bS], signal=(u_ == 1))
                S.act(lambda a: a.activation(out=PTc[:], in_=pS, func=AF.Exp, scale=QK_SCALE), r=[bS], w=[PTc])
                bT = k.rbank()
                pT = bT[:].bitcast(BF16)[:, 0:256].rearrange("p (a b) -> p a b", a=2)
                for qt in range(2):
                    bO = bank()
                    for u_ in range(2):
                        S.pe(lambda t: t.matmul(bO[:, 0:P + 1], lhsT=PTc[:, u_, qt * P:(qt + 1) * P], rhs=vaug[:, 16 + u_, :],
                                                start=(u_ == 0), stop=(u_ == 1)), r=[PTc, vaug], w=[bO], signal=(u_ == 1))
                    rc_, ob_ = rc[qt], ob[qt]
                    S.dve(lambda v: v.reciprocal(out=rc_[:], in_=bO[:, P:P + 1]), r=[bO], w=[rc_])
                    S.dve(lambda v: v.tensor_scalar(out=ob_[:], in0=bO[:, 0:P], scalar1=rc_[:, 0:1], scalar2=None, op0=ALU.mult),
                          r=[bO, rc_], w=[ob_])
                    S.pe(lambda t: t.transpose(out=pT[:, qt, :], in_=ob_[:], identity=k.identb[:]), r=[ob_, k.identb], w=[bT])
                S.act(lambda a: a.activation(out=YTs[:, TL:T], in_=bT[:].bitcast(BF16)[:, 0:256], func=AF.Copy), r=[bT], w=[YTs])
            ncols = T if ctx_out else TL
            S.dma("sync", k.YT[8 + h, :, 0:ncols], YTs[:, 0:ncols], r=[YTs])


def merge1(k, l, hT, mergedT, ctx_out):
    nc, S, sb, cst, bank = k.nc, k.S, k.sb, k.cst, k.bank
    blocks = TB if ctx_out else TB[:4]
    with contextlib.ExitStack() as st:
        wgt_ = [sb(st, "g_wg%d" % i, [P, 8, 3 * P], BF16) for i in range(2)]
        wbr = [sb(st, "g_wb%d" % i, [P, 12, P], BF16) for i in range(2)]
        yt = [sb(st, "g_yt%d" % i, [P, 12, 512], BF16) for i in range(2)]
        gs2 = [[sb(st, "g_gs%d_%d" % (i, j), [P, 512], F32) for i in range(3)] for j in range(2)]
        tn2 = [[sb(st, "g_tn%d_%d" % (i, j), [P, 512], F32) for i in range(3)] for j in range(2)]

        def load_fc(fc):
            wg_, wb_ = wgt_[fc % 2], wbr[fc % 2]
            for n_ in range(3):
                load_w(k, wg_, slice(n_ * P, (n_ + 1) * P), k.w_in[l], 5648 + n_ * D + fc * P, P)
            for n_ in range(3):
                S.dma("gpsimd", wb_[:, n_ * 4:(n_ + 1) * 4, :],
                      k.w_branch[l, n_].rearrange("(c p) n -> p c n", p=P)[:, :, fc * P:(fc + 1) * P], w=[wb_])

        load_fc(0)
        it = 0
        for fc in range(8):
            wg_, wb_ = wgt_[fc % 2], wbr[fc % 2]
            if fc + 1 < 8:
                load_fc(fc + 1)
            for (tok0, n) in blocks:
                y_ = yt[it % 2]
                gs, tn = gs2[it % 2], tn2[it % 2]
                it += 1
                S.dma("sync", y_[:, :, 0:n], k.YT[:, :, tok0:tok0 + n].rearrange("a p t -> p a t"), w=[y_])
                for n_ in range(3):
                    bG = bank()
                    proj_fm(k, bG[:, 0:n], bG, wg_, n_ * P, tok0, n, hT)
                    S.act(lambda a: a.activation(out=gs[n_][:, 0:n], in_=bG[:, 0:n], func=AF.Sigmoid), r=[bG], w=[gs[n_]])
                    bP = bank()
                    for kc in range(4):
                        S.pe(lambda t: t.matmul(bP[:, 0:n], lhsT=wb_[:, n_ * 4 + kc, :], rhs=y_[:, n_ * 4 + kc, 0:n],
                                                start=(kc == 0), stop=(kc == 3)), r=[wb_, y_], w=[bP], signal=(kc == 3))
                    S.dve(lambda v: v.tensor_tensor(out=tn[n_][:, 0:n], in0=bP[:, 0:n], in1=gs[n_][:, 0:n], op=ALU.mult),
                          r=[bP, gs[n_]], w=[tn[n_]])
                S.pool(lambda g: g.tensor_tensor(out=tn[0][:, 0:n], in0=tn[0][:, 0:n], in1=tn[1][:, 0:n], op=ALU.add),
                       r=[tn[0], tn[1]], w=[tn[0]])
                S.pool(lambda g: g.tensor_tensor(out=mergedT[:, fc, tok0:tok0 + n], in0=tn[0][:, 0:n], in1=tn[2][:, 0:n], op=ALU.add),
                       r=[tn[0], tn[2]], w=[mergedT])


def ap4(tile_ap, dims):
    a0 = list(tile_ap.ap[0])
    return bass.AP(tile_ap.tensor, tile_ap.offset, [a0] + [list(d) for d in dims])


def merge2(k, l, mergedT, n_tiles, mod_row):
    nc, S, sb, cst, bank = k.nc, k.S, k.sb, k.cst, k.bank
    NTn = n_tiles
    with contextlib.ExitStack() as st0:
        H2B = sb(st0, "o_H2B", [P, NTn, D], BF16)
        LALL = sb(st0, "o_LALL", [P, NTn, 36], F32)
        with contextlib.ExitStack() as st:
            wout = sb(st, "o_wout", [P, 8, D], BF16)
            S.dma("gpsimd", wout[:, :, 0:512], k.w_out[l].rearrange("(c p) n -> p c n", p=P)[:, :, 0:512], w=[wout])
            S.dma("gpsimd", wout[:, :, 512:D], k.w_out[l].rearrange("(c p) n -> p c n", p=P)[:, :, 512:D], w=[wout])
            wr = sb(st, "o_wr", [P, 8, 36], F32)
            S.dma("sync", wr[:], k.w_gr[l].rearrange("(c p) n -> p c n", p=P), w=[wr])
            rows = {}
            for which in range(2):
                for j, nm in ((2, "G1"), (3, "A2"), (4, "B2")):
                    rows[(which, nm)] = sb(st, "o_row%s%d" % (nm, which), [P, D], F32)
                    load_row_bc(k, rows[(which, nm)], mod_row(which, j))
            xt = [sb(st, "o_xt%d" % i, [P, D], F32) for i in range(2)]
            tmp_2 = [sb(st, "o_tmp0", [P, D], F32)] * 2
            xn = [sb(st, "o_xn%d" % i, [P, D], F32) for i in range(2)]
            h2_2 = [sb(st, "o_h2%d" % i, [P, D], F32) for i in range(2)]
            h2T_2 = [sb(st, "o_h2T0", [P, 8, P], F32)] * 2
            ss_2 = [sb(st, "o_ss%d" % i, [P, 1], F32) for i in range(2)]
            t1_2 = [sb(st, "o_t1%d" % i, [P, 1], F32) for i in range(2)]
            rstd_2 = [sb(st, "o_rstd%d" % i, [P, 1], F32) for i in range(2)]
            def part_a(t):
                which = 0 if t < NTL else 1
                x_, xn_ = xt[t % 2], xn[t % 2]
                tmp = tmp_2[t % 2]
                G1 = rows[(which, "G1")]
                S.dma("sync", x_[:], k.xres[t * P:(t + 1) * P, :], w=[x_])
                for half in range(2):
                    bY = bank()
                    for fc in range(8):
                        S.pe(lambda te: te.matmul(bY[:, :], lhsT=mergedT[:, fc, t * P:(t + 1) * P], rhs=wout[:, fc, half * 512:(half + 1) * 512],
                                                  start=(fc == 0), stop=(fc == 7)), r=[mergedT, wout], w=[bY], signal=(fc == 7))
                    S.dve(lambda v: v.tensor_tensor(out=tmp[:, half * 512:(half + 1) * 512], in0=bY[:, :], in1=G1[:, half * 512:(half + 1) * 512],
                                                    op=ALU.mult), r=[bY, G1], w=[tmp])
                S.dve(lambda v: v.tensor_tensor(out=xn_[:], in0=x_[:], in1=tmp[:], op=ALU.add), r=[x_, tmp], w=[xn_])
                S.dma("sync", k.xres[t * P:(t + 1) * P, :], xn_[:], r=[xn_])

            def part_b(t):
                which = 0 if t < NTL else 1
                xn_ = xn[t % 2]
                tmp, h2, h2T, ss, t1, rstd = tmp_2[t % 2], h2_2[t % 2], h2T_2[t % 2], ss_2[t % 2], t1_2[t % 2], rstd_2[t % 2]
                A2, B2 = rows[(which, "A2")], rows[(which, "B2")]
                S.act(lambda a: a.activation(out=h2[:], in_=xn_[:], func=AF.Square, accum_out=ss[:]), r=[xn_], w=[h2, ss])
                rstd_from_ss(k, ss, t1, rstd, 1.0 / D, EPS)
                S.dve(lambda v: v.scalar_tensor_tensor(out=tmp[:], in0=xn_[:], scalar=rstd[:, 0:1], in1=A2[:], op0=ALU.mult, op1=ALU.mult),
                      r=[xn_, rstd, A2], w=[tmp])
                S.dve(lambda v: v.tensor_tensor(out=h2[:], in0=tmp[:], in1=B2[:], op=ALU.add), r=[tmp, B2], w=[h2])
                S.act(lambda a: a.activation(out=H2B[:, t, :], in_=h2[:], func=AF.Copy), r=[h2], w=[H2B])
                for half in range(2):
                    bT = bank()
                    pT = bT[:].rearrange("p (a b) -> p a b", a=4)
                    for i in range(4):
                        c = half * 4 + i
                        S.pe(lambda te: te.transpose(out=pT[:, i, :], in_=h2[:, c * P:(c + 1) * P], identity=cst("ident")),
                             r=[h2, k.C], w=[bT], signal=(i == 3))
                    if half == 0:
                        S.act(lambda a: a.activation(out=h2T[:, 0:4, :], in_=pT, func=AF.Copy), r=[bT], w=[h2T])
                    else:
                        S.dve(lambda v: v.tensor_copy(out=h2T[:, 4:8, :], in_=pT), r=[bT], w=[h2T])
                bL = bank()
                for kc in range(8):
                    S.pe(lambda te: te.matmul(bL[:, 0:36], lhsT=h2T[:, kc, :], rhs=wr[:, kc, :], start=(kc == 0), stop=(kc == 7)),
                         r=[h2T, wr], w=[bL], signal=(kc == 7))
                S.dve(lambda v: v.tensor_copy(out=LALL[:, t, :], in_=bL[:, 0:36]), r=[bL], w=[LALL])

            part_a(0)
            for t in range(n_tiles):
                if t + 1 < n_tiles:
                    part_a(t + 1)
                part_b(t)
            S.fence()
        with contextlib.ExitStack() as st:
            route_all(k, st, LALL, NTn)
            scb = [Buf("scat0"), Buf("scat1")]
            for t in range(n_tiles):
                for kk in range(2):
                    S.idma(out=k.xs[:, :], out_offset=bass.IndirectOffsetOnAxis(ap=k.dests[:, t, kk:kk + 1], axis=0),
                           in_=H2B[:, t, :], in_offset=None, bounds=XS_ROWS - 1, r=[H2B, k.dests], w=[scb[kk]])
            if k.dbg_route is not None:
                dbg = sb(st, "o_dbg", [P, NT, 6], F32)
                S.dve(lambda v: v.tensor_copy(out=dbg[:, :, 0:2], in_=k.dests[:]), r=[k.dests], w=[dbg])
                S.dve(lambda v: v.tensor_copy(out=dbg[:, :, 2:4], in_=k.destg[:]), r=[k.destg], w=[dbg])
                S.dve(lambda v: v.tensor_copy(out=dbg[:, :, 4:6], in_=k.wgt[:]), r=[k.wgt], w=[dbg])
                S.dma("sync", k.dbg_route, dbg[:], r=[dbg])
            S.fence()


def route_all(k, st, LALL, NTn):
    S, sb, cst, bank = k.S, k.sb, k.cst, k.bank
    f3 = lambda nm, w: sb(st, "q_" + nm, [P, NTn, w], F32)
    f2 = lambda nm: sb(st, "q_" + nm, [P, NTn], F32)
    gmax, gsum, gw, t1, t2, dd, e2, rden, w1 = (f2(n) for n in ("gmax", "gsum", "gw", "t1", "t2", "dd", "e2", "rden", "w1"))
    gd, gm, pen = f3("gd", 4), f3("gm", 4), f3("pen", 4)
    mk, sel1, sel2, sel12, pos, CS, BASE, okm, t32 = (f3(n, NE) for n in ("mk", "sel1", "sel2", "sel12", "pos", "CS", "BASE", "okm", "t32"))
    dv, ok, ds, dg = (sb(st, "q_" + n, [P, 2, NTn], F32) for n in ("dv", "ok", "ds", "dg"))
    GL = LALL[:, :, 0:4]
    S.dve(lambda v: v.tensor_reduce(out=gmax[:], in_=GL, axis=AX.X, op=ALU.max), r=[LALL], w=[gmax])
    S.dve(lambda v: v.tensor_tensor(out=gd[:], in0=GL, in1=bc_last(gmax[:], 4), op=ALU.subtract), r=[LALL, gmax], w=[gd])
    S.dve(lambda v: v.tensor_tensor(out=gm[:], in0=GL, in1=bc_last(gmax[:], 4), op=ALU.is_ge), r=[LALL, gmax], w=[gm])
    S.act(lambda a: a.activation(out=gd[:], in_=gd[:], func=AF.Exp), r=[gd], w=[gd])
    S.dve(lambda v: v.tensor_reduce(out=gsum[:], in_=gd[:], axis=AX.X, op=ALU.add), r=[gd], w=[gsum])
    S.dve(lambda v: v.reciprocal(out=gw[:], in_=gsum[:]), r=[gsum], w=[gw])
    S.dve(lambda v: v.tensor_scalar(out=pen[:], in0=gm[:], scalar1=BIG, scalar2=-BIG, op0=ALU.mult, op1=ALU.add), r=[gm], w=[pen])
    el4 = ap4(LALL[:, :, 4:36], [[36, NTn], [8, 4], [1, 8]])
    pen4 = ap4(pen[:], [[4, NTn], [1, 4], [0, 8]])
    mk4 = ap4(mk[:], [[NE, NTn], [8, 4], [1, 8]])
    S.dve(lambda v: v.tensor_tensor(out=mk4, in0=el4, in1=pen4, op=ALU.add), r=[LALL, pen], w=[mk])
    S.dve(lambda v: v.tensor_reduce(out=t1[:], in_=mk[:], axis=AX.X, op=ALU.max), r=[mk], w=[t1])
    S.dve(lambda v: v.tensor_tensor(out=sel1[:], in0=mk[:], in1=bc_last(t1[:], NE), op=ALU.is_ge), r=[mk, t1], w=[sel1])
    S.dve(lambda v: v.scalar_tensor_tensor(out=mk[:], in0=sel1[:], scalar=-BIG, in1=mk[:], op0=ALU.mult, op1=ALU.add), r=[sel1, mk], w=[mk])
    S.dve(lambda v: v.tensor_reduce(out=t2[:], in_=mk[:], axis=AX.X, op=ALU.max), r=[mk], w=[t2])
    S.dve(lambda v: v.tensor_tensor(out=sel2[:], in0=mk[:], in1=bc_last(t2[:], NE), op=ALU.is_ge), r=[mk, t2], w=[sel2])
    S.dve(lambda v: v.tensor_tensor(out=sel12[:], in0=sel1[:], in1=sel2[:], op=ALU.add), r=[sel1, sel2], w=[sel12])
    S.dve(lambda v: v.tensor_tensor(out=dd[:], in0=t2[:], in1=t1[:], op=ALU.subtract), r=[t1, t2], w=[dd])
    S.act(lambda a: a.activation(out=e2[:], in_=dd[:], func=AF.Exp), r=[dd], w=[e2])
    S.dve(lambda v: v.tensor_scalar(out=rden[:], in0=e2[:], scalar1=1.0, scalar2=None, op0=ALU.add), r=[e2], w=[rden])
    S.dve(lambda v: v.reciprocal(out=rden[:], in_=rden[:]), r=[rden], w=[rden])
    S.dve(lambda v: v.tensor_tensor(out=w1[:], in0=gw[:], in1=rden[:], op=ALU.mult), r=[gw, rden], w=[w1])
    NB9 = 9
    for g0 in range(0, NTn, NB9):
        n = min(NB9, NTn - g0)
        bP = bank()
        bC = bank()
        for i in range(n):
            t = g0 + i
            S.pe(lambda te: te.matmul(bP[:, i * NE:(i + 1) * NE], lhsT=cst("tris"), rhs=sel12[:, t, :], start=True, stop=True),
                 r=[k.C, sel12], w=[bP], signal=(i == n - 1))
        for i in range(n):
            t = g0 + i
            S.pe(lambda te: te.matmul(bC[:, i * NE:(i + 1) * NE], lhsT=cst("ones"), rhs=sel12[:, t, :], start=True, stop=True),
                 r=[k.C, sel12], w=[bC], signal=(i == n - 1))
        S.dve(lambda v: v.tensor_copy(out=pos[:, g0:g0 + n, :], in_=bP[:, 0:n * NE].rearrange("p (a b) -> p a b", a=n)), r=[bP], w=[pos])
        S.dve(lambda v: v.tensor_copy(out=CS[:, g0:g0 + n, :], in_=bC[:, 0:n * NE].rearrange("p (a b) -> p a b", a=n)), r=[bC], w=[CS])
    S.dve(lambda v: v.memset(BASE[:, 0, :], 0.0), w=[BASE])
    for t in range(1, NTn):
        S.dve(lambda v: v.tensor_tensor(out=BASE[:, t, :], in0=BASE[:, t - 1, :], in1=CS[:, t - 1, :], op=ALU.add), r=[BASE, CS], w=[BASE])
    S.dve(lambda v: v.tensor_tensor(out=pos[:], in0=pos[:], in1=BASE[:], op=ALU.add), r=[pos, BASE], w=[pos])
    S.dve(lambda v: v.tensor_scalar(out=okm[:], in0=pos[:], scalar1=float(CAP), scalar2=None, op0=ALU.is_lt), r=[pos], w=[okm])
    S.dve(lambda v: v.tensor_tensor(out=pos[:], in0=pos[:], in1=bc_mid(cst("ecap"), NTn), op=ALU.add), r=[pos, k.C], w=[pos])
    for kk, sel in enumerate((sel1, sel2)):
        S.dve(lambda v: v.tensor_tensor(out=t32[:], in0=pos[:], in1=sel[:], op=ALU.mult), r=[pos, sel], w=[t32])
        S.dve(lambda v: v.tensor_reduce(out=dv[:, kk, :], in_=t32[:], axis=AX.X, op=ALU.add), r=[t32], w=[dv])
        S.dve(lambda v: v.tensor_tensor(out=t32[:], in0=okm[:], in1=sel[:], op=ALU.mult), r=[okm, sel], w=[t32])
        S.dve(lambda v: v.tensor_reduce(out=ok[:, kk, :], in_=t32[:], axis=AX.X, op=ALU.add), r=[t32], w=[ok])
    S.dve(lambda v: v.tensor_scalar(out=ds[:], in0=ok[:], scalar1=-1.0e6, scalar2=1.0e6, op0=ALU.mult, op1=ALU.add), r=[ok], w=[ds])
    S.dve(lambda v: v.tensor_tensor(out=ds[:], in0=ds[:], in1=dv[:], op=ALU.add), r=[ds, dv], w=[ds])
    S.dve(lambda v: v.tensor_scalar(out=dg[:], in0=ds[:], scalar1=float(XS_ROWS), scalar2=None, op0=ALU.min), r=[ds], w=[dg])
    for kk in range(2):
        S.dve(lambda v: v.tensor_copy(out=k.dests[:, 0:NTn, kk], in_=ds[:, kk, :]), r=[ds], w=[k.dests])
        S.dve(lambda v: v.tensor_copy(out=k.destg[:, 0:NTn, kk], in_=dg[:, kk, :]), r=[dg], w=[k.destg])
    S.dve(lambda v: v.tensor_tensor(out=k.wgt[:, 0:NTn, 0], in0=w1[:], in1=ok[:, 0, :], op=ALU.mult), r=[w1, ok], w=[k.wgt])
    S.dve(lambda v: v.tensor_tensor(out=w1[:], in0=w1[:], in1=e2[:], op=ALU.mult), r=[w1, e2], w=[w1])
    S.dve(lambda v: v.tensor_tensor(out=k.wgt[:, 0:NTn, 1], in0=w1[:], in1=ok[:, 1, :], op=ALU.mult), r=[w1, ok], w=[k.wgt])


def route_tile(k, t, L, base, rt):
    S, cst, bank = k.S, k.cst, k.bank
    sc = rt["sc"]
    gl = L[:, 0:4]
    S.dve(lambda v: v.reduce_max(out=sc[:, 0:1], in_=gl, axis=AX.X), r=[L], w=[sc])
    S.dve(lambda v: v.tensor_scalar(out=sc[:, 1:2], in0=sc[:, 0:1], scalar1=-1.0, scalar2=None, op0=ALU.mult), r=[sc], w=[sc])
    S.act(lambda a: a.activation(out=rt["ge"][:], in_=gl, func=AF.Exp, bias=sc[:, 1:2], accum_out=sc[:, 2:3]), r=[L, sc], w=[rt["ge"], sc])
    S.dve(lambda v: v.reciprocal(out=sc[:, 3:4], in_=sc[:, 2:3]), r=[sc], w=[sc])
    S.dve(lambda v: v.tensor_scalar(out=rt["gm"][:], in0=gl, scalar1=sc[:, 0:1], scalar2=None, op0=ALU.is_ge), r=[L, sc], w=[rt["gm"]])
    S.dve(lambda v: v.tensor_scalar(out=rt["pen"][:], in0=rt["gm"][:], scalar1=BIG, scalar2=-BIG, op0=ALU.mult, op1=ALU.add),
          r=[rt["gm"]], w=[rt["pen"]])
    for g in range(4):
        S.dve(lambda v: v.tensor_scalar(out=rt["mk"][:, g * 8:(g + 1) * 8], in0=L[:, 4 + g * 8:12 + g * 8], scalar1=rt["pen"][:, g:g + 1],
                                        scalar2=None, op0=ALU.add), r=[L, rt["pen"]], w=[rt["mk"]])
    S.dve(lambda v: v.max(out=rt["top"][:], in_=rt["mk"][:]), r=[rt["mk"]], w=[rt["top"]])
    S.dve(lambda v: v.tensor_scalar(out=rt["sel1"][:], in0=rt["mk"][:], scalar1=rt["top"][:, 0:1], scalar2=None, op0=ALU.is_ge),
          r=[rt["mk"], rt["top"]], w=[rt["sel1"]])
    S.dve(lambda v: v.tensor_scalar(out=rt["sel12"][:], in0=rt["mk"][:], scalar1=rt["top"][:, 1:2], scalar2=None, op0=ALU.is_ge),
          r=[rt["mk"], rt["top"]], w=[rt["sel12"]])
    S.dve(lambda v: v.tensor_tensor(out=rt["sel2"][:], in0=rt["sel12"][:], in1=rt["sel1"][:], op=ALU.subtract),
          r=[rt["sel12"], rt["sel1"]], w=[rt["sel2"]])
    S.dve(lambda v: v.tensor_tensor(out=sc[:, 4:5], in0=rt["top"][:, 1:2], in1=rt["top"][:, 0:1], op=ALU.subtract), r=[rt["top"]], w=[sc])
    S.act(lambda a: a.activation(out=sc[:, 5:6], in_=sc[:, 4:5], func=AF.Exp), r=[sc], w=[sc])
    S.dve(lambda v: v.tensor_scalar(out=sc[:, 6:7], in0=sc[:, 5:6], scalar1=1.0, scalar2=None, op0=ALU.add), r=[sc], w=[sc])
    S.dve(lambda v: v.reciprocal(out=sc[:, 7:8], in_=sc[:, 6:7]), r=[sc], w=[sc])
    bP = bank()
    S.pe(lambda te: te.matmul(bP[:, 0:NE], lhsT=cst("tris"), rhs=rt["sel12"][:], start=True, stop=True), r=[k.C, rt["sel12"]], w=[bP])
    S.dve(lambda v: v.tensor_tensor(out=rt["pos"][:], in0=bP[:, 0:NE], in1=base[:], op=ALU.add), r=[bP, base], w=[rt["pos"]])
    bC = bank()
    S.pe(lambda te: te.matmul(bC[:, 0:NE], lhsT=cst("ones"), rhs=rt["sel12"][:], start=True, stop=True), r=[k.C, rt["sel12"]], w=[bC])
    S.dve(lambda v: v.tensor_tensor(out=base[:], in0=base[:], in1=bC[:, 0:NE], op=ALU.add), r=[base, bC], w=[base])
    S.dve(lambda v: v.tensor_scalar(out=rt["okm"][:], in0=rt["pos"][:], scalar1=float(CAP), scalar2=None, op0=ALU.is_lt), r=[rt["pos"]], w=[rt["okm"]])
    S.dve(lambda v: v.tensor_tensor(out=rt["pos"][:], in0=rt["pos"][:], in1=cst("ecap"), op=ALU.add), r=[rt["pos"], k.C], w=[rt["pos"]])
    for kk, sel in enumerate((rt["sel1"], rt["sel2"])):
        S.dve(lambda v: v.tensor_tensor(out=rt["t32"][:], in0=rt["pos"][:], in1=sel[:], op=ALU.mult), r=[rt["pos"], sel], w=[rt["t32"]])
        S.dve(lambda v: v.reduce_sum(out=rt["dv"][:, kk:kk + 1], in_=rt["t32"][:], axis=AX.X), r=[rt["t32"]], w=[rt["dv"]])
        S.dve(lambda v: v.tensor_tensor(out=rt["t32"][:], in0=rt["okm"][:], in1=sel[:], op=ALU.mult), r=[rt["okm"], sel], w=[rt["t32"]])
        S.dve(lambda v: v.reduce_sum(out=rt["ok"][:, kk:kk + 1], in_=rt["t32"][:], axis=AX.X), r=[rt["t32"]], w=[rt["ok"]])
    S.dve(lambda v: v.tensor_scalar(out=rt["ds"][:], in0=rt["ok"][:], scalar1=-1.0e6, scalar2=1.0e6, op0=ALU.mult, op1=ALU.add), r=[rt["ok"]], w=[rt["ds"]])
    S.dve(lambda v: v.tensor_tensor(out=rt["ds"][:], in0=rt["ds"][:], in1=rt["dv"][:], op=ALU.add), r=[rt["ds"], rt["dv"]], w=[rt["ds"]])
    S.dve(lambda v: v.tensor_scalar(out=rt["dg"][:], in0=rt["ds"][:], scalar1=float(XS_ROWS), scalar2=None, op0=ALU.min), r=[rt["ds"]], w=[rt["dg"]])
    S.dve(lambda v: v.tensor_copy(out=k.dests[:, t, :], in_=rt["ds"][:]), r=[rt["ds"]], w=[k.dests])
    S.dve(lambda v: v.tensor_copy(out=k.destg[:, t, :], in_=rt["dg"][:]), r=[rt["dg"]], w=[k.destg])
    S.dve(lambda v: v.tensor_tensor(out=sc[:, 8:9], in0=sc[:, 3:4], in1=sc[:, 7:8], op=ALU.mult), r=[sc], w=[sc])
    S.dve(lambda v: v.tensor_tensor(out=k.wgt[:, t, 0:1], in0=sc[:, 8:9], in1=rt["ok"][:, 0:1], op=ALU.mult), r=[sc, rt["ok"]], w=[k.wgt])
    S.dve(lambda v: v.tensor_tensor(out=sc[:, 9:10], in0=sc[:, 8:9], in1=sc[:, 5:6], op=ALU.mult), r=[sc], w=[sc])
    S.dve(lambda v: v.tensor_tensor(out=k.wgt[:, t, 1:2], in0=sc[:, 9:10], in1=rt["ok"][:, 1:2], op=ALU.mult), r=[sc, rt["ok"]], w=[k.wgt])


def moe(k, l):
    nc, S, sb, cst, bank = k.nc, k.S, k.sb, k.cst, k.bank
    with contextlib.ExitStack() as st:
        wg = [sb(st, "e_wg%d" % i, [P, 8, 512], BF16) for i in range(2)]
        wu = [sb(st, "e_wu%d" % i, [P, 8, 512], BF16) for i in range(2)]
        wd = [sb(st, "e_wd%d" % i, [P, 4, D], BF16) for i in range(2)]
        xrow = [sb(st, "e_xrow%d" % i, [P, D], BF16) for i in range(2)]
        xsT2 = [sb(st, "e_xsT%d" % i, [P, 8, CAP], BF16) for i in range(2)]
        AT = sb(st, "e_AT", [P, 4, CAP], BF16)
        sg = [sb(st, "e_sg%d" % i, [P, CAP], BF16) for i in range(2)]
        ysb = [sb(st, "e_ysb%d" % i, [P, D], BF16) for i in range(2)]

        def load_e(e):
            S.dma("gpsimd", wg[e % 2][:], k.w_eg[l, e].rearrange("(c p) n -> p c n", p=P), w=[wg[e % 2]])
            S.dma("gpsimd", wu[e % 2][:], k.w_eu[l, e].rearrange("(c p) n -> p c n", p=P), w=[wu[e % 2]])
            S.dma("gpsimd", wd[e % 2][:], k.w_ed[l, e].rearrange("(c p) n -> p c n", p=P), w=[wd[e % 2]])

        load_e(0)
        itc = [0]

        def xs_transposes(e):
            xsT = xsT2[e % 2]
            for rt_ in range(CAP // P):
                xr = xrow[itc[0] % 2]
                itc[0] += 1
                r0 = e * CAP + rt_ * P
                S.dma("sync", xr[:], k.xs[r0:r0 + P, :], w=[xr])
                b = bank()
                pv = b[:].bitcast(BF16).rearrange("p (a b) -> p a b", a=8)
                for c in range(8):
                    S.pe(lambda te: te.transpose(out=pv[:, c, :], in_=xr[:, c * P:(c + 1) * P], identity=k.identb[:]),
                         r=[xr, k.identb], w=[b], signal=(c == 7))
                if rt_ % 2 == 0:
                    S.act(lambda a: a.activation(out=xsT[:, :, rt_ * P:(rt_ + 1) * P], in_=pv, func=AF.Copy), r=[b], w=[xsT])
                else:
                    S.dve(lambda v: v.tensor_copy(out=xsT[:, :, rt_ * P:(rt_ + 1) * P], in_=pv), r=[b], w=[xsT])

        xs_transposes(0)
        for e in range(NE):
            if e + 1 < NE:
                load_e(e + 1)
            wg_, wu_, wd_ = wg[e % 2], wu[e % 2], wd[e % 2]
            xsT = xsT2[e % 2]
            for fc in range(4):
                bG = bank()
                bU = bank()
                for kc in range(8):
                    S.pe(lambda te: te.matmul(bG[:, 0:CAP], lhsT=wg_[:, kc, fc * P:(fc + 1) * P], rhs=xsT[:, kc, :], start=(kc == 0), stop=(kc == 7)),
                         r=[wg_, xsT], w=[bG], signal=(kc == 7))
                for kc in range(8):
                    S.pe(lambda te: te.matmul(bU[:, 0:CAP], lhsT=wu_[:, kc, fc * P:(fc + 1) * P], rhs=xsT[:, kc, :], start=(kc == 0), stop=(kc == 7)),
                         r=[wu_, xsT], w=[bU], signal=(kc == 7))
                sg_ = sg[fc % 2]
                S.act(lambda a: a.activation(out=sg_[:], in_=bG[:, 0:CAP], func=AF.Silu), r=[bG], w=[sg_])
                S.dve(lambda v: v.tensor_tensor(out=AT[:, fc, :], in0=bU[:, 0:CAP], in1=sg_[:], op=ALU.mult), r=[bU, sg_], w=[AT])
            if e + 1 < NE:
                xs_transposes(e + 1)
            for rt_ in range(CAP // P):
                yb_ = ysb[rt_ % 2]
                for half in range(2):
                    bY = bank()
                    for fc in range(4):
                        S.pe(lambda te: te.matmul(bY[:, :], lhsT=AT[:, fc, rt_ * P:(rt_ + 1) * P], rhs=wd_[:, fc, half * 512:(half + 1) * 512],
                                                  start=(fc == 0), stop=(fc == 3)), r=[AT, wd_], w=[bY], signal=(fc == 3))
                    if half == 0:
                        S.act(lambda a: a.activation(out=yb_[:, 0:512], in_=bY[:, :], func=AF.Copy), r=[bY], w=[yb_])
                    else:
                        S.dve(lambda v: v.tensor_copy(out=yb_[:, 512:D], in_=bY[:, :]), r=[bY], w=[yb_])
                r0 = e * CAP + rt_ * P
                S.dma("sync", k.ys[r0:r0 + P, :], yb_[:], r=[yb_])


def combine(k, l, n_tiles, mod_row, final):
    nc, S, sb, cst, bank = k.nc, k.S, k.sb, k.cst, k.bank
    with contextlib.ExitStack() as st:
        G2 = [sb(st, "c_G2_%d" % i, [P, D], F32) for i in range(2)]
        for which in range(2):
            load_row_bc(k, G2[which], mod_row(which, 5))
        fg = None
        if final:
            fg = sb(st, "c_fg", [P, D], F32)
            load_row_bc(k, fg, k.final_g[0:1, :])
        y1 = [sb(st, "c_y1_%d" % i, [P, D], BF16) for i in range(2)]
        y2 = [sb(st, "c_y2_%d" % i, [P, D], BF16) for i in range(2)]
        xt = [sb(st, "c_xt%d" % i, [P, D], F32) for i in range(2)]
        f2 = [sb(st, "c_f%d" % i, [P, D], F32) for i in range(2)]
        xn = [sb(st, "c_xn%d" % i, [P, D], F32) for i in range(2)]
        ss = sb(st, "c_ss", [P, 1], F32)
        t1 = sb(st, "c_t1", [P, 1], F32)
        rstd = sb(st, "c_rstd", [P, 1], F32)
        for t in range(n_tiles):
            which = 0 if t < NTL else 1
            a, b2, x_, xn_ = y1[t % 2], y2[t % 2], xt[t % 2], xn[t % 2]
            f = f2[t % 2]
            S.idma(out=a[:, :], out_offset=None, in_=k.ys[:, :], in_offset=bass.IndirectOffsetOnAxis(ap=k.destg[:, t, 0:1], axis=0),
                   bounds=XS_ROWS + P - 1, r=[k.destg], w=[a])
            S.idma(out=b2[:, :], out_offset=None, in_=k.ys[:, :], in_offset=bass.IndirectOffsetOnAxis(ap=k.destg[:, t, 1:2], axis=0),
                   bounds=XS_ROWS + P - 1, r=[k.destg], w=[b2])
            S.dma("sync", x_[:], k.xres[t * P:(t + 1) * P, :], w=[x_])
            S.dve(lambda v: v.tensor_scalar(out=f[:], in0=a[:], scalar1=k.wgt[:, t, 0:1], scalar2=None, op0=ALU.mult), r=[a, k.wgt], w=[f])
            S.dve(lambda v: v.scalar_tensor_tensor(out=f[:], in0=b2[:], scalar=k.wgt[:, t, 1:2], in1=f[:], op0=ALU.mult, op1=ALU.add),
                  r=[b2, k.wgt, f], w=[f])
            S.dve(lambda v: v.tensor_tensor(out=f[:], in0=f[:], in1=G2[which][:], op=ALU.mult), r=[f, G2[which]], w=[f])
            S.dve(lambda v: v.tensor_tensor(out=xn_[:], in0=x_[:], in1=f[:], op=ALU.add), r=[x_, f], w=[xn_])
            if not final:
                S.dma("sync", k.xres[t * P:(t + 1) * P, :], xn_[:], r=[xn_])
            else:
                if "xres" in k.debug:
                    S.dma("sync", k.xres[t * P:(t + 1) * P, :], xn_[:], r=[xn_])
                if t >= NTL:
                    continue
                S.act(lambda a_: a_.activation(out=f[:], in_=xn_[:], func=AF.Square, accum_out=ss[:]), r=[xn_, f], w=[f, ss])
                rstd_from_ss(k, ss, t1, rstd, 1.0 / D, EPS)
                S.dve(lambda v: v.scalar_tensor_tensor(out=xn_[:], in0=xn_[:], scalar=rstd[:, 0:1], in1=fg[:], op0=ALU.mult, op1=ALU.mult),
                      r=[xn_, rstd, fg], w=[xn_])
                S.dma("sync", k.out[t * P:(t + 1) * P, :], xn_[:], r=[xn_])


def host_inputs(inputs, b, shared=None):
    f = lambda a: np.ascontiguousarray(np.asarray(a, dtype=np.float32))
    if shared is None:
        shared = {}
        perm = _swap_perm()
        w_in = f(inputs["w_in"])
        cols = []
        for base in (0, 512):
            for h in range(4):
                cols.append(base + h * P + perm)
        cols = np.concatenate(cols)
        shared["w_in"] = w_in
        shared["w_swap"] = np.ascontiguousarray(w_in[:, :, cols])
        shared["w_mod"] = f(inputs["w_mod"])
        shared["b_mod"] = f(inputs["b_mod"])
        shared["norm1_g"] = f(inputs["norm1_g"])
        shared["norm2_g"] = f(inputs["norm2_g"])
        shared["ret_decay"] = f(inputs["ret_decay"]).reshape(DEPTH, 8)
        shared["ret_norm_g"] = f(inputs["ret_norm_g"])
        cw = f(inputs["conv_w"])
        shared["convT"] = np.ascontiguousarray(cw.reshape(DEPTH, 5, 8, P).transpose(0, 3, 2, 1))
        shared["convbT"] = np.ascontiguousarray(f(inputs["conv_b"]).reshape(DEPTH, 8, P).transpose(0, 2, 1))
        shared["gate_b"] = f(inputs["mlstm_gate_b"]).reshape(DEPTH, 16)
        shared["mlstm_norm_g"] = f(inputs["mlstm_norm_g"])
        shared["na_bias"] = _na_bias(f(inputs["na_rpb"]))
        shared["w_branch"] = f(inputs["w_branch"])
        shared["w_out"] = f(inputs["w_out"])
        shared["w_gr"] = np.ascontiguousarray(np.concatenate([f(inputs["w_group"]), f(inputs["w_router"])], axis=-1))
        shared["w_eg"] = f(inputs["w_expert_gate"])
        shared["w_eu"] = f(inputs["w_expert_up"])
        shared["w_ed"] = f(inputs["w_expert_down"])
        shared["final_g"] = f(inputs["final_norm_g"]).reshape(1, D)
        shared["consts"] = CONST_NP
        shared["rope"] = _rope_tables()
        shared["c_ctx"] = f(inputs["c_ctx"])
    m = dict(shared)
    c_ctx = m.pop("c_ctx")
    cT = np.stack([f(inputs["c"])[b].reshape(8, P).T, c_ctx.reshape(8, P).T], axis=-1)
    m["cT"] = np.ascontiguousarray(cT)
    m["x"] = f(inputs["x"])[b]
    m["ctx"] = f(inputs["ctx"])[b]
    return m, shared


_NC_CACHE = {}


def kernel(**inputs):
    if "nc" not in _NC_CACHE:
        _NC_CACHE["nc"] = build()
    nc = _NC_CACHE["nc"]
    in_maps = []
    shared = None
    for b in range(8):
        m, shared = host_inputs(inputs, b, shared)
        in_maps.append(m)
    res = run_bass_kernel_spmd(nc, in_maps, core_ids=list(range(8)))
    return np.stack([np.asarray(r["out"], dtype=np.float32) for r in res.results], axis=0)
```
